# Optimizing a Trainium2 kernel written in Bass

```python
import jax, jax.numpy as jnp
from jax import lax
import numpy as np

D_MODEL = 1024
BATCH = 16
SEQ = 2048
DEPTH = 1

MLA_HEADS = 8
MLA_Q_RANK = 384
MLA_KV_RANK = 256
MLA_NOPE = 64
MLA_ROPE = 32
MLA_V = 64
MLA_WIDTH = MLA_HEADS * MLA_V
ROPE_THETA = 10000.0
Q_BLOCK = 128

GLA_HEADS = 4
GLA_HEAD_K = 64
GLA_HEAD_V = 128
GLA_DK = GLA_HEADS * GLA_HEAD_K
GLA_DV = GLA_HEADS * GLA_HEAD_V
GLA_GATE_RANK = 16
GLA_GATE_NORMALIZER = 16.0
GLA_CHUNK = 64

PLE_DIM = 256

EPS = 1e-6

IN_SPLITS = (
    MLA_Q_RANK,
    MLA_KV_RANK,
    MLA_ROPE,
    MLA_WIDTH,
    GLA_DK,
    GLA_DK,
    GLA_DV,
    GLA_GATE_RANK,
    GLA_DV,
    D_MODEL,
    D_MODEL,
)
IN_WIDTH = sum(IN_SPLITS)

kernel_name = 'hybrid_mla_gla_gated_merge_ple'


def rmsnorm(x, g):
    xf = x.astype(jnp.float32)
    y = xf * lax.rsqrt(jnp.mean(xf * xf, axis=-1, keepdims=True) + EPS)
    return (y * g.astype(jnp.float32)).astype(x.dtype)


def rope(x, pos):
    r = x.shape[-1]
    inv_freq = 1.0 / (ROPE_THETA ** (jnp.arange(0, r, 2, dtype=jnp.float32) / r))
    ang = pos.astype(jnp.float32)[..., None] * inv_freq
    cos = jnp.cos(ang)[:, :, None, :]
    sin = jnp.sin(ang)[:, :, None, :]
    xf = x.astype(jnp.float32)
    x1, x2 = jnp.split(xf, 2, axis=-1)
    out = jnp.concatenate([x1 * cos - x2 * sin, x2 * cos + x1 * sin], axis=-1)
    return out.astype(x.dtype)


def mla_attention(q_nope, q_rope, k_nope, k_rope, v):
    b, s, h, _ = q_nope.shape
    nb = s // Q_BLOCK
    scale = (MLA_NOPE + MLA_ROPE) ** -0.5
    qn = q_nope.reshape(b, nb, Q_BLOCK, h, MLA_NOPE).transpose(1, 0, 2, 3, 4)
    qr = q_rope.reshape(b, nb, Q_BLOCK, h, MLA_ROPE).transpose(1, 0, 2, 3, 4)
    kpos = jnp.arange(s)

    def block(args):
        qn_i, qr_i, i = args
        sc = (jnp.einsum('bqhd,bkhd->bhqk', qn_i, k_nope).astype(jnp.float32)
              + jnp.einsum('bqhr,bkr->bhqk', qr_i, k_rope).astype(jnp.float32)) * scale
        qpos = i * Q_BLOCK + jnp.arange(Q_BLOCK)
        mask = kpos[None, :] <= qpos[:, None]
        sc = jnp.where(mask[None, None], sc, -jnp.inf)
        pr = jax.nn.softmax(sc, axis=-1).astype(v.dtype)
        return jnp.einsum('bhqk,bkhd->bqhd', pr, v)

    o = lax.map(block, (qn, qr, jnp.arange(nb)))
    return o.transpose(1, 0, 2, 3, 4).reshape(b, s, h, MLA_V)


def gla_chunked(q, k, v, g):
    b, s, h, dk = q.shape
    dv = v.shape[-1]
    c = GLA_CHUNK
    n = s // c

    def to_chunks(t):
        return t.astype(jnp.float32).reshape(b, n, c, h, t.shape[-1]).transpose(1, 0, 3, 2, 4)

    qc = to_chunks(q) * (dk ** -0.5)
    kc = to_chunks(k)
    vc = to_chunks(v)
    bc = jnp.cumsum(to_chunks(g), axis=3)
    causal = jnp.tril(jnp.ones((c, c), dtype=bool))

    def step(state, inp):
        qi, ki, vi, bi = inp
        o_inter = jnp.einsum('bhcd,bhde->bhce', qi * jnp.exp(bi), state)
        diff = bi[:, :, :, None, :] - bi[:, :, None, :, :]
        decay = jnp.exp(jnp.where(causal[None, None, :, :, None], diff, -jnp.inf))
        attn = jnp.einsum('bhid,bhjd,bhijd->bhij', qi, ki, decay)
        o = o_inter + jnp.einsum('bhij,bhje->bhie', attn, vi)
        b_last = bi[:, :, -1:, :]
        k_dec = ki * jnp.exp(b_last - bi)
        state = (state * jnp.exp(b_last[:, :, 0, :])[..., None]
                 + jnp.einsum('bhcd,bhce->bhde', k_dec, vi))
        return state, o

    s0 = jnp.zeros((b, h, dk, dv), jnp.float32)
    _, o = lax.scan(step, s0, (qc, kc, vc, bc))
    return o.transpose(1, 0, 3, 2, 4).reshape(b, s, h, dv)


def split_in(proj):
    offsets = [int(o) for o in np.cumsum(IN_SPLITS)[:-1]]
    return jnp.split(proj, offsets, axis=-1)


def setup_inputs(seed: int = 0) -> dict:
    key = jax.random.key(seed)
    ks = jax.random.split(key, 24)
    f32 = jnp.float32

    def w(k, shape, fan_in):
        return jax.random.normal(k, shape, f32) * (fan_in ** -0.5)

    def gain(k, shape):
        return 1.0 + 0.02 * jax.random.normal(k, shape, f32)

    x = jax.random.normal(ks[0], (BATCH, SEQ, D_MODEL), f32)
    p = jax.random.normal(ks[1], (DEPTH, BATCH, SEQ, PLE_DIM), f32)
    offs = jax.random.randint(ks[2], (BATCH, 1), 0, 4096, dtype=jnp.int32)
    positions = offs + jnp.arange(SEQ, dtype=jnp.int32)[None, :]
    return {
        'x': x,
        'p': p,
        'positions': positions,
        'norm_in_g': gain(ks[3], (DEPTH, D_MODEL)),
        'w_in': w(ks[4], (DEPTH, D_MODEL, IN_WIDTH), D_MODEL),
        'q_norm_g': gain(ks[5], (DEPTH, MLA_Q_RANK)),
        'w_uq': w(ks[6], (DEPTH, MLA_Q_RANK, MLA_HEADS * (MLA_NOPE + MLA_ROPE)), MLA_Q_RANK),
        'kv_norm_g': gain(ks[7], (DEPTH, MLA_KV_RANK)),
        'w_ukv': w(ks[8], (DEPTH, MLA_KV_RANK, MLA_HEADS * (MLA_NOPE + MLA_V)), MLA_KV_RANK),
        'w_gk_up': w(ks[9], (DEPTH, GLA_GATE_RANK, GLA_DK), GLA_GATE_RANK),
        'b_gk': 0.1 * jax.random.normal(ks[10], (DEPTH, GLA_DK), f32),
        'gla_norm_g': gain(ks[11], (DEPTH, GLA_HEAD_V)),
        'w_mla_br': w(ks[12], (DEPTH, MLA_WIDTH, D_MODEL), MLA_WIDTH),
        'w_gla_br': w(ks[13], (DEPTH, GLA_DV, D_MODEL), GLA_DV),
        'w_out': w(ks[14], (DEPTH, D_MODEL, D_MODEL), D_MODEL),
        'w_ple': w(ks[15], (DEPTH, PLE_DIM, D_MODEL), PLE_DIM),
        'ple_norm_g': gain(ks[16], (DEPTH, D_MODEL)),
        'ple_gate_norm_g': gain(ks[17], (DEPTH, D_MODEL)),
        'w_ple_gate': w(ks[18], (DEPTH, D_MODEL, D_MODEL), D_MODEL),
        'final_norm_g': gain(ks[19], (D_MODEL,)),
    }


def reference(x, p, positions, norm_in_g, w_in, q_norm_g, w_uq, kv_norm_g, w_ukv,
              w_gk_up, b_gk, gla_norm_g, w_mla_br, w_gla_br, w_out, w_ple, ple_norm_g,
              ple_gate_norm_g, w_ple_gate, final_norm_g):
    b, s, _ = x.shape
    for l in range(DEPTH):
        h = rmsnorm(x, norm_in_g[l])
        proj = h @ w_in[l]
        (cq, ckv, kr, gate_mla, gq, gk, gv, gk_low, gate_gla,
         merge_a, merge_b) = split_in(proj)

        cq = rmsnorm(cq, q_norm_g[l])
        q = (cq @ w_uq[l]).reshape(b, s, MLA_HEADS, MLA_NOPE + MLA_ROPE)
        q_nope, q_rope = q[..., :MLA_NOPE], q[..., MLA_NOPE:]
        q_rope = rope(q_rope, positions)
        ckv = rmsnorm(ckv, kv_norm_g[l])
        kv = (ckv @ w_ukv[l]).reshape(b, s, MLA_HEADS, MLA_NOPE + MLA_V)
        k_nope, v_mla = kv[..., :MLA_NOPE], kv[..., MLA_NOPE:]
        k_rope = rope(kr[:, :, None, :], positions)[:, :, 0, :]
        o_mla = mla_attention(q_nope, q_rope, k_nope, k_rope, v_mla).reshape(b, s, MLA_WIDTH)
        y_a = (o_mla * jax.nn.silu(gate_mla)) @ w_mla_br[l]

        qg = gq.reshape(b, s, GLA_HEADS, GLA_HEAD_K)
        kg = gk.reshape(b, s, GLA_HEADS, GLA_HEAD_K)
        vg = gv.reshape(b, s, GLA_HEADS, GLA_HEAD_V)
        log_a = jax.nn.log_sigmoid((gk_low @ w_gk_up[l] + b_gk[l]).astype(jnp.float32)) / GLA_GATE_NORMALIZER
        log_a = log_a.reshape(b, s, GLA_HEADS, GLA_HEAD_K)
        o_gla = gla_chunked(qg, kg, vg, log_a).astype(x.dtype)
        o_gla = rmsnorm(o_gla, gla_norm_g[l]).reshape(b, s, GLA_DV)
        y_b = (o_gla * jax.nn.silu(gate_gla)) @ w_gla_br[l]

        merged = jax.nn.sigmoid(merge_a) * y_a + jax.nn.sigmoid(merge_b) * y_b
        x = x + merged @ w_out[l]

        e = rmsnorm(p[l] @ w_ple[l], ple_norm_g[l])
        g_ple = jax.nn.sigmoid(rmsnorm(x, ple_gate_norm_g[l]) @ w_ple_gate[l])
        x = x + g_ple * e
    return rmsnorm(x, final_norm_g)
```

```python
import contextlib
import numpy as np
import concourse.bass as bass
import concourse.mybir as mybir
from concourse.bass_utils import run_bass_kernel_spmd

F32 = mybir.dt.float32
BF16 = mybir.dt.bfloat16
I32 = mybir.dt.int32
AF = mybir.ActivationFunctionType
ALU = mybir.AluOpType

NCORES = 8
SEQ = 2048
D = 1024
EPS = 1e-6
TWO_PI = float(2.0 * np.pi * (1.0 - 2e-7))
ENGINES = ("sync", "scalar", "vector", "gpsimd", "tensor")

C_ID, C_TRI, C_L, C_U, C_SEL, C_ROPE, C_END = 0, 128, 256, 384, 512, 640, 644


class _Op:
    __slots__ = ("eng", "fn", "dma", "waits", "inc", "idx", "key", "pos")


class Sched:
    def __init__(self, nc, stack):
        self.nc = nc
        self.stack = stack
        self.sems = {}
        self.counts = {}
        self.seen = {e: {} for e in ENGINES}
        self._reset()

    def _reset(self):
        self.ops = []
        self.last_w = {}
        self.readers = {}

    def add(self, eng, fn, R=(), W=(), dma=False, stream=None):
        op = _Op()
        op.eng, op.fn, op.dma, op.inc, op.idx = eng, fn, dma, False, 0
        op.key = ("dma", stream) if dma else ("eng", eng)
        op.pos = len(self.ops)
        best = {}
        W = tuple(W) + tuple(r for r in R if isinstance(r, tuple) and r[0] == "ps" and r not in W)

        def dep(d, kind):
            if d is None or d is op:
                return
            if (not d.dma) and d.eng == eng and not dma:
                if eng == "tensor":
                    return
            cur = best.get(d.key)
            if cur is None or d.pos > cur.pos:
                best[d.key] = d

        for r in R:
            dep(self.last_w.get(r), "raw")
        for r in W:
            dep(self.last_w.get(r), "waw")
            for rd in self.readers.get(r, ()):
                dep(rd, "war")
        op.waits = list(best.values())
        for d in op.waits:
            d.inc = True
        for r in W:
            self.last_w[r] = op
            self.readers[r] = []
        for r in R:
            lst = self.readers.setdefault(r, [])
            for i, o in enumerate(lst):
                if o.key == op.key:
                    lst[i] = op
                    break
            else:
                lst.append(op)
        self.ops.append(op)
        return op

    def flush(self):
        nc = self.nc
        per_eng = {e: [] for e in ENGINES}
        for op in self.ops:
            per_eng[op.eng].append(op)
        for e in ENGINES:
            for op in reversed(per_eng[e]):
                if not op.dma:
                    op.inc = True
                    break
        for op in self.ops:
            if op.dma:
                op.inc = True
        for op in self.ops:
            if op.inc:
                k = op.key
                if k not in self.sems:
                    self.sems[k] = self.stack.enter_context(nc.semaphore("s%d" % len(self.sems)))
                    self.counts[k] = 0
                self.counts[k] += 16 if op.dma else 1
                op.idx = self.counts[k]
        finals = dict(self.counts)
        sems, seen_all = self.sems, self.seen

        def make(engname):
            def body(eng):
                seen = seen_all[engname]
                for op in per_eng[engname]:
                    for d in op.waits:
                        if seen.get(d.key, 0) < d.idx:
                            eng.wait_ge(sems[d.key], d.idx)
                            seen[d.key] = d.idx
                    ins = op.fn(eng)
                    if op.inc:
                        ins.then_inc(sems[op.key], 16 if op.dma else 1)
                for k, v in finals.items():
                    if seen.get(k, 0) < v:
                        eng.wait_ge(sems[k], v)
                        seen[k] = v
            return body

        with nc.Block() as block:
            block.sync(make("sync"))
            block.scalar(make("scalar"))
            block.vector(make("vector"))
            block.gpsimd(make("gpsimd"))
            block.tensor(make("tensor"))
        self._reset()

    def dma(self, eng, out, in_, R, W, stream):
        return self.add(eng, lambda e: e.dma_start(out=out, in_=in_), R, W, dma=True, stream=stream)

    def mm(self, out, lhsT, rhs, start, stop, R, W, tp=None):
        if tp is not None:
            return self.add("tensor", lambda e: e.matmul(out, lhsT=lhsT, rhs=rhs, start=start, stop=stop, tile_position=tp), R, W)
        return self.add("tensor", lambda e: e.matmul(out, lhsT=lhsT, rhs=rhs, start=start, stop=stop), R, W)

    def tr(self, out, in_, ident, R, W):
        return self.add("tensor", lambda e: e.transpose(out=out, in_=in_, identity=ident), R, W)

    def act(self, out, in_, func, R, W, scale=None, bias=None, accum=None):
        kw = {}
        if scale is not None:
            kw["scale"] = scale
        if bias is not None:
            kw["bias"] = bias
        if accum is not None:
            kw["accum_out"] = accum
        return self.add("scalar", lambda e: e.activation(out=out, in_=in_, func=func, **kw), R, W)

    def copy(self, eng, out, in_, R, W):
        if eng == "scalar":
            return self.add(eng, lambda e: e.copy(out=out, in_=in_), R, W)
        return self.add(eng, lambda e: e.tensor_copy(out=out, in_=in_), R, W)

    def tt(self, eng, out, in0, in1, op, R, W):
        return self.add(eng, lambda e: e.tensor_tensor(out=out, in0=in0, in1=in1, op=op), R, W)

    def ts(self, eng, out, in0, s1, op0, R, W, s2=None, op1=None):
        if op1 is None:
            return self.add(eng, lambda e: e.tensor_scalar(out=out, in0=in0, scalar1=s1, scalar2=None, op0=op0), R, W)
        return self.add(eng, lambda e: e.tensor_scalar(out=out, in0=in0, scalar1=s1, scalar2=s2, op0=op0, op1=op1), R, W)

    def stt(self, eng, out, in0, scalar, in1, op0, op1, R, W):
        return self.add(eng, lambda e: e.scalar_tensor_tensor(out=out, in0=in0, scalar=scalar, in1=in1, op0=op0, op1=op1), R, W)

    def memset(self, eng, ap, val, W):
        return self.add(eng, lambda e: e.memset(ap, val), (), W)

    def recip(self, out, in_, R, W):
        return self.add("vector", lambda e: e.reciprocal(out=out, in_=in_), R, W)


def build(debug=False, nseq=2, ng=4, passes="MGP", cut=99):
    nc = bass.Bass("TRN2", target_bir_lowering=False)

    def din(name, shape, dt=F32):
        return nc.dram_tensor(name, list(shape), dt, kind="ExternalInput").ap()

    x = din("x", [2 * SEQ, D])
    pin = din("p", [2 * SEQ, 256])
    pos = din("pos", [1, 2 * SEQ], I32)
    w_in = din("w_in", [D, 4784])
    w_krz = din("w_krz", [D, 192])
    w_uq = din("w_uq", [384, 768])
    w_uqsw = din("w_uqsw", [384, 768])
    w_ukvk = din("w_ukvk", [256, 512])
    w_ukvv = din("w_ukvv", [256, 512])
    w_gk17 = din("w_gk17", [17, 256])
    w_mla = din("w_mla", [512, D])
    w_gla = din("w_gla", [512, D])
    w_out = din("w_out", [D, D])
    w_ple = din("w_ple", [256, D])
    w_pg = din("w_pg", [D, D])
    g_in = din("g_in", [1, D])
    g_gla = din("g_gla", [1, 128])
    g_ple = din("g_ple", [1, D])
    g_pg = din("g_pg", [1, D])
    g_fin = din("g_fin", [1, D])
    gcols = din("gcols", [128, 8])
    consts = din("consts", [128, C_END])
    out = nc.dram_tensor("out", [2 * SEQ, D], F32, kind="ExternalOutput").ap()
    if debug:
        dbg_ua = nc.dram_tensor("dbg_ua", [2, 128, 4, SEQ], BF16, kind="ExternalOutput").ap()
        dbg_ub = nc.dram_tensor("dbg_ub", [2, 128, 4, SEQ], BF16, kind="ExternalOutput").ap()

    top = contextlib.ExitStack()
    with top:
        uid = [0]

        def sb(stack, name, shape, dt):
            uid[0] += 1
            return stack.enter_context(nc.sbuf_tensor("%s_%d" % (name, uid[0]), list(shape), dt))

        S = Sched(nc, top)
        cst = sb(top, "cst", [128, C_END], F32)
        gc = sb(top, "gc", [128, 8], F32)
        ident = sb(top, "ident", [128, 128], BF16)
        tri = sb(top, "tri", [128, 4, 128], BF16)
        ones_bf = sb(top, "ones_bf", [128, 128], BF16)
        uaT = sb(top, "uaT", [128, 4, SEQ], BF16)
        ubT = sb(top, "ubT", [128, 4, SEQ], BF16)
        ps = [top.enter_context(nc.psum_tensor("ps%d" % b, [128, 512], F32)) for b in range(8)]
        tpv = ps[0][:].bitcast(BF16)

        S.dma("sync", cst[:], consts, [], ["cst"], "cst")
        S.dma("sync", gc[:], gcols, [], ["gc"], "gc")
        S.copy("vector", ident[:], cst[:, C_ID:C_ID + 128], ["cst"], ["ident"])
        for h in range(4):
            S.copy("vector", tri[:, h, :], cst[:, C_TRI:C_TRI + 128], ["cst"], ["tri"])
        S.memset("vector", ones_bf[:], 1.0, ["ones_bf"])
        Lmat = cst[:, C_L:C_L + 128]
        Umat = cst[:, C_U:C_U + 128]

        def stage_hT(ctx, tok0, xs, col0, evac_eng, hT=None, htag=("hT",), parts="LST"):
            xt, hn, st, junk, gbc = ctx["xt"], ctx["hn"], ctx["st"], ctx["junk"], ctx["gin_bc"]
            if hT is None:
                hT = ctx["hT"]
            hs = xs % len(hn)
            if "L" in parts:
                S.dma("sync", xt[xs][:], x[tok0:tok0 + 128, :], [], [("xt", xs)], ("xt", xs))
            if "S" in parts:
                S.act(junk, xt[xs][:], AF.Square, [("xt", xs)], ["junk", ("ss", xs)], accum=st[:, xs:xs + 1])
                S.act(st[:, 8 + xs:9 + xs], st[:, xs:xs + 1], AF.Ln, [("ss", xs)], [("ln", xs)], scale=1.0 / D, bias=EPS)
                S.act(st[:, 16 + xs:17 + xs], st[:, 8 + xs:9 + xs], AF.Exp, [("ln", xs)], [("rs", xs)], scale=-0.5)
                S.stt("vector", hn[hs][:], xt[xs][:], st[:, 16 + xs:17 + xs], gbc[:], ALU.mult, ALU.mult,
                      [("xt", xs), ("rs", xs), "gin_bc"], [("hn", hs)])
            if "T" in parts:
                for c in range(8):
                    S.tr(tpv[:, c * 128:(c + 1) * 128], hn[hs][:, c * 128:(c + 1) * 128], ident[:],
                         [("hn", hs), "ident"], [("ps", 0)])
                S.copy(evac_eng, hT[:, :, col0:col0 + 128], tpv.rearrange("p (c t) -> p c t", c=8),
                       [("ps", 0)], [htag + (col0,)])

        def load_w(name, dst, src, R=()):
            S.dma("gpsimd", dst, src, list(R), [name], name)

        def kchunks(w):
            return w.rearrange("(c p) n -> p c n", p=128)

        def pass_M(s):
            with contextlib.ExitStack() as st_:
                wM = sb(st_, "wM", [128, 8, 1152], BF16)
                wkr = sb(st_, "wkr", [128, 8, 192], BF16)
                wuq = sb(st_, "wuq", [128, 3, 768], BF16)
                wuqs = sb(st_, "wuqs", [128, 3, 768], BF16)
                wkk = sb(st_, "wkk", [128, 2, 512], BF16)
                wkv = sb(st_, "wkv", [128, 2, 512], BF16)
                kT = sb(st_, "kT", [128, 8, SEQ], BF16)
                Ve = sb(st_, "Ve", [128, 16, 4, 128], BF16)
                Vo = sb(st_, "Vo", [128, 16, 4, 128], BF16)
                gin_bc = sb(st_, "gin_bc", [128, D], F32)
                xt = [sb(st_, "xt%d" % i, [128, D], F32) for i in range(2)]
                hn = [sb(st_, "hn%d" % i, [128, D], BF16) for i in range(2)]
                hTs = [sb(st_, "hT%d" % i, [128, 8, 512], BF16) for i in range(2)]
                scr = sb(st_, "scr", [128, 3, 512], F32)
                sq = sb(st_, "sq", [128, 3, 512], BF16)
                rq = sb(st_, "rq", [128, 512], F32)
                cqn = sb(st_, "cqn", [128, 3, 512], BF16)
                ckvn = sb(st_, "ckvn", [128, 2, 512], BF16)
                sg = sb(st_, "sg", [128, 4, 512], BF16)
                qT = sb(st_, "qT", [128, 8, 512], BF16)
                posi = sb(st_, "posi", [128, 512], I32)
                cos2 = sb(st_, "cos2", [128, 512], F32)
                sin2 = sb(st_, "sin2", [128, 512], F32)
                pT = [sb(st_, "pT%d" % i, [128, 512], BF16) for i in range(3)]
                stt_ = sb(st_, "stM", [128, 24], F32)
                junk = sq[:, 0:2, :].rearrange("p a b -> p (a b)")
                ctx = dict(xt=xt, hn=hn, st=stt_, junk=junk, hT=None, gin_bc=gin_bc)

                def stageM(gn, t):
                    stage_hT(ctx, s * SEQ + gn * 512 + t * 128, t % 2, t * 128, "scalar" if t % 2 else "vector",
                             hT=hTs[gn % 2], htag=("hT", gn % 2))

                wi = kchunks(w_in)
                load_w("wM_a", wM[:, :, 0:640], wi[:, :, 0:640])
                load_w("wM_b", wM[:, :, 640:1152], wi[:, :, 672:1184])
                load_w("wkr", wkr[:], kchunks(w_krz))
                load_w("wuq", wuq[:], kchunks(w_uq))
                load_w("wuqs", wuqs[:], kchunks(w_uqsw))
                load_w("wkk", wkk[:], kchunks(w_ukvk))
                load_w("wkv", wkv[:], kchunks(w_ukvv))
                S.dma("sync", gin_bc[:], g_in.partition_broadcast(128), [], ["gin_bc"], "gin_bc")
                S.memset("vector", Ve[:, :, :, 64:128], 1.0, ["Vones"])
                S.memset("vector", Vo[:, :, :, 0:64], 1.0, ["Vones"])
                WM = ["wM_a", "wM_b"]
                SCALE = float(96 ** -0.5)
                for t in range(4):
                    stageM(0, t)
                r64 = slice(64, 96)
                pj_rot = [1, 2, 3]
                pj_i = [0]

                def pj():
                    b = pj_rot[pj_i[0] % 3]
                    pj_i[0] += 1
                    return b

                for g in range(ng):
                    tok0 = s * SEQ + g * 512
                    c0g = g * 512
                    hT = hTs[g % 2]
                    HT = [("hT", g % 2, c0) for c0 in (0, 128, 256, 384)]
                    S.dma("sync", posi[r64, :], pos[0:1, tok0:tok0 + 512].partition_broadcast(32), [], ["posi"], "posi")
                    S.copy("vector", scr[r64, 0, :], posi[r64, :], ["posi"], [("scr", 0)])
                    S.ts("vector", scr[r64, 0, :], scr[r64, 0, :], cst[r64, C_ROPE:C_ROPE + 1], ALU.mult,
                         [("scr", 0), "cst"], [("scr", 0)])
                    S.copy("vector", posi[r64, :], scr[r64, 0, :], [("scr", 0)], ["posi"])
                    S.copy("vector", scr[r64, 1, :], posi[r64, :], ["posi"], [("scr", 1)])
                    S.tt("vector", scr[r64, 1, :], scr[r64, 0, :], scr[r64, 1, :], ALU.subtract,
                         [("scr", 0), ("scr", 1)], [("scr", 1)])
                    S.act(sin2[r64, :], scr[r64, 1, :], AF.Sin, [("scr", 1), "cst"], ["sin2"],
                          scale=cst[r64, C_ROPE + 1:C_ROPE + 2])
                    S.ts("vector", scr[r64, 2, :], scr[r64, 0, :], 0.25, ALU.add, [("scr", 0)], [("scr", 2)])
                    S.copy("vector", posi[r64, :], scr[r64, 2, :], [("scr", 2)], ["posi"])
                    S.copy("vector", scr[r64, 1, :], posi[r64, :], ["posi"], [("scr", 1)])
                    S.tt("vector", scr[r64, 1, :], scr[r64, 2, :], scr[r64, 1, :], ALU.subtract,
                         [("scr", 2), ("scr", 1)], [("scr", 1)])
                    S.act(cos2[r64, :], scr[r64, 1, :], AF.Sin, [("scr", 1)], ["cos2"], scale=TWO_PI)


                    def proj(col, width=128, w=wM, wn=WM, m0=0):
                        b = pj()
                        for k in range(8):
                            S.mm(ps[b][m0:m0 + width, :], w[:, k, col:col + width], hT[:, k, :],
                                 k == 0, k == 7, wn + HT, [("ps", b)])
                        return b

                    def lowrank_norm(col_base, nch, gcol0, dst, dname, inv_n):
                        for c in range(nch):
                            b = proj(col_base + c * 128)
                            S.act(sq[:, c, :], ps[b][:], AF.Square, [("ps", b)], [("sq", c)])
                            S.copy("vector", scr[:, c, :], ps[b][:], [("ps", b)], [("scr", c)])
                        b = pj()
                        for c in range(nch):
                            S.mm(ps[b][:], ones_bf[:], sq[:, c, :], c == 0, c == nch - 1,
                                 ["ones_bf", ("sq", c)], [("ps", b)])
                        if cut == 24:
                            return
                        S.act(rq[:], ps[b][:], AF.Ln, [("ps", b)], ["rq"], scale=inv_n, bias=EPS)
                        if cut == 25:
                            return
                        S.act(rq[:], rq[:], AF.Exp, ["rq"], ["rq"], scale=-0.5)
                        if cut == 26:
                            return
                        for c in range(nch):
                            S.stt("vector", dst[:, c, :], scr[:, c, :], gc[:, gcol0 + c:gcol0 + c + 1], rq[:],
                                  ALU.mult, ALU.mult, [("scr", c), "gc", "rq"], [(dname, c)])

                    if cut == 20:
                        b = proj(0)
                        S.flush()
                        return
                    if cut == 21:
                        b = proj(0)
                        S.act(sq[:, 0, :], ps[b][:], AF.Square, [("ps", b)], [("sq", 0)])
                        S.flush()
                        return
                    if cut == 22:
                        b = proj(0)
                        S.copy("vector", scr[:, 0, :], ps[b][:], [("ps", b)], [("scr", 0)])
                        S.flush()
                        return
                    lowrank_norm(0, 3, 0, cqn, "cqn", 1.0 / 384)
                    CQN = [("cqn", c) for c in range(3)]
                    if cut in (23, 24, 25, 26):
                        S.flush()
                        return
                    lowrank_norm(384, 2, 3, ckvn, "ckvn", 1.0 / 256)
                    CKV = [("ckvn", c) for c in range(2)]
                    for c in range(4):
                        b = proj(640 + c * 128)
                        S.act(sg[:, c, :], ps[b][:], AF.Silu, [("ps", b)], [("sg", c)])
                    if cut == 3:
                        S.flush()
                        return
                    ba = proj(0, 96, wkr, ["wkr"])
                    bb = proj(96, 96, wkr, ["wkr"])
                    S.tt("vector", scr[r64, 0, :], ps[ba][r64, :], cos2[r64, :], ALU.mult, [("ps", ba), "cos2"], [("scr", 0)])
                    S.tt("vector", scr[r64, 1, :], ps[bb][r64, :], sin2[r64, :], ALU.mult, [("ps", bb), "sin2"], [("scr", 1)])
                    S.tt("vector", kT[r64, 0, c0g:c0g + 512], scr[r64, 0, :], scr[r64, 1, :], ALU.add,
                         [("scr", 0), ("scr", 1)], [("kTr", 0, g)])
                    for h in range(1, 8):
                        S.copy("vector", kT[r64, h, c0g:c0g + 512], kT[r64, 0, c0g:c0g + 512],
                               [("kTr", 0, g)], [("kTr", h, g)])
                    if cut == 4:
                        S.flush()
                        return
                    def qk(i):
                        for h in (2 * i, 2 * i + 1):
                            ba, bb = pj(), pj()
                            for c in range(3):
                                S.mm(ps[ba][0:96, :], wuq[:, c, 96 * h:96 * h + 96], cqn[:, c, :], c == 0, c == 2,
                                     ["wuq"] + CQN, [("ps", ba)])
                            for c in range(3):
                                S.mm(ps[bb][0:96, :], wuqs[:, c, 96 * h:96 * h + 96], cqn[:, c, :], c == 0, c == 2,
                                     ["wuqs"] + CQN, [("ps", bb)])
                            S.copy("vector", qT[0:64, h, :], ps[ba][0:64, :], [("ps", ba)], [("qTn", h)])
                            S.tt("vector", scr[r64, 0, :], ps[ba][r64, :], cos2[r64, :], ALU.mult, [("ps", ba), "cos2"], [("scr", 0)])
                            S.tt("vector", scr[r64, 1, :], ps[bb][r64, :], sin2[r64, :], ALU.mult, [("ps", bb), "sin2"], [("scr", 1)])
                            S.tt("vector", qT[r64, h, :], scr[r64, 0, :], scr[r64, 1, :], ALU.add,
                                 [("scr", 0), ("scr", 1)], [("qTr", h)])
                            b = pj()
                            for c in range(2):
                                S.mm(ps[b][0:64, :], wkk[:, c, 64 * h:64 * h + 64], ckvn[:, c, :], c == 0, c == 1,
                                     ["wkk"] + CKV, [("ps", b)])
                            S.copy("vector", kT[0:64, h, c0g:c0g + 512], ps[b][0:64, :], [("ps", b)], [("kTn", h, g)])

                    for t in range(4):
                        T = g * 4 + t
                        b = pj()
                        for c in range(2):
                            S.mm(ps[b][:], ckvn[:, c, t * 128:(t + 1) * 128], wkv[:, c, :], c == 0, c == 1,
                                 ["wkv"] + CKV, [("ps", b)])
                        pv = ps[b][:].rearrange("p (i two d) -> p i two d", two=2, d=64)
                        S.copy("vector", Ve[:, T, :, 0:64], pv[:, :, 0, :], [("ps", b)], [("Ve", T)])
                        S.copy("scalar", Vo[:, T, :, 64:128], pv[:, :, 1, :], [("ps", b)], [("Vo", T)])

                    if cut == 5:
                        S.flush()
                        return
                    nk = 4 * (g + 1)
                    qk(0)
                    for i in range(4):
                        hA, hB = 2 * i, 2 * i + 1
                        obA = 4 if i % 2 == 0 else 6
                        obB = obA + 1
                        if g + 1 < ng:
                            stageM(g + 1, i)
                        steps = [(j, hh) for j in range(nk) for hh in (0, 1)]
                        pend = None
                        for n, (j, hh) in enumerate(steps):
                            h = hA if hh == 0 else hB
                            r = j - 4 * g
                            c0 = 128 * r if r > 0 else 0
                            gj = j // 4
                            b = pj()
                            slot = n % 3
                            S.mm(ps[b][:, c0:512], kT[0:96, h, j * 128:(j + 1) * 128], qT[0:96, h, c0:512], True, True,
                                 [("kTn", h, gj), ("kTr", h, gj), ("qTn", h), ("qTr", h)], [("ps", b)])
                            S.act(pT[slot][:, c0:512], ps[b][:, c0:512], AF.Exp, [("ps", b)], [("pT", slot)], scale=SCALE)
                            if r >= 0:
                                S.tt("vector", pT[slot][:, c0:c0 + 128], pT[slot][:, c0:c0 + 128], tri[:, 0, :], ALU.mult,
                                     [("pT", slot), "tri"], [("pT", slot)])
                            if pend is not None:
                                pend()
                            if hh == 0:
                                ob, lhsT, vr = obA, Ve[:, j, i, :], [("Ve", j), "Vones"]
                            else:
                                ob, lhsT, vr = obB, Vo[:, j, i, :], [("Vo", j), "Vones"]

                            def pend(ob=ob, c0=c0, lhsT=lhsT, slot=slot, j=j, vr=vr):
                                S.mm(ps[ob][:, c0:512], lhsT, pT[slot][:, c0:512], j == 0, j == nk - 1,
                                     vr + [("pT", slot)], [("ps", ob)])
                            if n == 3 and i + 1 < 4:
                                qk(i + 1)
                        pend()
                        S.recip(scr[0:64, 0, :], ps[obA][64:128, :], [("ps", obA)], [("scr", 0)])
                        S.recip(scr[64:128, 0, :], ps[obB][0:64, :], [("ps", obB)], [("scr", 0)])
                        S.tt("vector", scr[:, 2, :], scr[:, 0, :], sg[:, i, :], ALU.mult, [("scr", 0), ("sg", i)], [("scr", 2)])
                        S.tt("vector", uaT[0:64, i, c0g:c0g + 512], ps[obA][0:64, :], scr[0:64, 2, :], ALU.mult,
                             [("ps", obA), ("scr", 2)], [("uaT", i, g)])
                        S.tt("vector", uaT[64:128, i, c0g:c0g + 512], ps[obB][64:128, :], scr[64:128, 2, :], ALU.mult,
                             [("ps", obB), ("scr", 2)], [("uaT", i, g)])
                if debug:
                    S.dma("sync", dbg_ua[s][:, :, 0:ng * 512], uaT[:, :, 0:ng * 512], [("uaT", i, g) for i in range(4) for g in range(ng)], ["dbg_ua"], "dbg")
                S.flush()

        def pass_G(s, pre):
            with contextlib.ExitStack() as st_:
                wG = sb(st_, "wG", [128, 8, 1536], BF16)
                wgl = sb(st_, "wgl", [128, 8, 16], BF16)
                wg17 = sb(st_, "wg17", [128, 256], BF16)
                gin_bc = sb(st_, "gin_bcG", [128, D], F32)
                ggla_bc = sb(st_, "ggla_bc", [128, 128], F32)
                xt = [sb(st_, "xtG%d" % i, [128, D], F32) for i in range(2)]
                hn = [sb(st_, "hnG%d" % i, [128, D], BF16) for i in range(2)]
                hTs = [sb(st_, "hTG%d" % i, [128, 8, 512], BF16) for i in range(2)]
                junk_t = sb(st_, "junkG", [128, D], BF16)
                gqf = sb(st_, "gqf", [128, 4, 512], F32)
                gkf = sb(st_, "gkf", [128, 4, 512], F32)
                sgg = sb(st_, "sgg", [128, 4, 512], BF16)
                gkl = sb(st_, "gkl", [128, 512], BF16)
                vsb = [sb(st_, "vsb%d" % i, [128, 512], BF16) for i in range(2)]
                etmp = sb(st_, "etmp", [128, 256], F32)
                sp = sb(st_, "sp", [128, 256], F32)
                ebT = sb(st_, "ebT", [128, 4, 128], F32)
                enbT = sb(st_, "enbT", [128, 4, 128], F32)
                erev = sb(st_, "erev", [128, 256], F32)
                qtT = sb(st_, "qtT", [128, 4, 128], BF16)
                ktT = sb(st_, "ktT", [128, 4, 128], BF16)
                kdec = sb(st_, "kdec", [128, 256], BF16)
                AT = sb(st_, "AT", [128, 4, 128], BF16)
                onsb = sb(st_, "onsb", [128, 512], BF16)
                Sf = sb(st_, "Sf", [128, 4, 128], F32)
                Sb = sb(st_, "Sb", [128, 4, 128], BF16)
                junk2 = sb(st_, "junk2", [128, 128], BF16)
                stt_ = sb(st_, "stG", [128, 40], F32)
                ctx = dict(xt=xt, hn=hn, st=stt_, junk=junk_t[:], hT=None, gin_bc=gin_bc)
                r0_ = slice(0, 64)

                def stageG(gn, t, parts="LST"):
                    stage_hT(ctx, s * SEQ + gn * 512 + t * 128, t % 2, t * 128, "scalar" if t % 2 else "vector",
                             hT=hTs[gn % 2], htag=("hT", gn % 2), parts=parts)

                wi = kchunks(w_in)
                load_w("wG_a", wG[:, :, 0:1024], wi[:, :, 1184:2208])
                load_w("wG_b", wG[:, :, 1024:1536], wi[:, :, 2224:2736])
                load_w("wgl", wgl[:], wi[:, :, 2208:2224])
                load_w("wg17", wg17[0:17, :], w_gk17)
                load_w("wP", pre[0][:], wi[:, :, 2736:4784])
                load_w("wo", pre[1][:], kchunks(w_out))
                load_w("wpg", pre[2][:], kchunks(w_pg))
                S.dma("sync", gin_bc[:], g_in.partition_broadcast(128), [], ["gin_bc"], "gin_bc")
                S.dma("sync", ggla_bc[:], g_gla.partition_broadcast(128), [], ["ggla_bc"], "ggla_bc")
                S.memset("vector", gkl[:], 1.0, ["gkl"])
                S.memset("vector", Sf[:], 0.0, ["Sf"])
                S.memset("vector", Sb[:], 0.0, ["Sb"])
                WG = ["wG_a", "wG_b"]
                pj_i = [0]

                def pj():
                    b = 1 + pj_i[0] % 2
                    pj_i[0] += 1
                    return b

                for t in range(4):
                    stageG(0, t)
                for g in range(ng):
                    tok0 = s * SEQ + g * 512
                    c0g = g * 512
                    hT = hTs[g % 2]
                    HT = [("hT", g % 2, c0) for c0 in (0, 128, 256, 384)]

                    def proj(col, width=128, w=wG, wn=WG):
                        b = pj()
                        for k in range(8):
                            S.mm(ps[b][0:width, :], w[:, k, col:col + width], hT[:, k, :], k == 0, k == 7,
                                 wn + HT, [("ps", b)])
                        return b

                    for c in range(2):
                        b = proj(c * 128)
                        S.copy("scalar", gqf[r0_, 2 * c, :], ps[b][0:64, :], [("ps", b)], [("gqf", 2 * c)])
                        S.copy("vector", gqf[r0_, 2 * c + 1, :], ps[b][64:128, :], [("ps", b)], [("gqf", 2 * c + 1)])
                    for c in range(2):
                        b = proj(256 + c * 128)
                        S.copy("scalar", gkf[r0_, 2 * c, :], ps[b][0:64, :], [("ps", b)], [("gkf", 2 * c)])
                        S.copy("vector", gkf[r0_, 2 * c + 1, :], ps[b][64:128, :], [("ps", b)], [("gkf", 2 * c + 1)])
                    GQ = [("gqf", h) for h in range(4)]
                    GK = [("gkf", h) for h in range(4)]
                    for c in range(4):
                        b = proj(1024 + c * 128)
                        S.act(sgg[:, c, :], ps[b][:], AF.Silu, [("ps", b)], [("sgg", c)])
                    b = proj(0, 16, wgl, ["wgl"])
                    S.copy("vector", gkl[0:16, :], ps[b][0:16, :], [("ps", b)], ["gkl"])

                    def F(t):
                        cs = slice(t * 128, (t + 1) * 128)
                        vs = t % 2
                        htr = [("hT", g % 2, t * 128)]
                        for k in range(8):
                            S.mm(ps[3][:], hT[:, k, cs], wG[:, k, 512:1024], k == 0, k == 7, WG + htr, [("ps", 3)])
                        S.copy("scalar", vsb[vs][:], ps[3][:], [("ps", 3)], [("vsb", vs)])
                        for k in range(8):
                            S.mm(ps[4][:, 0:256], hT[:, k, cs], wG[:, k, 256:512], k == 0, k == 7, WG + htr, [("ps", 4)])
                        S.mm(ps[4][:, 256:512], gkl[0:17, cs], wg17[0:17, :], True, True, ["gkl", "wg17"], [("ps", 4)])
                        S.act(etmp[:], ps[4][:, 256:512], AF.Exp, [("ps", 4)], ["etmp"], scale=-1.0)
                        S.act(sp[:], etmp[:], AF.Ln, ["etmp"], ["sp"], bias=1.0)
                        for h in range(4):
                            S.mm(ps[5][0:64, h * 128:(h + 1) * 128], sp[:, h * 64:(h + 1) * 64], Lmat, True, True,
                                 ["sp", "cst"], [("ps", 5)])
                        S.mm(ps[2][:, 0:256], Umat, sp[:], True, True, ["sp", "cst"], [("ps", 2)])
                        bt = ps[5][0:64, :].rearrange("p (h t) -> p h t", h=4)
                        S.act(ebT[r0_], bt, AF.Exp, [("ps", 5)], ["ebT"])
                        S.act(enbT[r0_], bt, AF.Exp, [("ps", 5)], ["enbT"], scale=-1.0)
                        S.act(erev[:], ps[2][:, 0:256], AF.Exp, [("ps", 2)], ["erev"])
                        S.stt("vector", qtT[r0_], gqf[r0_, :, cs], 0.125, ebT[r0_], ALU.mult, ALU.mult, GQ + ["ebT"], ["qtT"])
                        S.tt("vector", ktT[r0_], gkf[r0_, :, cs], enbT[r0_], ALU.mult, GK + ["enbT"], ["ktT"])
                        S.tt("vector", kdec[:], ps[4][:, 0:256], erev[:], ALU.mult, [("ps", 4), "erev"], ["kdec"])
                        for h in range(4):
                            S.mm(ps[6][:, h * 128:(h + 1) * 128], ktT[r0_, h, :], qtT[r0_, h, :], True, True,
                                 ["ktT", "qtT"], [("ps", 6)])
                        S.tt("vector", AT[:], ps[6][:].rearrange("p (h t) -> p h t", h=4), tri[:], ALU.mult,
                             [("ps", 6), "tri"], ["AT"])
                    def O(t):
                        cs = slice(t * 128, (t + 1) * 128)
                        vs = t % 2
                        for h in range(4):
                            hs_ = slice(h * 128, (h + 1) * 128)
                            S.mm(ps[7][:, hs_], AT[:, h, :], vsb[vs][:, hs_], True, False, ["AT", ("vsb", vs)], [("ps", 7)])
                            S.mm(ps[7][:, hs_], qtT[r0_, h, :], Sb[r0_, h, :], False, True, ["qtT", "Sb"], [("ps", 7)])
                        for h in range(4):
                            hs_ = slice(h * 128, (h + 1) * 128)
                            S.mm(ps[1][0:64, hs_], kdec[:, h * 64:(h + 1) * 64], vsb[vs][:, hs_], True, True,
                                 ["kdec", ("vsb", vs)], [("ps", 1)])
                        for h in range(4):
                            hs_ = slice(h * 128, (h + 1) * 128)
                            S.stt("vector", Sf[r0_, h, :], Sf[r0_, h, :], ebT[r0_, h, 127:128], ps[1][0:64, hs_], ALU.mult, ALU.add,
                                  ["Sf", "ebT", ("ps", 1)], ["Sf"])
                        S.copy("scalar", Sb[r0_], Sf[r0_], ["Sf"], ["Sb"])
                    def N(t):
                        cs = slice(t * 128, (t + 1) * 128)
                        vs = t % 2
                        for h in range(4):
                            S.act(junk2[:], ps[7][:, h * 128:(h + 1) * 128], AF.Square, [("ps", 7)], ["junk2", "oss"],
                                  accum=stt_[:, 24 + h:25 + h])
                        S.act(stt_[:, 28:32], stt_[:, 24:28], AF.Ln, ["oss"], ["oln"], scale=1.0 / 128, bias=EPS)
                        S.act(stt_[:, 32:36], stt_[:, 28:32], AF.Exp, ["oln"], ["ors"], scale=-0.5)
                        for h in range(4):
                            hs_ = slice(h * 128, (h + 1) * 128)
                            S.stt("vector", onsb[:, hs_], ps[7][:, hs_], stt_[:, 32 + h:33 + h], ggla_bc[:], ALU.mult, ALU.mult,
                                  [("ps", 7), "ors", "ggla_bc"], ["onsb"])
                        for h in range(4):
                            hs_ = slice(h * 128, (h + 1) * 128)
                            S.tr(tpv[:, hs_], onsb[:, hs_], ident[:], ["onsb", "ident"], [("ps", 0)])
                        S.tt("vector", ubT[:, :, c0g + t * 128:c0g + (t + 1) * 128],
                             tpv[:, 0:512].rearrange("p (h t) -> p h t", h=4), sgg[:, :, cs], ALU.mult,
                             [("ps", 0)] + [("sgg", c) for c in range(4)], [("ubT", g, t)])
                    F(0)
                    for t in range(4):
                        if g + 1 < ng:
                            stageG(g + 1, t, "L")
                        O(t)
                        if t + 1 < 4:
                            F(t + 1)
                        N(t)
                        if g + 1 < ng:
                            stageG(g + 1, t, "ST")
                if debug:
                    S.dma("sync", dbg_ub[s][:, :, 0:ng * 512], ubT[:, :, 0:ng * 512], [("ubT", g, t) for g in range(ng) for t in range(4)], ["dbg_ub"], "dbg")
                S.flush()

        def pass_P(s, pre):
            wP, wo, wpg = pre
            with contextlib.ExitStack() as st_:
                wml = sb(st_, "wml", [128, 4, D], BF16)
                wgl_ = sb(st_, "wglb", [128, 4, D], BF16)
                wpl = sb(st_, "wpl", [128, 2, D], BF16)
                gin_bc = sb(st_, "gin_bcP", [128, D], F32)
                gpg_bc = sb(st_, "gpg_bc", [128, D], F32)
                gple_bc = sb(st_, "gple_bc", [128, D], F32)
                gfin_bc = sb(st_, "gfin_bc", [128, D], F32)
                NX = 5
                xt = [sb(st_, "xtP%d" % i, [128, D], F32) for i in range(NX)]
                hn = [sb(st_, "hnP%d" % i, [128, D], BF16) for i in range(2)]
                hT = sb(st_, "hTP", [128, 8, 512], BF16)
                junk_t = sb(st_, "junkP", [128, D], BF16)
                sa = [sb(st_, "sa%d" % i, [128, 512], BF16) for i in range(2)]
                sbb = [sb(st_, "sbb%d" % i, [128, 512], BF16) for i in range(2)]
                t1 = [sb(st_, "t1_%d" % i, [128, 512], BF16) for i in range(2)]
                t2 = [sb(st_, "t2_%d" % i, [128, 512], BF16) for i in range(2)]
                mg = sb(st_, "mg", [128, 8, 512], BF16)
                x1n = sb(st_, "x1n", [128, D], BF16)
                x1nT = sb(st_, "x1nT", [128, 8, 128], BF16)
                pbf = [sb(st_, "pbf%d" % i, [128, 256], BF16) for i in range(2)]
                pT_ = sb(st_, "pTP", [128, 2, 128], BF16)
                sig = sb(st_, "sig", [128, 512], F32)
                et = sb(st_, "et", [128, D], F32)
                ot = [sb(st_, "ot%d" % i, [128, D], F32) for i in range(2)]
                stt_ = sb(st_, "stP", [128, 48], F32)
                ctx = dict(xt=xt, hn=hn, st=stt_, junk=junk_t[:], hT=hT, gin_bc=gin_bc)

                load_w("wml", wml[:], kchunks(w_mla))
                load_w("wglb", wgl_[:], kchunks(w_gla))
                load_w("wpl", wpl[:], kchunks(w_ple))
                S.dma("sync", gin_bc[:], g_in.partition_broadcast(128), [], ["gin_bc"], "gin_bc")
                S.dma("sync", gpg_bc[:], g_pg.partition_broadcast(128), [], ["gpg_bc"], "gpg_bc")
                S.dma("sync", gple_bc[:], g_ple.partition_broadcast(128), [], ["gple_bc"], "gple_bc")
                S.dma("sync", gfin_bc[:], g_fin.partition_broadcast(128), [], ["gfin_bc"], "gfin_bc")
                pj_i = [0]
                rot = [1, 2, 3]

                def pj():
                    b = rot[pj_i[0] % 3]
                    pj_i[0] += 1
                    return b

                HT = [("hT", c0) for c0 in (0, 128, 256, 384)]

                def stageP(gn, t, parts="LST"):
                    stage_hT(ctx, s * SEQ + gn * 512 + t * 128, (4 * gn + t) % NX, t * 128, "vector", parts=parts)

                for t in range(4):
                    stageP(0, t)
                for g in range(ng):
                    tok0 = s * SEQ + g * 512
                    c0g = g * 512
                    for m in range(8):
                        sl = m % 2
                        ba = pj()
                        for k in range(8):
                            S.mm(ps[ba][:], wP[:, k, m * 128:(m + 1) * 128], hT[:, k, :], k == 0, k == 7, ["wP"] + HT, [("ps", ba)])
                        S.act(sa[sl][:], ps[ba][:], AF.Sigmoid, [("ps", ba)], [("sa", sl)])
                        by = pj()
                        for c in range(4):
                            S.mm(ps[by][:], wml[:, c, m * 128:(m + 1) * 128], uaT[:, c, c0g:c0g + 512], c == 0, c == 3, ["wml"], [("ps", by)])
                        S.tt("vector", t1[sl][:], ps[by][:], sa[sl][:], ALU.mult, [("ps", by), ("sa", sl)], [("t1", sl)])
                        bb = pj()
                        for k in range(8):
                            S.mm(ps[bb][:], wP[:, k, 1024 + m * 128:1024 + (m + 1) * 128], hT[:, k, :], k == 0, k == 7, ["wP"] + HT, [("ps", bb)])
                        S.act(sbb[sl][:], ps[bb][:], AF.Sigmoid, [("ps", bb)], [("sbb", sl)])
                        bz = pj()
                        for c in range(4):
                            S.mm(ps[bz][:], wgl_[:, c, m * 128:(m + 1) * 128], ubT[:, c, c0g:c0g + 512], c == 0, c == 3, ["wglb"], [("ps", bz)])
                        S.tt("vector", t2[sl][:], ps[bz][:], sbb[sl][:], ALU.mult, [("ps", bz), ("sbb", sl)], [("t2", sl)])
                        S.tt("gpsimd", mg[:, m, :], t1[sl][:], t2[sl][:], ALU.add, [("t1", sl), ("t2", sl)], [("mg", m)])
                    MG = [("mg", m) for m in range(8)]

                    def xs_of(t):
                        return (4 * g + t) % NX

                    def A1(t):
                        xs = xs_of(t)
                        cs = slice(t * 128, (t + 1) * 128)
                        for half in range(2):
                            b = 4 + half
                            hs_ = slice(half * 512, (half + 1) * 512)
                            for m in range(8):
                                S.mm(ps[b][:], mg[:, m, cs], wo[:, m, hs_], m == 0, m == 7, ["wo"] + MG, [("ps", b)])
                            S.tt("vector", xt[xs][:, hs_], xt[xs][:, hs_], ps[b][:], ALU.add, [("xt", xs), ("ps", b)], [("xt", xs)])
                        S.act(junk_t[:], xt[xs][:], AF.Square, [("xt", xs)], ["junk", "s1"], accum=stt_[:, 24:25])
                        S.act(stt_[:, 25:26], stt_[:, 24:25], AF.Ln, ["s1"], ["l1"], scale=1.0 / D, bias=EPS)
                        S.act(stt_[:, 26:27], stt_[:, 25:26], AF.Exp, ["l1"], ["r1"], scale=-0.5)

                    def A2(t):
                        xs = xs_of(t)
                        S.stt("vector", x1n[:], xt[xs][:], stt_[:, 26:27], gpg_bc[:], ALU.mult, ALU.mult, [("xt", xs), "r1", "gpg_bc"], ["x1n"])

                    def B(t):
                        tok = tok0 + t * 128
                        ps_ = t % 2
                        S.dma("gpsimd", pbf[ps_][:], pin[tok:tok + 128, :], [], [("pbf", ps_)], ("pbf", ps_))
                        for c in range(2):
                            S.tr(tpv[:, c * 128:(c + 1) * 128], pbf[ps_][:, c * 128:(c + 1) * 128], ident[:], [("pbf", ps_), "ident"], [("ps", 0)])
                        S.copy("vector", pT_[:], tpv[:, 0:256].rearrange("p (c t) -> p c t", c=2), [("ps", 0)], ["pTP"])
                        for half in range(2):
                            b = 1 + half
                            for c in range(2):
                                S.mm(ps[b][:], pT_[:, c, :], wpl[:, c, half * 512:(half + 1) * 512], c == 0, c == 1, ["wpl", "pTP"], [("ps", b)])
                            S.act(junk_t[:, 0:512], ps[b][:], AF.Square, [("ps", b)], ["junk", ("se", half)], accum=stt_[:, 27 + half:28 + half])
                        S.tt("vector", stt_[:, 29:30], stt_[:, 27:28], stt_[:, 28:29], ALU.add, [("se", 0), ("se", 1)], ["se2"])
                        S.act(stt_[:, 30:31], stt_[:, 29:30], AF.Ln, ["se2"], ["le"], scale=1.0 / D, bias=EPS)
                        S.act(stt_[:, 31:32], stt_[:, 30:31], AF.Exp, ["le"], ["re"], scale=-0.5)
                        for half in range(2):
                            b = 1 + half
                            hs_ = slice(half * 512, (half + 1) * 512)
                            S.stt("vector", et[:, hs_], ps[b][:], stt_[:, 31:32], gple_bc[:, hs_], ALU.mult, ALU.mult,
                                  [("ps", b), "re", "gple_bc"], [("et", half)])

                    def C1(t):
                        for c in range(8):
                            S.tr(tpv[:, c * 128:(c + 1) * 128], x1n[:, c * 128:(c + 1) * 128], ident[:], ["x1n", "ident"], [("ps", 0)])
                        S.copy("scalar", x1nT[:], tpv.rearrange("p (c t) -> p c t", c=8), [("ps", 0)], ["x1nT"])

                    def C2a(t):
                        for half in range(2):
                            b = 6 + half
                            hs_ = slice(half * 512, (half + 1) * 512)
                            for c in range(8):
                                S.mm(ps[b][:], x1nT[:, c, :], wpg[:, c, hs_], c == 0, c == 7, ["wpg", "x1nT"], [("ps", b)])

                    def C2b(t):
                        tok = tok0 + t * 128
                        xs = xs_of(t)
                        osl = t % 2
                        for half in range(2):
                            b = 6 + half
                            hs_ = slice(half * 512, (half + 1) * 512)
                            S.act(sig[:], ps[b][:], AF.Sigmoid, [("ps", b)], ["sig"])
                            S.tt("vector", et[:, hs_], et[:, hs_], sig[:], ALU.mult, [("et", half), "sig"], [("et", half)])
                        S.tt("vector", xt[xs][:], xt[xs][:], et[:], ALU.add, [("xt", xs), ("et", 0), ("et", 1)], [("xt", xs)])
                        S.act(junk_t[:], xt[xs][:], AF.Square, [("xt", xs)], ["junk", "s2"], accum=stt_[:, 32:33])
                        S.act(stt_[:, 33:34], stt_[:, 32:33], AF.Ln, ["s2"], ["l2"], scale=1.0 / D, bias=EPS)
                        S.act(stt_[:, 34:35], stt_[:, 33:34], AF.Exp, ["l2"], ["r2"], scale=-0.5)
                        S.stt("vector", ot[osl][:], xt[xs][:], stt_[:, 34:35], gfin_bc[:], ALU.mult, ALU.mult,
                              [("xt", xs), "r2", "gfin_bc"], [("ot", osl)])
                        S.dma("sync", out[tok:tok + 128, :], ot[osl][:], [("ot", osl)], [("out", tok)], ("ot", osl))

                    nxt = g + 1 < ng
                    A1(0)
                    A2(0)
                    for t in range(4):
                        if nxt and t >= 1:
                            stageP(g + 1, t - 1, "L")
                        C1(t)
                        if t + 1 < 4:
                            A1(t + 1)
                            A2(t + 1)
                        B(t)
                        if nxt and t >= 1:
                            stageP(g + 1, t - 1, "S")
                        C2a(t)
                        if nxt and t >= 1:
                            stageP(g + 1, t - 1, "T")
                        C2b(t)
                    if nxt:
                        stageP(g + 1, 3, "LST")
                S.flush()

        for s in range(nseq):
            if "M" in passes:
                pass_M(s)
            with contextlib.ExitStack() as pw:
                pre = (sb(pw, "wP", [128, 8, 2048], BF16), sb(pw, "wo", [128, 8, D], BF16), sb(pw, "wpg", [128, 8, D], BF16))
                if "G" in passes:
                    pass_G(s, pre)
                if "P" in passes:
                    pass_P(s, pre)
        if S.ops:
            S.flush()
    return nc


def _consts():
    c = np.zeros((128, C_END), np.float32)
    idx = np.arange(128)
    c[:, C_ID:C_ID + 128] = np.eye(128, dtype=np.float32)
    triu = (idx[None, :] >= idx[:, None]).astype(np.float32)
    c[:, C_TRI:C_TRI + 128] = triu
    c[:, C_L:C_L + 128] = -triu / 16.0
    c[:, C_U:C_U + 128] = -(idx[:, None] > idx[None, :]).astype(np.float32) / 16.0
    c[64, C_SEL:C_SEL + 64] = 1.0
    c[0, C_SEL + 64:C_SEL + 128] = 1.0
    inv_freq = 1.0 / (10000.0 ** (np.arange(0, 32, 2, dtype=np.float64) / 32.0))
    for r in range(32):
        c[64 + r, C_ROPE] = inv_freq[r % 16] / (2.0 * np.pi)
        c[64 + r, C_ROPE + 1] = -TWO_PI if r < 16 else TWO_PI
    return c


_NC_CACHE = {}


def _prep_shared(inp):
    f = lambda a: np.ascontiguousarray(np.asarray(a, dtype=np.float32))
    w_in = f(inp["w_in"][0])
    kr = w_in[:, 640:672]
    w_krz = np.zeros((D, 192), np.float32)
    w_krz[:, 64:96] = kr
    w_krz[:, 96 + 64:96 + 80] = kr[:, 16:32]
    w_krz[:, 96 + 80:96 + 96] = kr[:, 0:16]
    w_uq = f(inp["w_uq"][0])
    w_uqsw = np.zeros((384, 768), np.float32)
    for h in range(8):
        rp = w_uq[:, 96 * h + 64:96 * h + 96]
        w_uqsw[:, 96 * h + 64:96 * h + 80] = rp[:, 16:32]
        w_uqsw[:, 96 * h + 80:96 * h + 96] = rp[:, 0:16]
    w_ukv = f(inp["w_ukv"][0]).reshape(256, 8, 2, 64)
    w_ukvk = np.ascontiguousarray(w_ukv[:, :, 0, :].reshape(256, 512))
    w_ukvv = np.ascontiguousarray(w_ukv[:, :, 1, :].reshape(256, 512))
    w_gk17 = np.concatenate([f(inp["w_gk_up"][0]), f(inp["b_gk"][0]).reshape(1, 256)], axis=0)
    gcols = np.zeros((128, 8), np.float32)
    gcols[:, 0:3] = f(inp["q_norm_g"][0]).reshape(3, 128).T
    gcols[:, 3:5] = f(inp["kv_norm_g"][0]).reshape(2, 128).T
    return {
        "w_in": w_in, "w_krz": w_krz, "w_uq": w_uq, "w_uqsw": w_uqsw, "w_ukvk": w_ukvk, "w_ukvv": w_ukvv,
        "w_gk17": np.ascontiguousarray(w_gk17), "w_mla": f(inp["w_mla_br"][0]), "w_gla": f(inp["w_gla_br"][0]),
        "w_out": f(inp["w_out"][0]), "w_ple": f(inp["w_ple"][0]), "w_pg": f(inp["w_ple_gate"][0]),
        "g_in": f(inp["norm_in_g"][0]).reshape(1, D), "g_gla": f(inp["gla_norm_g"][0]).reshape(1, 128),
        "g_ple": f(inp["ple_norm_g"][0]).reshape(1, D), "g_pg": f(inp["ple_gate_norm_g"][0]).reshape(1, D),
        "g_fin": f(inp["final_norm_g"]).reshape(1, D), "gcols": gcols, "consts": _consts(),
    }


def kernel(**inputs):
    if "nc" not in _NC_CACHE:
        _NC_CACHE["nc"] = build(False)
    nc = _NC_CACHE["nc"]
    shared = _prep_shared(inputs)
    x = np.asarray(inputs["x"], dtype=np.float32)
    p = np.asarray(inputs["p"], dtype=np.float32)[0]
    pos = np.asarray(inputs["positions"], dtype=np.int32)
    in_maps = []
    for c in range(NCORES):
        m = dict(shared)
        m["x"] = np.ascontiguousarray(x[2 * c:2 * c + 2].reshape(2 * SEQ, D))
        m["p"] = np.ascontiguousarray(p[2 * c:2 * c + 2].reshape(2 * SEQ, 256))
        m["pos"] = np.ascontiguousarray(pos[2 * c:2 * c + 2].reshape(1, 2 * SEQ))
        in_maps.append(m)
    res = run_bass_kernel_spmd(nc, in_maps, core_ids=list(range(NCORES)))
    outs = [np.asarray(r["out"]).reshape(2, SEQ, D) for r in res.results]
    return np.concatenate(outs, axis=0).astype(np.float32)
```

```python
import contextlib
import numpy as np
import concourse.bass as bass
import concourse.mybir as mybir
from concourse.bass_utils import run_bass_kernel_spmd

F32 = mybir.dt.float32
BF16 = mybir.dt.bfloat16
I32 = mybir.dt.int32
AF = mybir.ActivationFunctionType
ALU = mybir.AluOpType

NCORES = 8
SEQ = 2048
D = 1024
EPS = 1e-6
TWO_PI = float(2.0 * np.pi * (1.0 - 2e-7))
ENGINES = ("sync", "scalar", "vector", "gpsimd", "tensor")

C_ID, C_TRI, C_L, C_U, C_SEL, C_ROPE, C_END = 0, 128, 256, 384, 512, 640, 644


class _Op:
    __slots__ = ("eng", "fn", "dma", "waits", "inc", "idx", "key", "pos")


class Sched:
    def __init__(self, nc, stack):
        self.nc = nc
        self.stack = stack
        self.sems = {}
        self.counts = {}
        self.seen = {e: {} for e in ENGINES}
        self._reset()

    def _reset(self):
        self.ops = []
        self.last_w = {}
        self.readers = {}

    def add(self, eng, fn, R=(), W=(), dma=False, stream=None):
        R = tuple(R) + tuple((r[0], r[1], h) for r in R if r == ("scr", 0) for h in ("a", "b"))
        W = tuple(W) + tuple((r[0], r[1], h) for r in W if r == ("scr", 0) for h in ("a", "b"))
        op = _Op()
        op.eng, op.fn, op.dma, op.inc, op.idx = eng, fn, dma, False, 0
        op.key = ("dma", stream) if dma else ("eng", eng)
        op.pos = len(self.ops)
        best = {}
        W = tuple(W) + tuple(r for r in R if isinstance(r, tuple) and r[0] == "ps" and r not in W)

        def dep(d, kind):
            if d is None or d is op:
                return
            if (not d.dma) and d.eng == eng and not dma:
                if eng == "tensor":
                    return
            cur = best.get(d.key)
            if cur is None or d.pos > cur.pos:
                best[d.key] = d

        for r in R:
            dep(self.last_w.get(r), "raw")
        for r in W:
            dep(self.last_w.get(r), "waw")
            for rd in self.readers.get(r, ()):
                dep(rd, "war")
        op.waits = list(best.values())
        for d in op.waits:
            d.inc = True
        for r in W:
            self.last_w[r] = op
            self.readers[r] = []
        for r in R:
            lst = self.readers.setdefault(r, [])
            for i, o in enumerate(lst):
                if o.key == op.key:
                    lst[i] = op
                    break
            else:
                lst.append(op)
        self.ops.append(op)
        return op

    def flush(self):
        nc = self.nc
        per_eng = {e: [] for e in ENGINES}
        for op in self.ops:
            per_eng[op.eng].append(op)
        for e in ENGINES:
            for op in reversed(per_eng[e]):
                if not op.dma:
                    op.inc = True
                    break
        for op in self.ops:
            if op.dma:
                op.inc = True
        for op in self.ops:
            if op.inc:
                k = op.key
                if k not in self.sems:
                    self.sems[k] = self.stack.enter_context(nc.semaphore("s%d" % len(self.sems)))
                    self.counts[k] = 0
                self.counts[k] += 16 if op.dma else 1
                op.idx = self.counts[k]
        finals = dict(self.counts)
        sems, seen_all = self.sems, self.seen

        def make(engname):
            def body(eng):
                seen = seen_all[engname]
                for op in per_eng[engname]:
                    for d in op.waits:
                        if seen.get(d.key, 0) < d.idx:
                            eng.wait_ge(sems[d.key], d.idx)
                            seen[d.key] = d.idx
                    ins = op.fn(eng)
                    if op.inc:
                        ins.then_inc(sems[op.key], 16 if op.dma else 1)
                for k, v in finals.items():
                    if seen.get(k, 0) < v:
                        eng.wait_ge(sems[k], v)
                        seen[k] = v
            return body

        with nc.Block() as block:
            block.sync(make("sync"))
            block.scalar(make("scalar"))
            block.vector(make("vector"))
            block.gpsimd(make("gpsimd"))
            block.tensor(make("tensor"))
        self._reset()

    def dma(self, eng, out, in_, R, W, stream):
        return self.add(eng, lambda e: e.dma_start(out=out, in_=in_), R, W, dma=True, stream=stream)

    def mm(self, out, lhsT, rhs, start, stop, R, W, tp=None):
        if tp is not None:
            return self.add("tensor", lambda e: e.matmul(out, lhsT=lhsT, rhs=rhs, start=start, stop=stop, tile_position=tp), R, W)
        return self.add("tensor", lambda e: e.matmul(out, lhsT=lhsT, rhs=rhs, start=start, stop=stop), R, W)

    def tr(self, out, in_, ident, R, W):
        return self.add("tensor", lambda e: e.transpose(out=out, in_=in_, identity=ident), R, W)

    def act(self, out, in_, func, R, W, scale=None, bias=None, accum=None):
        kw = {}
        if scale is not None:
            kw["scale"] = scale
        if bias is not None:
            kw["bias"] = bias
        if accum is not None:
            kw["accum_out"] = accum
        return self.add("scalar", lambda e: e.activation(out=out, in_=in_, func=func, **kw), R, W)

    def copy(self, eng, out, in_, R, W):
        if eng == "scalar":
            return self.add(eng, lambda e: e.copy(out=out, in_=in_), R, W)
        return self.add(eng, lambda e: e.tensor_copy(out=out, in_=in_), R, W)

    def tt(self, eng, out, in0, in1, op, R, W):
        return self.add(eng, lambda e: e.tensor_tensor(out=out, in0=in0, in1=in1, op=op), R, W)

    def ts(self, eng, out, in0, s1, op0, R, W, s2=None, op1=None):
        if op1 is None:
            return self.add(eng, lambda e: e.tensor_scalar(out=out, in0=in0, scalar1=s1, scalar2=None, op0=op0), R, W)
        return self.add(eng, lambda e: e.tensor_scalar(out=out, in0=in0, scalar1=s1, scalar2=s2, op0=op0, op1=op1), R, W)

    def stt(self, eng, out, in0, scalar, in1, op0, op1, R, W):
        return self.add(eng, lambda e: e.scalar_tensor_tensor(out=out, in0=in0, scalar=scalar, in1=in1, op0=op0, op1=op1), R, W)

    def memset(self, eng, ap, val, W):
        return self.add(eng, lambda e: e.memset(ap, val), (), W)

    def recip(self, out, in_, R, W):
        return self.add("vector", lambda e: e.reciprocal(out=out, in_=in_), R, W)


def build(debug=False, nseq=2, ng=4, passes="MGP", cut=99):
    nc = bass.Bass("TRN2", target_bir_lowering=False)

    def din(name, shape, dt=F32):
        return nc.dram_tensor(name, list(shape), dt, kind="ExternalInput").ap()

    x = din("x", [2 * SEQ, D])
    pin = din("p", [2 * SEQ, 256])
    pos = din("pos", [1, 2 * SEQ], I32)
    w_in = din("w_in", [D, 4784])
    w_krz = din("w_krz", [D, 192])
    w_uq = din("w_uq", [384, 768])
    w_uqsw = din("w_uqsw", [384, 768])
    w_ukvk = din("w_ukvk", [256, 512])
    w_ukvv = din("w_ukvv", [256, 512])
    w_gk17 = din("w_gk17", [17, 256])
    w_mla = din("w_mla", [512, D])
    w_gla = din("w_gla", [512, D])
    w_out = din("w_out", [D, D])
    w_ple = din("w_ple", [256, D])
    w_pg = din("w_pg", [D, D])
    g_in = din("g_in", [1, D])
    g_gla = din("g_gla", [1, 128])
    g_ple = din("g_ple", [1, D])
    g_pg = din("g_pg", [1, D])
    g_fin = din("g_fin", [1, D])
    gcols = din("gcols", [128, 8])
    consts = din("consts", [128, C_END])
    out = nc.dram_tensor("out", [2 * SEQ, D], F32, kind="ExternalOutput").ap()
    if debug:
        dbg_ua = nc.dram_tensor("dbg_ua", [2, 128, 4, SEQ], BF16, kind="ExternalOutput").ap()
        dbg_ub = nc.dram_tensor("dbg_ub", [2, 128, 4, SEQ], BF16, kind="ExternalOutput").ap()

    top = contextlib.ExitStack()
    with top:
        uid = [0]

        def sb(stack, name, shape, dt):
            uid[0] += 1
            return stack.enter_context(nc.sbuf_tensor("%s_%d" % (name, uid[0]), list(shape), dt))

        S = Sched(nc, top)
        cst = sb(top, "cst", [128, C_END], F32)
        gc = sb(top, "gc", [128, 8], F32)
        ident = sb(top, "ident", [128, 128], BF16)
        tri = sb(top, "tri", [128, 4, 128], BF16)
        ones_bf = sb(top, "ones_bf", [128, 128], BF16)
        uaT = sb(top, "uaT", [128, 4, SEQ], BF16)
        ubT = sb(top, "ubT", [128, 4, SEQ], BF16)
        ps = [top.enter_context(nc.psum_tensor("ps%d" % b, [128, 512], F32)) for b in range(8)]
        tpv = ps[0][:].bitcast(BF16)

        S.dma("sync", cst[:], consts, [], ["cst"], "cst")
        S.dma("sync", gc[:], gcols, [], ["gc"], "gc")
        S.copy("vector", ident[:], cst[:, C_ID:C_ID + 128], ["cst"], ["ident"])
        for h in range(4):
            S.copy("vector", tri[:, h, :], cst[:, C_TRI:C_TRI + 128], ["cst"], ["tri"])
        S.memset("vector", ones_bf[:], 1.0, ["ones_bf"])
        Lmat = cst[:, C_L:C_L + 128]
        Umat = cst[:, C_U:C_U + 128]

        def stage_hT(ctx, tok0, xs, col0, evac_eng, hT=None, htag=("hT",), parts="LST"):
            xt, hn, st, junk, gbc = ctx["xt"], ctx["hn"], ctx["st"], ctx["junk"], ctx["gin_bc"]
            if hT is None:
                hT = ctx["hT"]
            hs = xs % len(hn)
            if "L" in parts:
                S.dma("sync", xt[xs][:], x[tok0:tok0 + 128, :], [], [("xt", xs)], ("xt", xs))
            if "S" in parts:
                S.act(junk, xt[xs][:], AF.Square, [("xt", xs)], ["junk", ("ss", xs)], accum=st[:, xs:xs + 1])
                S.act(st[:, 8 + xs:9 + xs], st[:, xs:xs + 1], AF.Ln, [("ss", xs)], [("ln", xs)], scale=1.0 / D, bias=EPS)
                S.act(st[:, 16 + xs:17 + xs], st[:, 8 + xs:9 + xs], AF.Exp, [("ln", xs)], [("rs", xs)], scale=-0.5)
                S.stt("vector", hn[hs][:], xt[xs][:], st[:, 16 + xs:17 + xs], gbc[:], ALU.mult, ALU.mult,
                      [("xt", xs), ("rs", xs), "gin_bc"], [("hn", hs)])
            if "T" in parts:
                for c in range(8):
                    S.tr(tpv[:, c * 128:(c + 1) * 128], hn[hs][:, c * 128:(c + 1) * 128], ident[:],
                         [("hn", hs), "ident"], [("ps", 0)])
                S.copy(evac_eng, hT[:, :, col0:col0 + 128], tpv.rearrange("p (c t) -> p c t", c=8),
                       [("ps", 0)], [htag + (col0,)])

        def load_w(name, dst, src, R=()):
            S.dma("gpsimd", dst, src, list(R), [name], name)

        def kchunks(w):
            return w.rearrange("(c p) n -> p c n", p=128)

        def pass_M(s):
            with contextlib.ExitStack() as st_:
                wM = sb(st_, "wM", [128, 8, 1152], BF16)
                wkr = sb(st_, "wkr", [128, 8, 192], BF16)
                wuq = sb(st_, "wuq", [128, 3, 768], BF16)
                wuqs = sb(st_, "wuqs", [128, 3, 768], BF16)
                wkk = sb(st_, "wkk", [128, 2, 512], BF16)
                wkv = sb(st_, "wkv", [128, 2, 512], BF16)
                kT = sb(st_, "kT", [128, 8, SEQ], BF16)
                Ve = sb(st_, "Ve", [128, 16, 4, 128], BF16)
                Vo = sb(st_, "Vo", [128, 16, 4, 128], BF16)
                gin_bc = sb(st_, "gin_bc", [128, D], F32)
                xt = [sb(st_, "xt%d" % i, [128, D], F32) for i in range(2)]
                hn = [sb(st_, "hn%d" % i, [128, D], BF16) for i in range(2)]
                hTs = [sb(st_, "hT%d" % i, [128, 8, 512], BF16) for i in range(2)]
                scr = sb(st_, "scr", [128, 3, 512], F32)
                sq = sb(st_, "sq", [128, 3, 512], BF16)
                rq = sb(st_, "rq", [128, 512], F32)
                cqn = sb(st_, "cqn", [128, 3, 512], BF16)
                ckvn = sb(st_, "ckvn", [128, 2, 512], BF16)
                sg = sb(st_, "sg", [128, 4, 512], BF16)
                qT = sb(st_, "qT", [128, 8, 512], BF16)
                posi = sb(st_, "posi", [128, 512], I32)
                cos2 = sb(st_, "cos2", [128, 512], F32)
                sin2 = sb(st_, "sin2", [128, 512], F32)
                pT = [sb(st_, "pT%d" % i, [128, 512], BF16) for i in range(4)]
                stt_ = sb(st_, "stM", [128, 24], F32)
                junk = sq[:, 0:2, :].rearrange("p a b -> p (a b)")
                ctx = dict(xt=xt, hn=hn, st=stt_, junk=junk, hT=None, gin_bc=gin_bc)

                def stageM(gn, t):
                    stage_hT(ctx, s * SEQ + gn * 512 + t * 128, t % 2, t * 128, "scalar" if t % 2 else "vector",
                             hT=hTs[gn % 2], htag=("hT", gn % 2))

                wi = kchunks(w_in)
                load_w("wM_a", wM[:, :, 0:640], wi[:, :, 0:640])
                load_w("wM_b", wM[:, :, 640:1152], wi[:, :, 672:1184])
                load_w("wkr", wkr[:], kchunks(w_krz))
                load_w("wuq", wuq[:], kchunks(w_uq))
                load_w("wuqs", wuqs[:], kchunks(w_uqsw))
                load_w("wkk", wkk[:], kchunks(w_ukvk))
                load_w("wkv", wkv[:], kchunks(w_ukvv))
                S.dma("sync", gin_bc[:], g_in.partition_broadcast(128), [], ["gin_bc"], "gin_bc")
                S.memset("vector", Ve[:, :, :, 64:128], 1.0, ["Vones"])
                S.memset("vector", Vo[:, :, :, 0:64], 1.0, ["Vones"])
                WM = ["wM_a", "wM_b"]
                SCALE = float(96 ** -0.5)
                for t in range(4):
                    stageM(0, t)
                r64 = slice(64, 96)
                pj_rot = [1, 2, 3, 4, 7]
                pj_i = [0]

                def pj():
                    b = pj_rot[pj_i[0] % 5]
                    pj_i[0] += 1
                    return b

                for g in range(ng):
                    tok0 = s * SEQ + g * 512
                    c0g = g * 512
                    hT = hTs[g % 2]
                    HT = [("hT", g % 2, c0) for c0 in (0, 128, 256, 384)]
                    S.dma("sync", posi[r64, :], pos[0:1, tok0:tok0 + 512].partition_broadcast(32), [], ["posi"], "posi")
                    S.copy("vector", scr[r64, 0, :], posi[r64, :], ["posi"], [("scr", 0)])
                    S.ts("vector", scr[r64, 0, :], scr[r64, 0, :], cst[r64, C_ROPE:C_ROPE + 1], ALU.mult,
                         [("scr", 0), "cst"], [("scr", 0)])
                    S.copy("vector", posi[r64, :], scr[r64, 0, :], [("scr", 0)], ["posi"])
                    S.copy("vector", scr[r64, 1, :], posi[r64, :], ["posi"], [("scr", 1)])
                    S.tt("vector", scr[r64, 1, :], scr[r64, 0, :], scr[r64, 1, :], ALU.subtract,
                         [("scr", 0), ("scr", 1)], [("scr", 1)])
                    S.act(sin2[r64, :], scr[r64, 1, :], AF.Sin, [("scr", 1), "cst"], ["sin2"],
                          scale=cst[r64, C_ROPE + 1:C_ROPE + 2])
                    S.ts("vector", scr[r64, 2, :], scr[r64, 0, :], 0.25, ALU.add, [("scr", 0)], [("scr", 2)])
                    S.copy("vector", posi[r64, :], scr[r64, 2, :], [("scr", 2)], ["posi"])
                    S.copy("vector", scr[r64, 1, :], posi[r64, :], ["posi"], [("scr", 1)])
                    S.tt("vector", scr[r64, 1, :], scr[r64, 2, :], scr[r64, 1, :], ALU.subtract,
                         [("scr", 2), ("scr", 1)], [("scr", 1)])
                    S.act(cos2[r64, :], scr[r64, 1, :], AF.Sin, [("scr", 1)], ["cos2"], scale=TWO_PI)


                    def proj(col, width=128, w=wM, wn=WM, m0=0):
                        b = pj()
                        for k in range(8):
                            S.mm(ps[b][m0:m0 + width, :], w[:, k, col:col + width], hT[:, k, :],
                                 k == 0, k == 7, wn + HT, [("ps", b)])
                        return b

                    def lowrank_norm(col_base, nch, gcol0, dst, dname, inv_n):
                        for c in range(nch):
                            b = proj(col_base + c * 128)
                            S.act(sq[:, c, :], ps[b][:], AF.Square, [("ps", b)], [("sq", c)])
                            S.copy("vector", scr[:, c, :], ps[b][:], [("ps", b)], [("scr", c)])
                        b = pj()
                        for c in range(nch):
                            S.mm(ps[b][:], ones_bf[:], sq[:, c, :], c == 0, c == nch - 1,
                                 ["ones_bf", ("sq", c)], [("ps", b)])
                        if cut == 24:
                            return
                        S.act(rq[:], ps[b][:], AF.Ln, [("ps", b)], ["rq"], scale=inv_n, bias=EPS)
                        if cut == 25:
                            return
                        S.act(rq[:], rq[:], AF.Exp, ["rq"], ["rq"], scale=-0.5)
                        if cut == 26:
                            return
                        for c in range(nch):
                            S.stt("vector", dst[:, c, :], scr[:, c, :], gc[:, gcol0 + c:gcol0 + c + 1], rq[:],
                                  ALU.mult, ALU.mult, [("scr", c), "gc", "rq"], [(dname, c)])

                    if cut == 20:
                        b = proj(0)
                        S.flush()
                        return
                    if cut == 21:
                        b = proj(0)
                        S.act(sq[:, 0, :], ps[b][:], AF.Square, [("ps", b)], [("sq", 0)])
                        S.flush()
                        return
                    if cut == 22:
                        b = proj(0)
                        S.copy("vector", scr[:, 0, :], ps[b][:], [("ps", b)], [("scr", 0)])
                        S.flush()
                        return
                    lowrank_norm(0, 3, 0, cqn, "cqn", 1.0 / 384)
                    CQN = [("cqn", c) for c in range(3)]
                    if cut in (23, 24, 25, 26):
                        S.flush()
                        return
                    lowrank_norm(384, 2, 3, ckvn, "ckvn", 1.0 / 256)
                    CKV = [("ckvn", c) for c in range(2)]
                    for c in range(4):
                        b = proj(640 + c * 128)
                        S.act(sg[:, c, :], ps[b][:], AF.Silu, [("ps", b)], [("sg", c)])
                    if cut == 3:
                        S.flush()
                        return
                    ba = proj(0, 96, wkr, ["wkr"])
                    bb = proj(96, 96, wkr, ["wkr"])
                    S.tt("vector", scr[r64, 0, :], ps[ba][r64, :], cos2[r64, :], ALU.mult, [("ps", ba), "cos2"], [("scr", 0)])
                    S.tt("vector", scr[r64, 1, :], ps[bb][r64, :], sin2[r64, :], ALU.mult, [("ps", bb), "sin2"], [("scr", 1)])
                    S.tt("vector", kT[r64, 0, c0g:c0g + 512], scr[r64, 0, :], scr[r64, 1, :], ALU.add,
                         [("scr", 0), ("scr", 1)], [("kTr", 0, g)])
                    for h in range(1, 8):
                        S.copy("vector", kT[r64, h, c0g:c0g + 512], kT[r64, 0, c0g:c0g + 512],
                               [("kTr", 0, g)], [("kTr", h, g)])
                    if cut == 4:
                        S.flush()
                        return
                    for h in range(8):
                        ba, bb = pj(), pj()
                        for c in range(3):
                            S.mm(ps[ba][0:96, :], wuq[:, c, 96 * h:96 * h + 96], cqn[:, c, :], c == 0, c == 2,
                                 ["wuq"] + CQN, [("ps", ba)])
                        for c in range(3):
                            S.mm(ps[bb][0:96, :], wuqs[:, c, 96 * h:96 * h + 96], cqn[:, c, :], c == 0, c == 2,
                                 ["wuqs"] + CQN, [("ps", bb)])
                        S.copy("scalar", qT[0:64, h, :], ps[ba][0:64, :], [("ps", ba)], [("qTn", h)])
                        S.tt("vector", scr[r64, 0, :], ps[ba][r64, :], cos2[r64, :], ALU.mult, [("ps", ba), "cos2"], [("scr", 0)])
                        S.tt("vector", scr[r64, 1, :], ps[bb][r64, :], sin2[r64, :], ALU.mult, [("ps", bb), "sin2"], [("scr", 1)])
                        S.tt("vector", qT[r64, h, :], scr[r64, 0, :], scr[r64, 1, :], ALU.add,
                             [("scr", 0), ("scr", 1)], [("qTr", h)])
                    for h in range(8):
                        b = pj()
                        for c in range(2):
                            S.mm(ps[b][0:64, :], wkk[:, c, 64 * h:64 * h + 64], ckvn[:, c, :], c == 0, c == 1,
                                 ["wkk"] + CKV, [("ps", b)])
                        S.copy("scalar" if h % 2 else "vector", kT[0:64, h, c0g:c0g + 512], ps[b][0:64, :],
                               [("ps", b)], [("kTn", h, g)])
                    for t in range(4):
                        T = g * 4 + t
                        b = pj()
                        for c in range(2):
                            S.mm(ps[b][:], ckvn[:, c, t * 128:(t + 1) * 128], wkv[:, c, :], c == 0, c == 1,
                                 ["wkv"] + CKV, [("ps", b)])
                        pv = ps[b][:].rearrange("p (i two d) -> p i two d", two=2, d=64)
                        S.copy("vector", Ve[:, T, :, 0:64], pv[:, :, 0, :], [("ps", b)], [("Ve", T)])
                        S.copy("scalar", Vo[:, T, :, 64:128], pv[:, :, 1, :], [("ps", b)], [("Vo", T)])

                    if cut == 5:
                        S.flush()
                        return
                    nk = 4 * (g + 1)
                    for i in range(4):
                        hA, hB = 2 * i, 2 * i + 1
                        if g + 1 < ng:
                            stageM(g + 1, i)
                        steps = [(j, hh) for j in range(nk) for hh in (0, 1)]
                        pend = None
                        for n, (j, hh) in enumerate(steps):
                            h = hA if hh == 0 else hB
                            r = j - 4 * g
                            c0 = 128 * r if r > 0 else 0
                            gj = j // 4
                            b = pj()
                            slot = n % 4
                            S.mm(ps[b][:, c0:512], kT[0:96, h, j * 128:(j + 1) * 128], qT[0:96, h, c0:512], True, True,
                                 [("kTn", h, gj), ("kTr", h, gj), ("qTn", h), ("qTr", h)], [("ps", b)])
                            S.act(pT[slot][:, c0:512], ps[b][:, c0:512], AF.Exp, [("ps", b)], [("pT", slot)], scale=SCALE)
                            if r >= 0:
                                S.tt("vector", pT[slot][:, c0:c0 + 128], pT[slot][:, c0:c0 + 128], tri[:, 0, :], ALU.mult,
                                     [("pT", slot), "tri"], [("pT", slot)])
                            if pend is not None:
                                pend()
                            ob = 5 + hh
                            if hh == 0:
                                lhsT = Ve[:, j, i, :]
                                orow = slice(0, 128)
                                vr = [("Ve", j), "Vones"]
                            else:
                                lhsT = Vo[:, j, i, :]
                                orow = slice(0, 128)
                                vr = [("Vo", j), "Vones"]

                            def pend(ob=ob, orow=orow, c0=c0, lhsT=lhsT, slot=slot, j=j, vr=vr):
                                S.mm(ps[ob][orow, c0:512], lhsT, pT[slot][:, c0:512], j == 0, j == nk - 1,
                                     vr + [("pT", slot)], [("ps", ob)])
                        pend()
                        if cut == 6:
                            S.flush()
                            return
                        S.act(scr[64:128, 0, :], ps[5][64:128, :], AF.Ln, [("ps", 5)], [("scr", 0, "a")])
                        S.act(scr[64:128, 0, :], scr[64:128, 0, :], AF.Exp, [("scr", 0, "a")], [("scr", 0, "a")], scale=-1.0)
                        S.act(scr[0:64, 0, :], ps[6][0:64, :], AF.Ln, [("ps", 6)], [("scr", 0, "b")])
                        S.act(scr[0:64, 0, :], scr[0:64, 0, :], AF.Exp, [("scr", 0, "b")], [("scr", 0, "b")], scale=-1.0)
                        S.copy("vector", scr[0:64, 2, :], scr[64:128, 0, :], [("scr", 0, "a")], [("scr", 2)])
                        S.copy("vector", scr[64:128, 2, :], scr[0:64, 0, :], [("scr", 0, "b")], [("scr", 2)])
                        S.tt("vector", scr[:, 1, :], scr[:, 2, :], sg[:, i, :], ALU.mult, [("scr", 2), ("sg", i)], [("scr", 1)])
                        S.tt("vector", uaT[0:64, i, c0g:c0g + 512], ps[5][0:64, :], scr[0:64, 1, :], ALU.mult,
                             [("ps", 5), ("scr", 1)], [("uaT", i, g)])
                        S.tt("vector", uaT[64:128, i, c0g:c0g + 512], ps[6][64:128, :], scr[64:128, 1, :], ALU.mult,
                             [("ps", 6), ("scr", 1)], [("uaT", i, g)])
                if debug:
                    S.dma("sync", dbg_ua[s][:, :, 0:ng * 512], uaT[:, :, 0:ng * 512], [("uaT", i, g) for i in range(4) for g in range(ng)], ["dbg_ua"], "dbg")
                S.flush()

        def pass_G(s, pre):
            with contextlib.ExitStack() as st_:
                wG = sb(st_, "wG", [128, 8, 1536], BF16)
                wgl = sb(st_, "wgl", [128, 8, 16], BF16)
                wg17 = sb(st_, "wg17", [128, 256], BF16)
                gin_bc = sb(st_, "gin_bcG", [128, D], F32)
                ggla_bc = sb(st_, "ggla_bc", [128, 128], F32)
                xt = [sb(st_, "xtG%d" % i, [128, D], F32) for i in range(2)]
                hn = [sb(st_, "hnG%d" % i, [128, D], BF16) for i in range(2)]
                hTs = [sb(st_, "hTG%d" % i, [128, 8, 512], BF16) for i in range(2)]
                junk_t = sb(st_, "junkG", [128, D], BF16)
                gqf = sb(st_, "gqf", [128, 4, 512], F32)
                gkf = sb(st_, "gkf", [128, 4, 512], F32)
                sgg = sb(st_, "sgg", [128, 4, 512], BF16)
                gkl = sb(st_, "gkl", [128, 512], BF16)
                vsb = [sb(st_, "vsb%d" % i, [128, 512], BF16) for i in range(2)]
                etmp = sb(st_, "etmp", [128, 256], F32)
                sp = sb(st_, "sp", [128, 256], F32)
                ebT = sb(st_, "ebT", [128, 4, 128], F32)
                enbT = sb(st_, "enbT", [128, 4, 128], F32)
                erev = sb(st_, "erev", [128, 256], F32)
                qtT = sb(st_, "qtT", [128, 4, 128], BF16)
                ktT = sb(st_, "ktT", [128, 4, 128], BF16)
                kdec = sb(st_, "kdec", [128, 256], BF16)
                AT = sb(st_, "AT", [128, 4, 128], BF16)
                onsb = sb(st_, "onsb", [128, 512], BF16)
                Sf = sb(st_, "Sf", [128, 4, 128], F32)
                Sb = sb(st_, "Sb", [128, 4, 128], BF16)
                junk2 = sb(st_, "junk2", [128, 128], BF16)
                stt_ = sb(st_, "stG", [128, 40], F32)
                ctx = dict(xt=xt, hn=hn, st=stt_, junk=junk_t[:], hT=None, gin_bc=gin_bc)
                r0_ = slice(0, 64)

                def stageG(gn, t, parts="LST"):
                    stage_hT(ctx, s * SEQ + gn * 512 + t * 128, t % 2, t * 128, "scalar" if t % 2 else "vector",
                             hT=hTs[gn % 2], htag=("hT", gn % 2), parts=parts)

                wi = kchunks(w_in)
                load_w("wG_a", wG[:, :, 0:1024], wi[:, :, 1184:2208])
                load_w("wG_b", wG[:, :, 1024:1536], wi[:, :, 2224:2736])
                load_w("wgl", wgl[:], wi[:, :, 2208:2224])
                load_w("wg17", wg17[0:17, :], w_gk17)
                load_w("wP", pre[0][:], wi[:, :, 2736:4784])
                load_w("wo", pre[1][:], kchunks(w_out))
                load_w("wpg", pre[2][:], kchunks(w_pg))
                S.dma("sync", gin_bc[:], g_in.partition_broadcast(128), [], ["gin_bc"], "gin_bc")
                S.dma("sync", ggla_bc[:], g_gla.partition_broadcast(128), [], ["ggla_bc"], "ggla_bc")
                S.memset("vector", gkl[:], 1.0, ["gkl"])
                S.memset("vector", Sf[:], 0.0, ["Sf"])
                S.memset("vector", Sb[:], 0.0, ["Sb"])
                WG = ["wG_a", "wG_b"]
                pj_i = [0]

                def pj():
                    b = 1 + pj_i[0] % 2
                    pj_i[0] += 1
                    return b

                for t in range(4):
                    stageG(0, t)
                for g in range(ng):
                    tok0 = s * SEQ + g * 512
                    c0g = g * 512
                    hT = hTs[g % 2]
                    HT = [("hT", g % 2, c0) for c0 in (0, 128, 256, 384)]

                    def proj(col, width=128, w=wG, wn=WG):
                        b = pj()
                        for k in range(8):
                            S.mm(ps[b][0:width, :], w[:, k, col:col + width], hT[:, k, :], k == 0, k == 7,
                                 wn + HT, [("ps", b)])
                        return b

                    for c in range(2):
                        b = proj(c * 128)
                        S.copy("scalar", gqf[r0_, 2 * c, :], ps[b][0:64, :], [("ps", b)], [("gqf", 2 * c)])
                        S.copy("vector", gqf[r0_, 2 * c + 1, :], ps[b][64:128, :], [("ps", b)], [("gqf", 2 * c + 1)])
                    for c in range(2):
                        b = proj(256 + c * 128)
                        S.copy("scalar", gkf[r0_, 2 * c, :], ps[b][0:64, :], [("ps", b)], [("gkf", 2 * c)])
                        S.copy("vector", gkf[r0_, 2 * c + 1, :], ps[b][64:128, :], [("ps", b)], [("gkf", 2 * c + 1)])
                    GQ = [("gqf", h) for h in range(4)]
                    GK = [("gkf", h) for h in range(4)]
                    for c in range(4):
                        b = proj(1024 + c * 128)
                        S.act(sgg[:, c, :], ps[b][:], AF.Silu, [("ps", b)], [("sgg", c)])
                    b = proj(0, 16, wgl, ["wgl"])
                    S.copy("vector", gkl[0:16, :], ps[b][0:16, :], [("ps", b)], ["gkl"])

                    def F(t):
                        cs = slice(t * 128, (t + 1) * 128)
                        vs = t % 2
                        htr = [("hT", g % 2, t * 128)]
                        for k in range(8):
                            S.mm(ps[3][:], hT[:, k, cs], wG[:, k, 512:1024], k == 0, k == 7, WG + htr, [("ps", 3)])
                        S.copy("scalar", vsb[vs][:], ps[3][:], [("ps", 3)], [("vsb", vs)])
                        for k in range(8):
                            S.mm(ps[4][:, 0:256], hT[:, k, cs], wG[:, k, 256:512], k == 0, k == 7, WG + htr, [("ps", 4)])
                        S.mm(ps[4][:, 256:512], gkl[0:17, cs], wg17[0:17, :], True, True, ["gkl", "wg17"], [("ps", 4)])
                        S.act(etmp[:], ps[4][:, 256:512], AF.Exp, [("ps", 4)], ["etmp"], scale=-1.0)
                        S.act(sp[:], etmp[:], AF.Ln, ["etmp"], ["sp"], bias=1.0)
                        for h in range(4):
                            S.mm(ps[5][0:64, h * 128:(h + 1) * 128], sp[:, h * 64:(h + 1) * 64], Lmat, True, True,
                                 ["sp", "cst"], [("ps", 5)])
                        S.mm(ps[2][:, 0:256], Umat, sp[:], True, True, ["sp", "cst"], [("ps", 2)])
                        bt = ps[5][0:64, :].rearrange("p (h t) -> p h t", h=4)
                        S.act(ebT[r0_], bt, AF.Exp, [("ps", 5)], ["ebT"])
                        S.act(enbT[r0_], bt, AF.Exp, [("ps", 5)], ["enbT"], scale=-1.0)
                        S.act(erev[:], ps[2][:, 0:256], AF.Exp, [("ps", 2)], ["erev"])
                        S.stt("vector", qtT[r0_], gqf[r0_, :, cs], 0.125, ebT[r0_], ALU.mult, ALU.mult, GQ + ["ebT"], ["qtT"])
                        S.tt("vector", ktT[r0_], gkf[r0_, :, cs], enbT[r0_], ALU.mult, GK + ["enbT"], ["ktT"])
                        S.tt("vector", kdec[:], ps[4][:, 0:256], erev[:], ALU.mult, [("ps", 4), "erev"], ["kdec"])
                        for h in range(4):
                            S.mm(ps[6][:, h * 128:(h + 1) * 128], ktT[r0_, h, :], qtT[r0_, h, :], True, True,
                                 ["ktT", "qtT"], [("ps", 6)])
                        S.tt("vector", AT[:], ps[6][:].rearrange("p (h t) -> p h t", h=4), tri[:], ALU.mult,
                             [("ps", 6), "tri"], ["AT"])
                    def O(t):
                        cs = slice(t * 128, (t + 1) * 128)
                        vs = t % 2
                        for h in range(4):
                            hs_ = slice(h * 128, (h + 1) * 128)
                            S.mm(ps[7][:, hs_], AT[:, h, :], vsb[vs][:, hs_], True, False, ["AT", ("vsb", vs)], [("ps", 7)])
                            S.mm(ps[7][:, hs_], qtT[r0_, h, :], Sb[r0_, h, :], False, True, ["qtT", "Sb"], [("ps", 7)])
                        for h in range(4):
                            hs_ = slice(h * 128, (h + 1) * 128)
                            S.mm(ps[1][0:64, hs_], kdec[:, h * 64:(h + 1) * 64], vsb[vs][:, hs_], True, True,
                                 ["kdec", ("vsb", vs)], [("ps", 1)])
                        for h in range(4):
                            hs_ = slice(h * 128, (h + 1) * 128)
                            S.stt("vector", Sf[r0_, h, :], Sf[r0_, h, :], ebT[r0_, h, 127:128], ps[1][0:64, hs_], ALU.mult, ALU.add,
                                  ["Sf", "ebT", ("ps", 1)], ["Sf"])
                        S.copy("scalar", Sb[r0_], Sf[r0_], ["Sf"], ["Sb"])
                    def N(t):
                        cs = slice(t * 128, (t + 1) * 128)
                        vs = t % 2
                        for h in range(4):
                            S.act(junk2[:], ps[7][:, h * 128:(h + 1) * 128], AF.Square, [("ps", 7)], ["junk2", "oss"],
                                  accum=stt_[:, 24 + h:25 + h])
                        S.act(stt_[:, 28:32], stt_[:, 24:28], AF.Ln, ["oss"], ["oln"], scale=1.0 / 128, bias=EPS)
                        S.act(stt_[:, 32:36], stt_[:, 28:32], AF.Exp, ["oln"], ["ors"], scale=-0.5)
                        for h in range(4):
                            hs_ = slice(h * 128, (h + 1) * 128)
                            S.stt("vector", onsb[:, hs_], ps[7][:, hs_], stt_[:, 32 + h:33 + h], ggla_bc[:], ALU.mult, ALU.mult,
                                  [("ps", 7), "ors", "ggla_bc"], ["onsb"])
                        for h in range(4):
                            hs_ = slice(h * 128, (h + 1) * 128)
                            S.tr(tpv[:, hs_], onsb[:, hs_], ident[:], ["onsb", "ident"], [("ps", 0)])
                        S.tt("vector", ubT[:, :, c0g + t * 128:c0g + (t + 1) * 128],
                             tpv[:, 0:512].rearrange("p (h t) -> p h t", h=4), sgg[:, :, cs], ALU.mult,
                             [("ps", 0)] + [("sgg", c) for c in range(4)], [("ubT", g, t)])
                    F(0)
                    for t in range(4):
                        if g + 1 < ng:
                            stageG(g + 1, t, "L")
                        O(t)
                        if t + 1 < 4:
                            F(t + 1)
                        N(t)
                        if g + 1 < ng:
                            stageG(g + 1, t, "ST")
                if debug:
                    S.dma("sync", dbg_ub[s][:, :, 0:ng * 512], ubT[:, :, 0:ng * 512], [("ubT", g, t) for g in range(ng) for t in range(4)], ["dbg_ub"], "dbg")
                S.flush()

        def pass_P(s, pre):
            wP, wo, wpg = pre
            with contextlib.ExitStack() as st_:
                wml = sb(st_, "wml", [128, 4, D], BF16)
                wgl_ = sb(st_, "wglb", [128, 4, D], BF16)
                wpl = sb(st_, "wpl", [128, 2, D], BF16)
                gin_bc = sb(st_, "gin_bcP", [128, D], F32)
                gpg_bc = sb(st_, "gpg_bc", [128, D], F32)
                gple_bc = sb(st_, "gple_bc", [128, D], F32)
                gfin_bc = sb(st_, "gfin_bc", [128, D], F32)
                NX = 5
                xt = [sb(st_, "xtP%d" % i, [128, D], F32) for i in range(NX)]
                hn = [sb(st_, "hnP%d" % i, [128, D], BF16) for i in range(2)]
                hT = sb(st_, "hTP", [128, 8, 512], BF16)
                junk_t = sb(st_, "junkP", [128, D], BF16)
                sa = [sb(st_, "sa%d" % i, [128, 512], BF16) for i in range(2)]
                sbb = [sb(st_, "sbb%d" % i, [128, 512], BF16) for i in range(2)]
                t1 = [sb(st_, "t1_%d" % i, [128, 512], BF16) for i in range(2)]
                t2 = [sb(st_, "t2_%d" % i, [128, 512], BF16) for i in range(2)]
                mg = sb(st_, "mg", [128, 8, 512], BF16)
                x1n = sb(st_, "x1n", [128, D], BF16)
                x1nT = sb(st_, "x1nT", [128, 8, 128], BF16)
                pbf = [sb(st_, "pbf%d" % i, [128, 256], BF16) for i in range(2)]
                pT_ = sb(st_, "pTP", [128, 2, 128], BF16)
                sig = sb(st_, "sig", [128, 512], F32)
                et = sb(st_, "et", [128, D], F32)
                ot = [sb(st_, "ot%d" % i, [128, D], F32) for i in range(2)]
                stt_ = sb(st_, "stP", [128, 48], F32)
                ctx = dict(xt=xt, hn=hn, st=stt_, junk=junk_t[:], hT=hT, gin_bc=gin_bc)

                load_w("wml", wml[:], kchunks(w_mla))
                load_w("wglb", wgl_[:], kchunks(w_gla))
                load_w("wpl", wpl[:], kchunks(w_ple))
                S.dma("sync", gin_bc[:], g_in.partition_broadcast(128), [], ["gin_bc"], "gin_bc")
                S.dma("sync", gpg_bc[:], g_pg.partition_broadcast(128), [], ["gpg_bc"], "gpg_bc")
                S.dma("sync", gple_bc[:], g_ple.partition_broadcast(128), [], ["gple_bc"], "gple_bc")
                S.dma("sync", gfin_bc[:], g_fin.partition_broadcast(128), [], ["gfin_bc"], "gfin_bc")
                pj_i = [0]
                rot = [1, 2, 3]

                def pj():
                    b = rot[pj_i[0] % 3]
                    pj_i[0] += 1
                    return b

                HT = [("hT", c0) for c0 in (0, 128, 256, 384)]

                def stageP(gn, t, parts="LST"):
                    stage_hT(ctx, s * SEQ + gn * 512 + t * 128, (4 * gn + t) % NX, t * 128, "vector", parts=parts)

                for t in range(4):
                    stageP(0, t)
                for g in range(ng):
                    tok0 = s * SEQ + g * 512
                    c0g = g * 512
                    for m in range(8):
                        sl = m % 2
                        ba = pj()
                        for k in range(8):
                            S.mm(ps[ba][:], wP[:, k, m * 128:(m + 1) * 128], hT[:, k, :], k == 0, k == 7, ["wP"] + HT, [("ps", ba)])
                        S.act(sa[sl][:], ps[ba][:], AF.Sigmoid, [("ps", ba)], [("sa", sl)])
                        by = pj()
                        for c in range(4):
                            S.mm(ps[by][:], wml[:, c, m * 128:(m + 1) * 128], uaT[:, c, c0g:c0g + 512], c == 0, c == 3, ["wml"], [("ps", by)])
                        S.tt("vector", t1[sl][:], ps[by][:], sa[sl][:], ALU.mult, [("ps", by), ("sa", sl)], [("t1", sl)])
                        bb = pj()
                        for k in range(8):
                            S.mm(ps[bb][:], wP[:, k, 1024 + m * 128:1024 + (m + 1) * 128], hT[:, k, :], k == 0, k == 7, ["wP"] + HT, [("ps", bb)])
                        S.act(sbb[sl][:], ps[bb][:], AF.Sigmoid, [("ps", bb)], [("sbb", sl)])
                        bz = pj()
                        for c in range(4):
                            S.mm(ps[bz][:], wgl_[:, c, m * 128:(m + 1) * 128], ubT[:, c, c0g:c0g + 512], c == 0, c == 3, ["wglb"], [("ps", bz)])
                        S.tt("vector", t2[sl][:], ps[bz][:], sbb[sl][:], ALU.mult, [("ps", bz), ("sbb", sl)], [("t2", sl)])
                        S.tt("gpsimd", mg[:, m, :], t1[sl][:], t2[sl][:], ALU.add, [("t1", sl), ("t2", sl)], [("mg", m)])
                    MG = [("mg", m) for m in range(8)]

                    def xs_of(t):
                        return (4 * g + t) % NX

                    def A1(t):
                        xs = xs_of(t)
                        cs = slice(t * 128, (t + 1) * 128)
                        for half in range(2):
                            b = 4 + half
                            hs_ = slice(half * 512, (half + 1) * 512)
                            for m in range(8):
                                S.mm(ps[b][:], mg[:, m, cs], wo[:, m, hs_], m == 0, m == 7, ["wo"] + MG, [("ps", b)])
                            S.tt("vector", xt[xs][:, hs_], xt[xs][:, hs_], ps[b][:], ALU.add, [("xt", xs), ("ps", b)], [("xt", xs)])
                        S.act(junk_t[:], xt[xs][:], AF.Square, [("xt", xs)], ["junk", "s1"], accum=stt_[:, 24:25])
                        S.act(stt_[:, 25:26], stt_[:, 24:25], AF.Ln, ["s1"], ["l1"], scale=1.0 / D, bias=EPS)
                        S.act(stt_[:, 26:27], stt_[:, 25:26], AF.Exp, ["l1"], ["r1"], scale=-0.5)

                    def A2(t):
                        xs = xs_of(t)
                        S.stt("vector", x1n[:], xt[xs][:], stt_[:, 26:27], gpg_bc[:], ALU.mult, ALU.mult, [("xt", xs), "r1", "gpg_bc"], ["x1n"])

                    def B(t):
                        tok = tok0 + t * 128
                        ps_ = t % 2
                        S.dma("gpsimd", pbf[ps_][:], pin[tok:tok + 128, :], [], [("pbf", ps_)], ("pbf", ps_))
                        for c in range(2):
                            S.tr(tpv[:, c * 128:(c + 1) * 128], pbf[ps_][:, c * 128:(c + 1) * 128], ident[:], [("pbf", ps_), "ident"], [("ps", 0)])
                        S.copy("vector", pT_[:], tpv[:, 0:256].rearrange("p (c t) -> p c t", c=2), [("ps", 0)], ["pTP"])
                        for half in range(2):
                            b = 1 + half
                            for c in range(2):
                                S.mm(ps[b][:], pT_[:, c, :], wpl[:, c, half * 512:(half + 1) * 512], c == 0, c == 1, ["wpl", "pTP"], [("ps", b)])
                            S.act(junk_t[:, 0:512], ps[b][:], AF.Square, [("ps", b)], ["junk", ("se", half)], accum=stt_[:, 27 + half:28 + half])
                        S.tt("vector", stt_[:, 29:30], stt_[:, 27:28], stt_[:, 28:29], ALU.add, [("se", 0), ("se", 1)], ["se2"])
                        S.act(stt_[:, 30:31], stt_[:, 29:30], AF.Ln, ["se2"], ["le"], scale=1.0 / D, bias=EPS)
                        S.act(stt_[:, 31:32], stt_[:, 30:31], AF.Exp, ["le"], ["re"], scale=-0.5)
                        for half in range(2):
                            b = 1 + half
                            hs_ = slice(half * 512, (half + 1) * 512)
                            S.stt("vector", et[:, hs_], ps[b][:], stt_[:, 31:32], gple_bc[:, hs_], ALU.mult, ALU.mult,
                                  [("ps", b), "re", "gple_bc"], [("et", half)])

                    def C1(t):
                        for c in range(8):
                            S.tr(tpv[:, c * 128:(c + 1) * 128], x1n[:, c * 128:(c + 1) * 128], ident[:], ["x1n", "ident"], [("ps", 0)])
                        S.copy("scalar", x1nT[:], tpv.rearrange("p (c t) -> p c t", c=8), [("ps", 0)], ["x1nT"])

                    def C2a(t):
                        for half in range(2):
                            b = 6 + half
                            hs_ = slice(half * 512, (half + 1) * 512)
                            for c in range(8):
                                S.mm(ps[b][:], x1nT[:, c, :], wpg[:, c, hs_], c == 0, c == 7, ["wpg", "x1nT"], [("ps", b)])

                    def C2b(t):
                        tok = tok0 + t * 128
                        xs = xs_of(t)
                        osl = t % 2
                        for half in range(2):
                            b = 6 + half
                            hs_ = slice(half * 512, (half + 1) * 512)
                            S.act(sig[:], ps[b][:], AF.Sigmoid, [("ps", b)], ["sig"])
                            S.tt("vector", et[:, hs_], et[:, hs_], sig[:], ALU.mult, [("et", half), "sig"], [("et", half)])
                        S.tt("vector", xt[xs][:], xt[xs][:], et[:], ALU.add, [("xt", xs), ("et", 0), ("et", 1)], [("xt", xs)])
                        S.act(junk_t[:], xt[xs][:], AF.Square, [("xt", xs)], ["junk", "s2"], accum=stt_[:, 32:33])
                        S.act(stt_[:, 33:34], stt_[:, 32:33], AF.Ln, ["s2"], ["l2"], scale=1.0 / D, bias=EPS)
                        S.act(stt_[:, 34:35], stt_[:, 33:34], AF.Exp, ["l2"], ["r2"], scale=-0.5)
                        S.stt("vector", ot[osl][:], xt[xs][:], stt_[:, 34:35], gfin_bc[:], ALU.mult, ALU.mult,
                              [("xt", xs), "r2", "gfin_bc"], [("ot", osl)])
                        S.dma("sync", out[tok:tok + 128, :], ot[osl][:], [("ot", osl)], [("out", tok)], ("ot", osl))

                    nxt = g + 1 < ng
                    A1(0)
                    A2(0)
                    for t in range(4):
                        if nxt and t >= 1:
                            stageP(g + 1, t - 1, "L")
                        C1(t)
                        if t + 1 < 4:
                            A1(t + 1)
                            A2(t + 1)
                        B(t)
                        if nxt and t >= 1:
                            stageP(g + 1, t - 1, "S")
                        C2a(t)
                        if nxt and t >= 1:
                            stageP(g + 1, t - 1, "T")
                        C2b(t)
                    if nxt:
                        stageP(g + 1, 3, "LST")
                S.flush()

        for s in range(nseq):
            if "M" in passes:
                pass_M(s)
            with contextlib.ExitStack() as pw:
                pre = (sb(pw, "wP", [128, 8, 2048], BF16), sb(pw, "wo", [128, 8, D], BF16), sb(pw, "wpg", [128, 8, D], BF16))
                if "G" in passes:
                    pass_G(s, pre)
                if "P" in passes:
                    pass_P(s, pre)
        if S.ops:
            S.flush()
    return nc


def _consts():
    c = np.zeros((128, C_END), np.float32)
    idx = np.arange(128)
    c[:, C_ID:C_ID + 128] = np.eye(128, dtype=np.float32)
    triu = (idx[None, :] >= idx[:, None]).astype(np.float32)
    c[:, C_TRI:C_TRI + 128] = triu
    c[:, C_L:C_L + 128] = -triu / 16.0
    c[:, C_U:C_U + 128] = -(idx[:, None] > idx[None, :]).astype(np.float32) / 16.0
    c[64, C_SEL:C_SEL + 64] = 1.0
    c[0, C_SEL + 64:C_SEL + 128] = 1.0
    inv_freq = 1.0 / (10000.0 ** (np.arange(0, 32, 2, dtype=np.float64) / 32.0))
    for r in range(32):
        c[64 + r, C_ROPE] = inv_freq[r % 16] / (2.0 * np.pi)
        c[64 + r, C_ROPE + 1] = -TWO_PI if r < 16 else TWO_PI
    return c


_NC_CACHE = {}


def _prep_shared(inp):
    f = lambda a: np.ascontiguousarray(np.asarray(a, dtype=np.float32))
    w_in = f(inp["w_in"][0])
    kr = w_in[:, 640:672]
    w_krz = np.zeros((D, 192), np.float32)
    w_krz[:, 64:96] = kr
    w_krz[:, 96 + 64:96 + 80] = kr[:, 16:32]
    w_krz[:, 96 + 80:96 + 96] = kr[:, 0:16]
    w_uq = f(inp["w_uq"][0])
    w_uqsw = np.zeros((384, 768), np.float32)
    for h in range(8):
        rp = w_uq[:, 96 * h + 64:96 * h + 96]
        w_uqsw[:, 96 * h + 64:96 * h + 80] = rp[:, 16:32]
        w_uqsw[:, 96 * h + 80:96 * h + 96] = rp[:, 0:16]
    w_ukv = f(inp["w_ukv"][0]).reshape(256, 8, 2, 64)
    w_ukvk = np.ascontiguousarray(w_ukv[:, :, 0, :].reshape(256, 512))
    w_ukvv = np.ascontiguousarray(w_ukv[:, :, 1, :].reshape(256, 512))
    w_gk17 = np.concatenate([f(inp["w_gk_up"][0]), f(inp["b_gk"][0]).reshape(1, 256)], axis=0)
    gcols = np.zeros((128, 8), np.float32)
    gcols[:, 0:3] = f(inp["q_norm_g"][0]).reshape(3, 128).T
    gcols[:, 3:5] = f(inp["kv_norm_g"][0]).reshape(2, 128).T
    return {
        "w_in": w_in, "w_krz": w_krz, "w_uq": w_uq, "w_uqsw": w_uqsw, "w_ukvk": w_ukvk, "w_ukvv": w_ukvv,
        "w_gk17": np.ascontiguousarray(w_gk17), "w_mla": f(inp["w_mla_br"][0]), "w_gla": f(inp["w_gla_br"][0]),
        "w_out": f(inp["w_out"][0]), "w_ple": f(inp["w_ple"][0]), "w_pg": f(inp["w_ple_gate"][0]),
        "g_in": f(inp["norm_in_g"][0]).reshape(1, D), "g_gla": f(inp["gla_norm_g"][0]).reshape(1, 128),
        "g_ple": f(inp["ple_norm_g"][0]).reshape(1, D), "g_pg": f(inp["ple_gate_norm_g"][0]).reshape(1, D),
        "g_fin": f(inp["final_norm_g"]).reshape(1, D), "gcols": gcols, "consts": _consts(),
    }


def kernel(**inputs):
    if "nc" not in _NC_CACHE:
        _NC_CACHE["nc"] = build(False)
    nc = _NC_CACHE["nc"]
    shared = _prep_shared(inputs)
    x = np.asarray(inputs["x"], dtype=np.float32)
    p = np.asarray(inputs["p"], dtype=np.float32)[0]
    pos = np.asarray(inputs["positions"], dtype=np.int32)
    in_maps = []
    for c in range(NCORES):
        m = dict(shared)
        m["x"] = np.ascontiguousarray(x[2 * c:2 * c + 2].reshape(2 * SEQ, D))
        m["p"] = np.ascontiguousarray(p[2 * c:2 * c + 2].reshape(2 * SEQ, 256))
        m["pos"] = np.ascontiguousarray(pos[2 * c:2 * c + 2].reshape(1, 2 * SEQ))
        in_maps.append(m)
    res = run_bass_kernel_spmd(nc, in_maps, core_ids=list(range(NCORES)))
    outs = [np.asarray(r["out"]).reshape(2, SEQ, D) for r in res.results]
    return np.concatenate(outs, axis=0).astype(np.float32)
```

```python
import contextlib
import numpy as np
import concourse.bass as bass
import concourse.mybir as mybir
from concourse.bass_utils import run_bass_kernel_spmd

F32 = mybir.dt.float32
BF16 = mybir.dt.bfloat16
I32 = mybir.dt.int32
AF = mybir.ActivationFunctionType
ALU = mybir.AluOpType

NCORES = 8
SEQ = 2048
D = 1024
EPS = 1e-6
TWO_PI = float(2.0 * np.pi * (1.0 - 2e-7))
ENGINES = ("sync", "scalar", "vector", "gpsimd", "tensor")

C_ID, C_TRI, C_L, C_U, C_SEL, C_ROPE, C_END = 0, 128, 256, 384, 512, 640, 644


class _Op:
    __slots__ = ("eng", "fn", "dma", "waits", "inc", "idx", "key", "pos")


class Sched:
    def __init__(self, nc, stack):
        self.nc = nc
        self.stack = stack
        self.sems = {}
        self.counts = {}
        self.seen = {e: {} for e in ENGINES}
        self._reset()

    def _reset(self):
        self.ops = []
        self.last_w = {}
        self.readers = {}

    def add(self, eng, fn, R=(), W=(), dma=False, stream=None):
        R = tuple(R) + tuple((r[0], r[1], h) for r in R if r == ("scr", 0) for h in ("a", "b"))
        W = tuple(W) + tuple((r[0], r[1], h) for r in W if r == ("scr", 0) for h in ("a", "b"))
        op = _Op()
        op.eng, op.fn, op.dma, op.inc, op.idx = eng, fn, dma, False, 0
        op.key = ("dma", stream) if dma else ("eng", eng)
        op.pos = len(self.ops)
        best = {}
        W = tuple(W) + tuple(r for r in R if isinstance(r, tuple) and r[0] == "ps" and r not in W)

        def dep(d, kind):
            if d is None or d is op:
                return
            if (not d.dma) and d.eng == eng and not dma:
                if eng == "tensor":
                    return
            cur = best.get(d.key)
            if cur is None or d.pos > cur.pos:
                best[d.key] = d

        for r in R:
            dep(self.last_w.get(r), "raw")
        for r in W:
            dep(self.last_w.get(r), "waw")
            for rd in self.readers.get(r, ()):
                dep(rd, "war")
        op.waits = list(best.values())
        for d in op.waits:
            d.inc = True
        for r in W:
            self.last_w[r] = op
            self.readers[r] = []
        for r in R:
            lst = self.readers.setdefault(r, [])
            for i, o in enumerate(lst):
                if o.key == op.key:
                    lst[i] = op
                    break
            else:
                lst.append(op)
        self.ops.append(op)
        return op

    def flush(self):
        nc = self.nc
        per_eng = {e: [] for e in ENGINES}
        for op in self.ops:
            per_eng[op.eng].append(op)
        for e in ENGINES:
            for op in reversed(per_eng[e]):
                if not op.dma:
                    op.inc = True
                    break
        for op in self.ops:
            if op.dma:
                op.inc = True
        for op in self.ops:
            if op.inc:
                k = op.key
                if k not in self.sems:
                    self.sems[k] = self.stack.enter_context(nc.semaphore("s%d" % len(self.sems)))
                    self.counts[k] = 0
                self.counts[k] += 16 if op.dma else 1
                op.idx = self.counts[k]
        finals = dict(self.counts)
        sems, seen_all = self.sems, self.seen

        def make(engname):
            def body(eng):
                seen = seen_all[engname]
                for op in per_eng[engname]:
                    for d in op.waits:
                        if seen.get(d.key, 0) < d.idx:
                            eng.wait_ge(sems[d.key], d.idx)
                            seen[d.key] = d.idx
                    ins = op.fn(eng)
                    if op.inc:
                        ins.then_inc(sems[op.key], 16 if op.dma else 1)
                for k, v in finals.items():
                    if seen.get(k, 0) < v:
                        eng.wait_ge(sems[k], v)
                        seen[k] = v
            return body

        with nc.Block() as block:
            block.sync(make("sync"))
            block.scalar(make("scalar"))
            block.vector(make("vector"))
            block.gpsimd(make("gpsimd"))
            block.tensor(make("tensor"))
        self._reset()

    def dma(self, eng, out, in_, R, W, stream):
        return self.add(eng, lambda e: e.dma_start(out=out, in_=in_), R, W, dma=True, stream=stream)

    def mm(self, out, lhsT, rhs, start, stop, R, W, tp=None):
        if tp is not None:
            return self.add("tensor", lambda e: e.matmul(out, lhsT=lhsT, rhs=rhs, start=start, stop=stop, tile_position=tp), R, W)
        return self.add("tensor", lambda e: e.matmul(out, lhsT=lhsT, rhs=rhs, start=start, stop=stop), R, W)

    def tr(self, out, in_, ident, R, W):
        return self.add("tensor", lambda e: e.transpose(out=out, in_=in_, identity=ident), R, W)

    def act(self, out, in_, func, R, W, scale=None, bias=None, accum=None):
        kw = {}
        if scale is not None:
            kw["scale"] = scale
        if bias is not None:
            kw["bias"] = bias
        if accum is not None:
            kw["accum_out"] = accum
        return self.add("scalar", lambda e: e.activation(out=out, in_=in_, func=func, **kw), R, W)

    def copy(self, eng, out, in_, R, W):
        if eng == "scalar":
            return self.add(eng, lambda e: e.copy(out=out, in_=in_), R, W)
        return self.add(eng, lambda e: e.tensor_copy(out=out, in_=in_), R, W)

    def tt(self, eng, out, in0, in1, op, R, W):
        return self.add(eng, lambda e: e.tensor_tensor(out=out, in0=in0, in1=in1, op=op), R, W)

    def ts(self, eng, out, in0, s1, op0, R, W, s2=None, op1=None):
        if op1 is None:
            return self.add(eng, lambda e: e.tensor_scalar(out=out, in0=in0, scalar1=s1, scalar2=None, op0=op0), R, W)
        return self.add(eng, lambda e: e.tensor_scalar(out=out, in0=in0, scalar1=s1, scalar2=s2, op0=op0, op1=op1), R, W)

    def stt(self, eng, out, in0, scalar, in1, op0, op1, R, W):
        return self.add(eng, lambda e: e.scalar_tensor_tensor(out=out, in0=in0, scalar=scalar, in1=in1, op0=op0, op1=op1), R, W)

    def memset(self, eng, ap, val, W):
        return self.add(eng, lambda e: e.memset(ap, val), (), W)

    def recip(self, out, in_, R, W):
        return self.add("vector", lambda e: e.reciprocal(out=out, in_=in_), R, W)


def build(debug=False, nseq=2, ng=4, passes="MGP", cut=99):
    nc = bass.Bass("TRN2", target_bir_lowering=False)

    def din(name, shape, dt=F32):
        return nc.dram_tensor(name, list(shape), dt, kind="ExternalInput").ap()

    x = din("x", [2 * SEQ, D])
    pin = din("p", [2 * SEQ, 256])
    pos = din("pos", [1, 2 * SEQ], I32)
    w_in = din("w_in", [D, 4784])
    w_krz = din("w_krz", [D, 192])
    w_uq = din("w_uq", [384, 768])
    w_uqsw = din("w_uqsw", [384, 768])
    w_ukvk = din("w_ukvk", [256, 512])
    w_ukvv = din("w_ukvv", [256, 512])
    w_gk17 = din("w_gk17", [17, 256])
    w_mla = din("w_mla", [512, D])
    w_gla = din("w_gla", [512, D])
    w_out = din("w_out", [D, D])
    w_ple = din("w_ple", [256, D])
    w_pg = din("w_pg", [D, D])
    g_in = din("g_in", [1, D])
    g_gla = din("g_gla", [1, 128])
    g_ple = din("g_ple", [1, D])
    g_pg = din("g_pg", [1, D])
    g_fin = din("g_fin", [1, D])
    gcols = din("gcols", [128, 8])
    consts = din("consts", [128, C_END])
    out = nc.dram_tensor("out", [2 * SEQ, D], F32, kind="ExternalOutput").ap()
    if debug:
        dbg_ua = nc.dram_tensor("dbg_ua", [2, 128, 4, SEQ], BF16, kind="ExternalOutput").ap()
        dbg_ub = nc.dram_tensor("dbg_ub", [2, 128, 4, SEQ], BF16, kind="ExternalOutput").ap()

    top = contextlib.ExitStack()
    with top:
        uid = [0]

        def sb(stack, name, shape, dt):
            uid[0] += 1
            return stack.enter_context(nc.sbuf_tensor("%s_%d" % (name, uid[0]), list(shape), dt))

        S = Sched(nc, top)
        cst = sb(top, "cst", [128, C_END], F32)
        gc = sb(top, "gc", [128, 8], F32)
        ident = sb(top, "ident", [128, 128], BF16)
        tri = sb(top, "tri", [128, 4, 128], BF16)
        ones_bf = sb(top, "ones_bf", [128, 128], BF16)
        uaT = sb(top, "uaT", [128, 4, SEQ], BF16)
        ubT = sb(top, "ubT", [128, 4, SEQ], BF16)
        ps = [top.enter_context(nc.psum_tensor("ps%d" % b, [128, 512], F32)) for b in range(8)]
        tpv = ps[0][:].bitcast(BF16)

        S.dma("sync", cst[:], consts, [], ["cst"], "cst")
        S.dma("sync", gc[:], gcols, [], ["gc"], "gc")
        S.copy("vector", ident[:], cst[:, C_ID:C_ID + 128], ["cst"], ["ident"])
        for h in range(4):
            S.copy("vector", tri[:, h, :], cst[:, C_TRI:C_TRI + 128], ["cst"], ["tri"])
        S.memset("vector", ones_bf[:], 1.0, ["ones_bf"])
        Lmat = cst[:, C_L:C_L + 128]
        Umat = cst[:, C_U:C_U + 128]

        def stage_hT(ctx, tok0, xs, col0, evac_eng, hT=None, htag=("hT",), parts="LST"):
            xt, hn, st, junk, gbc = ctx["xt"], ctx["hn"], ctx["st"], ctx["junk"], ctx["gin_bc"]
            if hT is None:
                hT = ctx["hT"]
            hs = xs % len(hn)
            if "L" in parts:
                S.dma("sync", xt[xs][:], x[tok0:tok0 + 128, :], [], [("xt", xs)], ("xt", xs))
            if "S" in parts:
                S.act(junk, xt[xs][:], AF.Square, [("xt", xs)], ["junk", ("ss", xs)], accum=st[:, xs:xs + 1])
                S.act(st[:, 8 + xs:9 + xs], st[:, xs:xs + 1], AF.Ln, [("ss", xs)], [("ln", xs)], scale=1.0 / D, bias=EPS)
                S.act(st[:, 16 + xs:17 + xs], st[:, 8 + xs:9 + xs], AF.Exp, [("ln", xs)], [("rs", xs)], scale=-0.5)
                S.stt("vector", hn[hs][:], xt[xs][:], st[:, 16 + xs:17 + xs], gbc[:], ALU.mult, ALU.mult,
                      [("xt", xs), ("rs", xs), "gin_bc"], [("hn", hs)])
            if "T" in parts:
                for c in range(8):
                    S.tr(tpv[:, c * 128:(c + 1) * 128], hn[hs][:, c * 128:(c + 1) * 128], ident[:],
                         [("hn", hs), "ident"], [("ps", 0)])
                S.copy(evac_eng, hT[:, :, col0:col0 + 128], tpv.rearrange("p (c t) -> p c t", c=8),
                       [("ps", 0)], [htag + (col0,)])

        def load_w(name, dst, src, R=()):
            S.dma("gpsimd", dst, src, list(R), [name], name)

        def kchunks(w):
            return w.rearrange("(c p) n -> p c n", p=128)

        def pass_M(s):
            with contextlib.ExitStack() as st_:
                wM = sb(st_, "wM", [128, 8, 1152], BF16)
                wkr = sb(st_, "wkr", [128, 8, 192], BF16)
                wuq = sb(st_, "wuq", [128, 3, 768], BF16)
                wuqs = sb(st_, "wuqs", [128, 3, 768], BF16)
                wkk = sb(st_, "wkk", [128, 2, 512], BF16)
                wkv = sb(st_, "wkv", [128, 2, 512], BF16)
                kT = sb(st_, "kT", [128, 8, SEQ], BF16)
                Ve = sb(st_, "Ve", [128, 16, 4, 128], BF16)
                Vo = sb(st_, "Vo", [128, 16, 4, 128], BF16)
                gin_bc = sb(st_, "gin_bc", [128, D], F32)
                xt = [sb(st_, "xt%d" % i, [128, D], F32) for i in range(2)]
                hn = [sb(st_, "hn%d" % i, [128, D], BF16) for i in range(2)]
                hTs = [sb(st_, "hT%d" % i, [128, 8, 512], BF16) for i in range(2)]
                scr = sb(st_, "scr", [128, 3, 512], F32)
                sq = sb(st_, "sq", [128, 3, 512], BF16)
                rq = sb(st_, "rq", [128, 512], F32)
                cqn = sb(st_, "cqn", [128, 3, 512], BF16)
                ckvn = sb(st_, "ckvn", [128, 2, 512], BF16)
                sg = sb(st_, "sg", [128, 4, 512], BF16)
                qT = sb(st_, "qT", [128, 8, 512], BF16)
                posi = sb(st_, "posi", [128, 512], I32)
                cos2 = sb(st_, "cos2", [128, 512], F32)
                sin2 = sb(st_, "sin2", [128, 512], F32)
                pT = [sb(st_, "pT%d" % i, [128, 512], BF16) for i in range(4)]
                stt_ = sb(st_, "stM", [128, 24], F32)
                junk = sq[:, 0:2, :].rearrange("p a b -> p (a b)")
                ctx = dict(xt=xt, hn=hn, st=stt_, junk=junk, hT=None, gin_bc=gin_bc)

                def stageM(gn, t):
                    stage_hT(ctx, s * SEQ + gn * 512 + t * 128, t % 2, t * 128, "scalar" if t % 2 else "vector",
                             hT=hTs[gn % 2], htag=("hT", gn % 2))

                wi = kchunks(w_in)
                load_w("wM_a", wM[:, :, 0:640], wi[:, :, 0:640])
                load_w("wM_b", wM[:, :, 640:1152], wi[:, :, 672:1184])
                load_w("wkr", wkr[:], kchunks(w_krz))
                load_w("wuq", wuq[:], kchunks(w_uq))
                load_w("wuqs", wuqs[:], kchunks(w_uqsw))
                load_w("wkk", wkk[:], kchunks(w_ukvk))
                load_w("wkv", wkv[:], kchunks(w_ukvv))
                S.dma("sync", gin_bc[:], g_in.partition_broadcast(128), [], ["gin_bc"], "gin_bc")
                S.memset("vector", Ve[:, :, :, 64:128], 1.0, ["Vones"])
                S.memset("vector", Vo[:, :, :, 0:64], 1.0, ["Vones"])
                WM = ["wM_a", "wM_b"]
                SCALE = float(96 ** -0.5)
                for t in range(4):
                    stageM(0, t)
                r64 = slice(64, 96)
                pj_rot = [1, 2, 3, 4, 7]
                pj_i = [0]

                def pj():
                    b = pj_rot[pj_i[0] % 5]
                    pj_i[0] += 1
                    return b

                for g in range(ng):
                    tok0 = s * SEQ + g * 512
                    c0g = g * 512
                    hT = hTs[g % 2]
                    HT = [("hT", g % 2, c0) for c0 in (0, 128, 256, 384)]
                    S.dma("sync", posi[r64, :], pos[0:1, tok0:tok0 + 512].partition_broadcast(32), [], ["posi"], "posi")
                    S.copy("vector", scr[r64, 0, :], posi[r64, :], ["posi"], [("scr", 0)])
                    S.ts("vector", scr[r64, 0, :], scr[r64, 0, :], cst[r64, C_ROPE:C_ROPE + 1], ALU.mult,
                         [("scr", 0), "cst"], [("scr", 0)])
                    S.copy("vector", posi[r64, :], scr[r64, 0, :], [("scr", 0)], ["posi"])
                    S.copy("vector", scr[r64, 1, :], posi[r64, :], ["posi"], [("scr", 1)])
                    S.tt("vector", scr[r64, 1, :], scr[r64, 0, :], scr[r64, 1, :], ALU.subtract,
                         [("scr", 0), ("scr", 1)], [("scr", 1)])
                    S.act(sin2[r64, :], scr[r64, 1, :], AF.Sin, [("scr", 1), "cst"], ["sin2"],
                          scale=cst[r64, C_ROPE + 1:C_ROPE + 2])
                    S.ts("vector", scr[r64, 2, :], scr[r64, 0, :], 0.25, ALU.add, [("scr", 0)], [("scr", 2)])
                    S.copy("vector", posi[r64, :], scr[r64, 2, :], [("scr", 2)], ["posi"])
                    S.copy("vector", scr[r64, 1, :], posi[r64, :], ["posi"], [("scr", 1)])
                    S.tt("vector", scr[r64, 1, :], scr[r64, 2, :], scr[r64, 1, :], ALU.subtract,
                         [("scr", 2), ("scr", 1)], [("scr", 1)])
                    S.act(cos2[r64, :], scr[r64, 1, :], AF.Sin, [("scr", 1)], ["cos2"], scale=TWO_PI)


                    def proj(col, width=128, w=wM, wn=WM, m0=0):
                        b = pj()
                        for k in range(8):
                            S.mm(ps[b][m0:m0 + width, :], w[:, k, col:col + width], hT[:, k, :],
                                 k == 0, k == 7, wn + HT, [("ps", b)])
                        return b

                    def lowrank_norm(col_base, nch, gcol0, dst, dname, inv_n):
                        for c in range(nch):
                            b = proj(col_base + c * 128)
                            S.act(sq[:, c, :], ps[b][:], AF.Square, [("ps", b)], [("sq", c)])
                            S.copy("vector", scr[:, c, :], ps[b][:], [("ps", b)], [("scr", c)])
                        b = pj()
                        for c in range(nch):
                            S.mm(ps[b][:], ones_bf[:], sq[:, c, :], c == 0, c == nch - 1,
                                 ["ones_bf", ("sq", c)], [("ps", b)])
                        if cut == 24:
                            return
                        S.act(rq[:], ps[b][:], AF.Ln, [("ps", b)], ["rq"], scale=inv_n, bias=EPS)
                        if cut == 25:
                            return
                        S.act(rq[:], rq[:], AF.Exp, ["rq"], ["rq"], scale=-0.5)
                        if cut == 26:
                            return
                        for c in range(nch):
                            S.stt("vector", dst[:, c, :], scr[:, c, :], gc[:, gcol0 + c:gcol0 + c + 1], rq[:],
                                  ALU.mult, ALU.mult, [("scr", c), "gc", "rq"], [(dname, c)])

                    if cut == 20:
                        b = proj(0)
                        S.flush()
                        return
                    if cut == 21:
                        b = proj(0)
                        S.act(sq[:, 0, :], ps[b][:], AF.Square, [("ps", b)], [("sq", 0)])
                        S.flush()
                        return
                    if cut == 22:
                        b = proj(0)
                        S.copy("vector", scr[:, 0, :], ps[b][:], [("ps", b)], [("scr", 0)])
                        S.flush()
                        return
                    lowrank_norm(0, 3, 0, cqn, "cqn", 1.0 / 384)
                    CQN = [("cqn", c) for c in range(3)]
                    if cut in (23, 24, 25, 26):
                        S.flush()
                        return
                    lowrank_norm(384, 2, 3, ckvn, "ckvn", 1.0 / 256)
                    CKV = [("ckvn", c) for c in range(2)]
                    for c in range(4):
                        b = proj(640 + c * 128)
                        S.act(sg[:, c, :], ps[b][:], AF.Silu, [("ps", b)], [("sg", c)])
                    if cut == 3:
                        S.flush()
                        return
                    ba = proj(0, 96, wkr, ["wkr"])
                    bb = proj(96, 96, wkr, ["wkr"])
                    S.tt("vector", scr[r64, 0, :], ps[ba][r64, :], cos2[r64, :], ALU.mult, [("ps", ba), "cos2"], [("scr", 0)])
                    S.tt("vector", scr[r64, 1, :], ps[bb][r64, :], sin2[r64, :], ALU.mult, [("ps", bb), "sin2"], [("scr", 1)])
                    S.tt("vector", kT[r64, 0, c0g:c0g + 512], scr[r64, 0, :], scr[r64, 1, :], ALU.add,
                         [("scr", 0), ("scr", 1)], [("kTr", 0, g)])
                    for h in range(1, 8):
                        S.copy("vector", kT[r64, h, c0g:c0g + 512], kT[r64, 0, c0g:c0g + 512],
                               [("kTr", 0, g)], [("kTr", h, g)])
                    if cut == 4:
                        S.flush()
                        return
                    for h in range(8):
                        ba, bb = pj(), pj()
                        for c in range(3):
                            S.mm(ps[ba][0:96, :], wuq[:, c, 96 * h:96 * h + 96], cqn[:, c, :], c == 0, c == 2,
                                 ["wuq"] + CQN, [("ps", ba)])
                        for c in range(3):
                            S.mm(ps[bb][0:96, :], wuqs[:, c, 96 * h:96 * h + 96], cqn[:, c, :], c == 0, c == 2,
                                 ["wuqs"] + CQN, [("ps", bb)])
                        S.copy("scalar", qT[0:64, h, :], ps[ba][0:64, :], [("ps", ba)], [("qTn", h)])
                        S.tt("vector", scr[r64, 0, :], ps[ba][r64, :], cos2[r64, :], ALU.mult, [("ps", ba), "cos2"], [("scr", 0)])
                        S.tt("vector", scr[r64, 1, :], ps[bb][r64, :], sin2[r64, :], ALU.mult, [("ps", bb), "sin2"], [("scr", 1)])
                        S.tt("vector", qT[r64, h, :], scr[r64, 0, :], scr[r64, 1, :], ALU.add,
                             [("scr", 0), ("scr", 1)], [("qTr", h)])
                    for h in range(8):
                        b = pj()
                        for c in range(2):
                            S.mm(ps[b][0:64, :], wkk[:, c, 64 * h:64 * h + 64], ckvn[:, c, :], c == 0, c == 1,
                                 ["wkk"] + CKV, [("ps", b)])
                        S.copy("scalar" if h % 2 else "vector", kT[0:64, h, c0g:c0g + 512], ps[b][0:64, :],
                               [("ps", b)], [("kTn", h, g)])
                    for t in range(4):
                        T = g * 4 + t
                        b = pj()
                        for c in range(2):
                            S.mm(ps[b][:], ckvn[:, c, t * 128:(t + 1) * 128], wkv[:, c, :], c == 0, c == 1,
                                 ["wkv"] + CKV, [("ps", b)])
                        pv = ps[b][:].rearrange("p (i two d) -> p i two d", two=2, d=64)
                        S.copy("vector", Ve[:, T, :, 0:64], pv[:, :, 0, :], [("ps", b)], [("Ve", T)])
                        S.copy("scalar", Vo[:, T, :, 64:128], pv[:, :, 1, :], [("ps", b)], [("Vo", T)])

                    if cut == 5:
                        S.flush()
                        return
                    nk = 4 * (g + 1)
                    for i in range(4):
                        hA, hB = 2 * i, 2 * i + 1
                        if g + 1 < ng:
                            stageM(g + 1, i)
                        steps = [(j, hh) for j in range(nk) for hh in (0, 1)]
                        pend = None
                        for n, (j, hh) in enumerate(steps):
                            h = hA if hh == 0 else hB
                            r = j - 4 * g
                            c0 = 128 * r if r > 0 else 0
                            gj = j // 4
                            b = pj()
                            slot = n % 4
                            S.mm(ps[b][:, c0:512], kT[0:96, h, j * 128:(j + 1) * 128], qT[0:96, h, c0:512], True, True,
                                 [("kTn", h, gj), ("kTr", h, gj), ("qTn", h), ("qTr", h)], [("ps", b)])
                            S.act(pT[slot][:, c0:512], ps[b][:, c0:512], AF.Exp, [("ps", b)], [("pT", slot)], scale=SCALE)
                            if r >= 0:
                                S.tt("vector", pT[slot][:, c0:c0 + 128], pT[slot][:, c0:c0 + 128], tri[:, 0, :], ALU.mult,
                                     [("pT", slot), "tri"], [("pT", slot)])
                            if pend is not None:
                                pend()
                            ob = 5 + hh
                            if hh == 0:
                                lhsT = Ve[:, j, i, :]
                                orow = slice(0, 128)
                                vr = [("Ve", j), "Vones"]
                            else:
                                lhsT = Vo[:, j, i, :]
                                orow = slice(0, 128)
                                vr = [("Vo", j), "Vones"]

                            def pend(ob=ob, orow=orow, c0=c0, lhsT=lhsT, slot=slot, j=j, vr=vr):
                                S.mm(ps[ob][orow, c0:512], lhsT, pT[slot][:, c0:512], j == 0, j == nk - 1,
                                     vr + [("pT", slot)], [("ps", ob)])
                        pend()
                        if cut == 6:
                            S.flush()
                            return
                        S.act(scr[64:128, 0, :], ps[5][64:128, :], AF.Ln, [("ps", 5)], [("scr", 0, "a")])
                        S.act(scr[64:128, 0, :], scr[64:128, 0, :], AF.Exp, [("scr", 0, "a")], [("scr", 0, "a")], scale=-1.0)
                        S.act(scr[0:64, 0, :], ps[6][0:64, :], AF.Ln, [("ps", 6)], [("scr", 0, "b")])
                        S.act(scr[0:64, 0, :], scr[0:64, 0, :], AF.Exp, [("scr", 0, "b")], [("scr", 0, "b")], scale=-1.0)
                        S.copy("vector", scr[0:64, 2, :], scr[64:128, 0, :], [("scr", 0, "a")], [("scr", 2)])
                        S.copy("vector", scr[64:128, 2, :], scr[0:64, 0, :], [("scr", 0, "b")], [("scr", 2)])
                        S.tt("vector", scr[:, 1, :], scr[:, 2, :], sg[:, i, :], ALU.mult, [("scr", 2), ("sg", i)], [("scr", 1)])
                        S.tt("vector", uaT[0:64, i, c0g:c0g + 512], ps[5][0:64, :], scr[0:64, 1, :], ALU.mult,
                             [("ps", 5), ("scr", 1)], [("uaT", i, g)])
                        S.tt("vector", uaT[64:128, i, c0g:c0g + 512], ps[6][64:128, :], scr[64:128, 1, :], ALU.mult,
                             [("ps", 6), ("scr", 1)], [("uaT", i, g)])
                if debug:
                    S.dma("sync", dbg_ua[s][:, :, 0:ng * 512], uaT[:, :, 0:ng * 512], [("uaT", i, g) for i in range(4) for g in range(ng)], ["dbg_ua"], "dbg")
                S.flush()

        def pass_G(s, pre):
            with contextlib.ExitStack() as st_:
                wG = sb(st_, "wG", [128, 8, 1536], BF16)
                wgl = sb(st_, "wgl", [128, 8, 16], BF16)
                wg17 = sb(st_, "wg17", [128, 256], BF16)
                gin_bc = sb(st_, "gin_bcG", [128, D], F32)
                ggla_bc = sb(st_, "ggla_bc", [128, 128], F32)
                xt = [sb(st_, "xtG%d" % i, [128, D], F32) for i in range(2)]
                hn = [sb(st_, "hnG%d" % i, [128, D], BF16) for i in range(2)]
                hTs = [sb(st_, "hTG%d" % i, [128, 8, 512], BF16) for i in range(2)]
                junk_t = sb(st_, "junkG", [128, D], BF16)
                gqf = sb(st_, "gqf", [128, 4, 512], F32)
                gkf = sb(st_, "gkf", [128, 4, 512], F32)
                sgg = sb(st_, "sgg", [128, 4, 512], BF16)
                gkl = sb(st_, "gkl", [128, 512], BF16)
                vsb = [sb(st_, "vsb%d" % i, [128, 512], BF16) for i in range(2)]
                etmp = sb(st_, "etmp", [128, 256], F32)
                sp = sb(st_, "sp", [128, 256], F32)
                ebT = sb(st_, "ebT", [128, 4, 128], F32)
                enbT = sb(st_, "enbT", [128, 4, 128], F32)
                erev = sb(st_, "erev", [128, 256], F32)
                qtT = sb(st_, "qtT", [128, 4, 128], BF16)
                ktT = sb(st_, "ktT", [128, 4, 128], BF16)
                kdec = sb(st_, "kdec", [128, 256], BF16)
                AT = sb(st_, "AT", [128, 4, 128], BF16)
                onsb = sb(st_, "onsb", [128, 512], BF16)
                Sf = sb(st_, "Sf", [128, 4, 128], F32)
                Sb = sb(st_, "Sb", [128, 4, 128], BF16)
                junk2 = sb(st_, "junk2", [128, 128], BF16)
                stt_ = sb(st_, "stG", [128, 40], F32)
                ctx = dict(xt=xt, hn=hn, st=stt_, junk=junk_t[:], hT=None, gin_bc=gin_bc)
                r0_ = slice(0, 64)

                def stageG(gn, t, parts="LST"):
                    stage_hT(ctx, s * SEQ + gn * 512 + t * 128, t % 2, t * 128, "scalar" if t % 2 else "vector",
                             hT=hTs[gn % 2], htag=("hT", gn % 2), parts=parts)

                wi = kchunks(w_in)
                load_w("wG_a", wG[:, :, 0:1024], wi[:, :, 1184:2208])
                load_w("wG_b", wG[:, :, 1024:1536], wi[:, :, 2224:2736])
                load_w("wgl", wgl[:], wi[:, :, 2208:2224])
                load_w("wg17", wg17[0:17, :], w_gk17)
                load_w("wP", pre[0][:], wi[:, :, 2736:4784])
                load_w("wo", pre[1][:], kchunks(w_out))
                load_w("wpg", pre[2][:], kchunks(w_pg))
                S.dma("sync", gin_bc[:], g_in.partition_broadcast(128), [], ["gin_bc"], "gin_bc")
                S.dma("sync", ggla_bc[:], g_gla.partition_broadcast(128), [], ["ggla_bc"], "ggla_bc")
                S.memset("vector", gkl[:], 1.0, ["gkl"])
                S.memset("vector", Sf[:], 0.0, ["Sf"])
                S.memset("vector", Sb[:], 0.0, ["Sb"])
                WG = ["wG_a", "wG_b"]
                pj_i = [0]

                def pj():
                    b = 1 + pj_i[0] % 2
                    pj_i[0] += 1
                    return b

                for t in range(4):
                    stageG(0, t)
                for g in range(ng):
                    tok0 = s * SEQ + g * 512
                    c0g = g * 512
                    hT = hTs[g % 2]
                    HT = [("hT", g % 2, c0) for c0 in (0, 128, 256, 384)]

                    def proj(col, width=128, w=wG, wn=WG):
                        b = pj()
                        for k in range(8):
                            S.mm(ps[b][0:width, :], w[:, k, col:col + width], hT[:, k, :], k == 0, k == 7,
                                 wn + HT, [("ps", b)])
                        return b

                    for c in range(2):
                        b = proj(c * 128)
                        S.copy("scalar", gqf[r0_, 2 * c, :], ps[b][0:64, :], [("ps", b)], [("gqf", 2 * c)])
                        S.copy("vector", gqf[r0_, 2 * c + 1, :], ps[b][64:128, :], [("ps", b)], [("gqf", 2 * c + 1)])
                    for c in range(2):
                        b = proj(256 + c * 128)
                        S.copy("scalar", gkf[r0_, 2 * c, :], ps[b][0:64, :], [("ps", b)], [("gkf", 2 * c)])
                        S.copy("vector", gkf[r0_, 2 * c + 1, :], ps[b][64:128, :], [("ps", b)], [("gkf", 2 * c + 1)])
                    GQ = [("gqf", h) for h in range(4)]
                    GK = [("gkf", h) for h in range(4)]
                    for c in range(4):
                        b = proj(1024 + c * 128)
                        S.act(sgg[:, c, :], ps[b][:], AF.Silu, [("ps", b)], [("sgg", c)])
                    b = proj(0, 16, wgl, ["wgl"])
                    S.copy("vector", gkl[0:16, :], ps[b][0:16, :], [("ps", b)], ["gkl"])

                    def F(t):
                        cs = slice(t * 128, (t + 1) * 128)
                        vs = t % 2
                        htr = [("hT", g % 2, t * 128)]
                        for k in range(8):
                            S.mm(ps[3][:], hT[:, k, cs], wG[:, k, 512:1024], k == 0, k == 7, WG + htr, [("ps", 3)])
                        S.copy("scalar", vsb[vs][:], ps[3][:], [("ps", 3)], [("vsb", vs)])
                        for k in range(8):
                            S.mm(ps[4][:, 0:256], hT[:, k, cs], wG[:, k, 256:512], k == 0, k == 7, WG + htr, [("ps", 4)])
                        S.mm(ps[4][:, 256:512], gkl[0:17, cs], wg17[0:17, :], True, True, ["gkl", "wg17"], [("ps", 4)])
                        S.act(etmp[:], ps[4][:, 256:512], AF.Exp, [("ps", 4)], ["etmp"], scale=-1.0)
                        S.act(sp[:], etmp[:], AF.Ln, ["etmp"], ["sp"], bias=1.0)
                        for h in range(4):
                            S.mm(ps[5][0:64, h * 128:(h + 1) * 128], sp[:, h * 64:(h + 1) * 64], Lmat, True, True,
                                 ["sp", "cst"], [("ps", 5)])
                        S.mm(ps[2][:, 0:256], Umat, sp[:], True, True, ["sp", "cst"], [("ps", 2)])
                        bt = ps[5][0:64, :].rearrange("p (h t) -> p h t", h=4)
                        S.act(ebT[r0_], bt, AF.Exp, [("ps", 5)], ["ebT"])
                        S.act(enbT[r0_], bt, AF.Exp, [("ps", 5)], ["enbT"], scale=-1.0)
                        S.act(erev[:], ps[2][:, 0:256], AF.Exp, [("ps", 2)], ["erev"])
                        S.stt("vector", qtT[r0_], gqf[r0_, :, cs], 0.125, ebT[r0_], ALU.mult, ALU.mult, GQ + ["ebT"], ["qtT"])
                        S.tt("vector", ktT[r0_], gkf[r0_, :, cs], enbT[r0_], ALU.mult, GK + ["enbT"], ["ktT"])
                        S.tt("vector", kdec[:], ps[4][:, 0:256], erev[:], ALU.mult, [("ps", 4), "erev"], ["kdec"])
                        for h in range(4):
                            S.mm(ps[6][:, h * 128:(h + 1) * 128], ktT[r0_, h, :], qtT[r0_, h, :], True, True,
                                 ["ktT", "qtT"], [("ps", 6)])
                        S.tt("vector", AT[:], ps[6][:].rearrange("p (h t) -> p h t", h=4), tri[:], ALU.mult,
                             [("ps", 6), "tri"], ["AT"])
                    def O(t):
                        cs = slice(t * 128, (t + 1) * 128)
                        vs = t % 2
                        for h in range(4):
                            hs_ = slice(h * 128, (h + 1) * 128)
                            S.mm(ps[7][:, hs_], AT[:, h, :], vsb[vs][:, hs_], True, False, ["AT", ("vsb", vs)], [("ps", 7)])
                            S.mm(ps[7][:, hs_], qtT[r0_, h, :], Sb[r0_, h, :], False, True, ["qtT", "Sb"], [("ps", 7)])
                        for h in range(4):
                            hs_ = slice(h * 128, (h + 1) * 128)
                            S.mm(ps[1][0:64, hs_], kdec[:, h * 64:(h + 1) * 64], vsb[vs][:, hs_], True, True,
                                 ["kdec", ("vsb", vs)], [("ps", 1)])
                        for h in range(4):
                            hs_ = slice(h * 128, (h + 1) * 128)
                            S.stt("vector", Sf[r0_, h, :], Sf[r0_, h, :], ebT[r0_, h, 127:128], ps[1][0:64, hs_], ALU.mult, ALU.add,
                                  ["Sf", "ebT", ("ps", 1)], ["Sf"])
                        S.copy("scalar", Sb[r0_], Sf[r0_], ["Sf"], ["Sb"])
                    def N(t):
                        cs = slice(t * 128, (t + 1) * 128)
                        vs = t % 2
                        for h in range(4):
                            S.act(junk2[:], ps[7][:, h * 128:(h + 1) * 128], AF.Square, [("ps", 7)], ["junk2", "oss"],
                                  accum=stt_[:, 24 + h:25 + h])
                        S.act(stt_[:, 28:32], stt_[:, 24:28], AF.Ln, ["oss"], ["oln"], scale=1.0 / 128, bias=EPS)
                        S.act(stt_[:, 32:36], stt_[:, 28:32], AF.Exp, ["oln"], ["ors"], scale=-0.5)
                        for h in range(4):
                            hs_ = slice(h * 128, (h + 1) * 128)
                            S.stt("vector", onsb[:, hs_], ps[7][:, hs_], stt_[:, 32 + h:33 + h], ggla_bc[:], ALU.mult, ALU.mult,
                                  [("ps", 7), "ors", "ggla_bc"], ["onsb"])
                        for h in range(4):
                            hs_ = slice(h * 128, (h + 1) * 128)
                            S.tr(tpv[:, hs_], onsb[:, hs_], ident[:], ["onsb", "ident"], [("ps", 0)])
                        S.tt("vector", ubT[:, :, c0g + t * 128:c0g + (t + 1) * 128],
                             tpv[:, 0:512].rearrange("p (h t) -> p h t", h=4), sgg[:, :, cs], ALU.mult,
                             [("ps", 0)] + [("sgg", c) for c in range(4)], [("ubT", g, t)])
                    F(0)
                    for t in range(4):
                        if g + 1 < ng:
                            stageG(g + 1, t, "L")
                        O(t)
                        if t + 1 < 4:
                            F(t + 1)
                        N(t)
                        if g + 1 < ng:
                            stageG(g + 1, t, "ST")
                if debug:
                    S.dma("sync", dbg_ub[s][:, :, 0:ng * 512], ubT[:, :, 0:ng * 512], [("ubT", g, t) for g in range(ng) for t in range(4)], ["dbg_ub"], "dbg")
                S.flush()

        def pass_P(s, pre):
            wP, wo, wpg = pre
            with contextlib.ExitStack() as st_:
                wml = sb(st_, "wml", [128, 4, D], BF16)
                wgl_ = sb(st_, "wglb", [128, 4, D], BF16)
                wpl = sb(st_, "wpl", [128, 2, D], BF16)
                gin_bc = sb(st_, "gin_bcP", [128, D], F32)
                gpg_bc = sb(st_, "gpg_bc", [128, D], F32)
                gple_bc = sb(st_, "gple_bc", [128, D], F32)
                gfin_bc = sb(st_, "gfin_bc", [128, D], F32)
                NX = 6
                xt = [sb(st_, "xtP%d" % i, [128, D], F32) for i in range(NX)]
                hn = [sb(st_, "hnP%d" % i, [128, D], BF16) for i in range(1)]
                hT = sb(st_, "hTP", [128, 8, 512], BF16)
                junk_t = sb(st_, "junkP", [128, D], BF16)
                sa = [sb(st_, "sa%d" % i, [128, 512], BF16) for i in range(2)]
                sbb = [sb(st_, "sbb%d" % i, [128, 512], BF16) for i in range(2)]
                t1 = [sb(st_, "t1_%d" % i, [128, 512], BF16) for i in range(2)]
                t2 = [sb(st_, "t2_%d" % i, [128, 512], BF16) for i in range(2)]
                mg = sb(st_, "mg", [128, 8, 512], BF16)
                x1n = sb(st_, "x1n", [128, D], BF16)
                x1nT = sb(st_, "x1nT", [128, 8, 128], BF16)
                pbf = [sb(st_, "pbf%d" % i, [128, 256], BF16) for i in range(2)]
                pT_ = sb(st_, "pTP", [128, 2, 128], BF16)
                sig = sb(st_, "sig", [128, 512], F32)
                et = sb(st_, "et", [128, D], F32)
                ot = [sb(st_, "ot%d" % i, [128, D], F32) for i in range(2)]
                stt_ = sb(st_, "stP", [128, 48], F32)
                ctx = dict(xt=xt, hn=hn, st=stt_, junk=junk_t[:], hT=hT, gin_bc=gin_bc)

                load_w("wml", wml[:], kchunks(w_mla))
                load_w("wglb", wgl_[:], kchunks(w_gla))
                load_w("wpl", wpl[:], kchunks(w_ple))
                S.dma("sync", gin_bc[:], g_in.partition_broadcast(128), [], ["gin_bc"], "gin_bc")
                S.dma("sync", gpg_bc[:], g_pg.partition_broadcast(128), [], ["gpg_bc"], "gpg_bc")
                S.dma("sync", gple_bc[:], g_ple.partition_broadcast(128), [], ["gple_bc"], "gple_bc")
                S.dma("sync", gfin_bc[:], g_fin.partition_broadcast(128), [], ["gfin_bc"], "gfin_bc")
                pj_i = [0]
                rot = [1, 2, 3]

                def pj():
                    b = rot[pj_i[0] % 3]
                    pj_i[0] += 1
                    return b

                HT = [("hT", c0) for c0 in (0, 128, 256, 384)]

                def stageP(gn, t, parts="LST"):
                    stage_hT(ctx, s * SEQ + gn * 512 + t * 128, (4 * gn + t) % NX, t * 128, "vector", parts=parts)

                for t in range(4):
                    stageP(0, t)
                for g in range(ng):
                    tok0 = s * SEQ + g * 512
                    c0g = g * 512
                    for m in range(8):
                        sl = m % 2
                        ba = pj()
                        for k in range(8):
                            S.mm(ps[ba][:], wP[:, k, m * 128:(m + 1) * 128], hT[:, k, :], k == 0, k == 7, ["wP"] + HT, [("ps", ba)])
                        S.act(sa[sl][:], ps[ba][:], AF.Sigmoid, [("ps", ba)], [("sa", sl)])
                        by = pj()
                        for c in range(4):
                            S.mm(ps[by][:], wml[:, c, m * 128:(m + 1) * 128], uaT[:, c, c0g:c0g + 512], c == 0, c == 3, ["wml"], [("ps", by)])
                        S.tt("vector", t1[sl][:], ps[by][:], sa[sl][:], ALU.mult, [("ps", by), ("sa", sl)], [("t1", sl)])
                        bb = pj()
                        for k in range(8):
                            S.mm(ps[bb][:], wP[:, k, 1024 + m * 128:1024 + (m + 1) * 128], hT[:, k, :], k == 0, k == 7, ["wP"] + HT, [("ps", bb)])
                        S.act(sbb[sl][:], ps[bb][:], AF.Sigmoid, [("ps", bb)], [("sbb", sl)])
                        bz = pj()
                        for c in range(4):
                            S.mm(ps[bz][:], wgl_[:, c, m * 128:(m + 1) * 128], ubT[:, c, c0g:c0g + 512], c == 0, c == 3, ["wglb"], [("ps", bz)])
                        S.tt("vector", t2[sl][:], ps[bz][:], sbb[sl][:], ALU.mult, [("ps", bz), ("sbb", sl)], [("t2", sl)])
                        S.tt("gpsimd", mg[:, m, :], t1[sl][:], t2[sl][:], ALU.add, [("t1", sl), ("t2", sl)], [("mg", m)])
                    MG = [("mg", m) for m in range(8)]

                    def xs_of(t):
                        return (4 * g + t) % NX

                    def A1(t):
                        xs = xs_of(t)
                        cs = slice(t * 128, (t + 1) * 128)
                        for half in range(2):
                            b = 4 + half
                            hs_ = slice(half * 512, (half + 1) * 512)
                            for m in range(8):
                                S.mm(ps[b][:], mg[:, m, cs], wo[:, m, hs_], m == 0, m == 7, ["wo"] + MG, [("ps", b)])
                            S.tt("vector", xt[xs][:, hs_], xt[xs][:, hs_], ps[b][:], ALU.add, [("xt", xs), ("ps", b)], [("xt", xs)])
                        S.act(junk_t[:], xt[xs][:], AF.Square, [("xt", xs)], ["junk", "s1"], accum=stt_[:, 24:25])
                        S.act(stt_[:, 25:26], stt_[:, 24:25], AF.Ln, ["s1"], ["l1"], scale=1.0 / D, bias=EPS)
                        S.act(stt_[:, 26:27], stt_[:, 25:26], AF.Exp, ["l1"], ["r1"], scale=-0.5)

                    def A2(t):
                        xs = xs_of(t)
                        S.stt("vector", x1n[:], xt[xs][:], stt_[:, 26:27], gpg_bc[:], ALU.mult, ALU.mult, [("xt", xs), "r1", "gpg_bc"], ["x1n"])

                    def B(t):
                        tok = tok0 + t * 128
                        ps_ = t % 2
                        S.dma("gpsimd", pbf[ps_][:], pin[tok:tok + 128, :], [], [("pbf", ps_)], ("pbf", ps_))
                        for c in range(2):
                            S.tr(tpv[:, c * 128:(c + 1) * 128], pbf[ps_][:, c * 128:(c + 1) * 128], ident[:], [("pbf", ps_), "ident"], [("ps", 0)])
                        S.copy("vector", pT_[:], tpv[:, 0:256].rearrange("p (c t) -> p c t", c=2), [("ps", 0)], ["pTP"])
                        for half in range(2):
                            b = 1 + half
                            for c in range(2):
                                S.mm(ps[b][:], pT_[:, c, :], wpl[:, c, half * 512:(half + 1) * 512], c == 0, c == 1, ["wpl", "pTP"], [("ps", b)])
                            S.act(junk_t[:, 0:512], ps[b][:], AF.Square, [("ps", b)], ["junk", ("se", half)], accum=stt_[:, 27 + half:28 + half])
                        S.tt("vector", stt_[:, 29:30], stt_[:, 27:28], stt_[:, 28:29], ALU.add, [("se", 0), ("se", 1)], ["se2"])
                        S.act(stt_[:, 30:31], stt_[:, 29:30], AF.Ln, ["se2"], ["le"], scale=1.0 / D, bias=EPS)
                        S.act(stt_[:, 31:32], stt_[:, 30:31], AF.Exp, ["le"], ["re"], scale=-0.5)
                        for half in range(2):
                            b = 1 + half
                            hs_ = slice(half * 512, (half + 1) * 512)
                            S.stt("vector", et[:, hs_], ps[b][:], stt_[:, 31:32], gple_bc[:, hs_], ALU.mult, ALU.mult,
                                  [("ps", b), "re", "gple_bc"], [("et", half)])

                    def C1(t):
                        for c in range(8):
                            S.tr(tpv[:, c * 128:(c + 1) * 128], x1n[:, c * 128:(c + 1) * 128], ident[:], ["x1n", "ident"], [("ps", 0)])
                        S.copy("scalar", x1nT[:], tpv.rearrange("p (c t) -> p c t", c=8), [("ps", 0)], ["x1nT"])

                    def C2a(t):
                        for half in range(2):
                            b = 6 + half
                            hs_ = slice(half * 512, (half + 1) * 512)
                            for c in range(8):
                                S.mm(ps[b][:], x1nT[:, c, :], wpg[:, c, hs_], c == 0, c == 7, ["wpg", "x1nT"], [("ps", b)])

                    def C2b(t):
                        tok = tok0 + t * 128
                        xs = xs_of(t)
                        osl = t % 2
                        for half in range(2):
                            b = 6 + half
                            hs_ = slice(half * 512, (half + 1) * 512)
                            S.act(sig[:], ps[b][:], AF.Sigmoid, [("ps", b)], ["sig"])
                            S.tt("vector", et[:, hs_], et[:, hs_], sig[:], ALU.mult, [("et", half), "sig"], [("et", half)])
                        S.tt("vector", xt[xs][:], xt[xs][:], et[:], ALU.add, [("xt", xs), ("et", 0), ("et", 1)], [("xt", xs)])
                        S.act(junk_t[:], xt[xs][:], AF.Square, [("xt", xs)], ["junk", "s2"], accum=stt_[:, 32:33])
                        S.act(stt_[:, 33:34], stt_[:, 32:33], AF.Ln, ["s2"], ["l2"], scale=1.0 / D, bias=EPS)
                        S.act(stt_[:, 34:35], stt_[:, 33:34], AF.Exp, ["l2"], ["r2"], scale=-0.5)
                        S.stt("vector", ot[osl][:], xt[xs][:], stt_[:, 34:35], gfin_bc[:], ALU.mult, ALU.mult,
                              [("xt", xs), "r2", "gfin_bc"], [("ot", osl)])
                        S.dma("sync", out[tok:tok + 128, :], ot[osl][:], [("ot", osl)], [("out", tok)], ("ot", osl))

                    nxt = g + 1 < ng
                    A1(0)
                    A2(0)
                    for t in range(4):
                        if nxt:
                            if t == 0:
                                stageP(g + 1, 0, "L")
                            if t + 1 < 4:
                                stageP(g + 1, t + 1, "L")
                        C1(t)
                        if t + 1 < 4:
                            A1(t + 1)
                            A2(t + 1)
                        B(t)
                        if nxt:
                            stageP(g + 1, t, "S")
                        C2a(t)
                        if nxt:
                            stageP(g + 1, t, "T")
                        C2b(t)
                S.flush()

        for s in range(nseq):
            if "M" in passes:
                pass_M(s)
            with contextlib.ExitStack() as pw:
                pre = (sb(pw, "wP", [128, 8, 2048], BF16), sb(pw, "wo", [128, 8, D], BF16), sb(pw, "wpg", [128, 8, D], BF16))
                if "G" in passes:
                    pass_G(s, pre)
                if "P" in passes:
                    pass_P(s, pre)
        if S.ops:
            S.flush()
    return nc


def _consts():
    c = np.zeros((128, C_END), np.float32)
    idx = np.arange(128)
    c[:, C_ID:C_ID + 128] = np.eye(128, dtype=np.float32)
    triu = (idx[None, :] >= idx[:, None]).astype(np.float32)
    c[:, C_TRI:C_TRI + 128] = triu
    c[:, C_L:C_L + 128] = -triu / 16.0
    c[:, C_U:C_U + 128] = -(idx[:, None] > idx[None, :]).astype(np.float32) / 16.0
    c[64, C_SEL:C_SEL + 64] = 1.0
    c[0, C_SEL + 64:C_SEL + 128] = 1.0
    inv_freq = 1.0 / (10000.0 ** (np.arange(0, 32, 2, dtype=np.float64) / 32.0))
    for r in range(32):
        c[64 + r, C_ROPE] = inv_freq[r % 16] / (2.0 * np.pi)
        c[64 + r, C_ROPE + 1] = -TWO_PI if r < 16 else TWO_PI
    return c


_NC_CACHE = {}


def _prep_shared(inp):
    f = lambda a: np.ascontiguousarray(np.asarray(a, dtype=np.float32))
    w_in = f(inp["w_in"][0])
    kr = w_in[:, 640:672]
    w_krz = np.zeros((D, 192), np.float32)
    w_krz[:, 64:96] = kr
    w_krz[:, 96 + 64:96 + 80] = kr[:, 16:32]
    w_krz[:, 96 + 80:96 + 96] = kr[:, 0:16]
    w_uq = f(inp["w_uq"][0])
    w_uqsw = np.zeros((384, 768), np.float32)
    for h in range(8):
        rp = w_uq[:, 96 * h + 64:96 * h + 96]
        w_uqsw[:, 96 * h + 64:96 * h + 80] = rp[:, 16:32]
        w_uqsw[:, 96 * h + 80:96 * h + 96] = rp[:, 0:16]
    w_ukv = f(inp["w_ukv"][0]).reshape(256, 8, 2, 64)
    w_ukvk = np.ascontiguousarray(w_ukv[:, :, 0, :].reshape(256, 512))
    w_ukvv = np.ascontiguousarray(w_ukv[:, :, 1, :].reshape(256, 512))
    w_gk17 = np.concatenate([f(inp["w_gk_up"][0]), f(inp["b_gk"][0]).reshape(1, 256)], axis=0)
    gcols = np.zeros((128, 8), np.float32)
    gcols[:, 0:3] = f(inp["q_norm_g"][0]).reshape(3, 128).T
    gcols[:, 3:5] = f(inp["kv_norm_g"][0]).reshape(2, 128).T
    return {
        "w_in": w_in, "w_krz": w_krz, "w_uq": w_uq, "w_uqsw": w_uqsw, "w_ukvk": w_ukvk, "w_ukvv": w_ukvv,
        "w_gk17": np.ascontiguousarray(w_gk17), "w_mla": f(inp["w_mla_br"][0]), "w_gla": f(inp["w_gla_br"][0]),
        "w_out": f(inp["w_out"][0]), "w_ple": f(inp["w_ple"][0]), "w_pg": f(inp["w_ple_gate"][0]),
        "g_in": f(inp["norm_in_g"][0]).reshape(1, D), "g_gla": f(inp["gla_norm_g"][0]).reshape(1, 128),
        "g_ple": f(inp["ple_norm_g"][0]).reshape(1, D), "g_pg": f(inp["ple_gate_norm_g"][0]).reshape(1, D),
        "g_fin": f(inp["final_norm_g"]).reshape(1, D), "gcols": gcols, "consts": _consts(),
    }


def kernel(**inputs):
    if "nc" not in _NC_CACHE:
        _NC_CACHE["nc"] = build(False)
    nc = _NC_CACHE["nc"]
    shared = _prep_shared(inputs)
    x = np.asarray(inputs["x"], dtype=np.float32)
    p = np.asarray(inputs["p"], dtype=np.float32)[0]
    pos = np.asarray(inputs["positions"], dtype=np.int32)
    in_maps = []
    for c in range(NCORES):
        m = dict(shared)
        m["x"] = np.ascontiguousarray(x[2 * c:2 * c + 2].reshape(2 * SEQ, D))
        m["p"] = np.ascontiguousarray(p[2 * c:2 * c + 2].reshape(2 * SEQ, 256))
        m["pos"] = np.ascontiguousarray(pos[2 * c:2 * c + 2].reshape(1, 2 * SEQ))
        in_maps.append(m)
    res = run_bass_kernel_spmd(nc, in_maps, core_ids=list(range(NCORES)))
    outs = [np.asarray(r["out"]).reshape(2, SEQ, D) for r in res.results]
    return np.concatenate(outs, axis=0).astype(np.float32)
```

```python
import contextlib
import numpy as np
import concourse.bass as bass
import concourse.mybir as mybir
from concourse.bass_utils import run_bass_kernel_spmd

F32 = mybir.dt.float32
BF16 = mybir.dt.bfloat16
I32 = mybir.dt.int32
AF = mybir.ActivationFunctionType
ALU = mybir.AluOpType

NCORES = 8
SEQ = 2048
D = 1024
EPS = 1e-6
TWO_PI = float(2.0 * np.pi * (1.0 - 2e-7))
ENGINES = ("sync", "scalar", "vector", "gpsimd", "tensor")

C_ID, C_TRI, C_L, C_U, C_SEL, C_ROPE, C_END = 0, 128, 256, 384, 512, 640, 644


class _Op:
    __slots__ = ("eng", "fn", "dma", "waits", "inc", "idx", "key", "pos")


class Sched:
    def __init__(self, nc, stack):
        self.nc = nc
        self.stack = stack
        self.sems = {}
        self.counts = {}
        self.seen = {e: {} for e in ENGINES}
        self._reset()

    def _reset(self):
        self.ops = []
        self.last_w = {}
        self.readers = {}

    def add(self, eng, fn, R=(), W=(), dma=False, stream=None):
        R = tuple(R) + tuple((r[0], r[1], h) for r in R if r == ("scr", 0) for h in ("a", "b"))
        W = tuple(W) + tuple((r[0], r[1], h) for r in W if r == ("scr", 0) for h in ("a", "b"))
        op = _Op()
        op.eng, op.fn, op.dma, op.inc, op.idx = eng, fn, dma, False, 0
        op.key = ("dma", stream) if dma else ("eng", eng)
        op.pos = len(self.ops)
        best = {}
        W = tuple(W) + tuple(r for r in R if isinstance(r, tuple) and r[0] == "ps" and r not in W)

        def dep(d, kind):
            if d is None or d is op:
                return
            if (not d.dma) and d.eng == eng and not dma:
                if eng == "tensor":
                    return
            cur = best.get(d.key)
            if cur is None or d.pos > cur.pos:
                best[d.key] = d

        for r in R:
            dep(self.last_w.get(r), "raw")
        for r in W:
            dep(self.last_w.get(r), "waw")
            for rd in self.readers.get(r, ()):
                dep(rd, "war")
        op.waits = list(best.values())
        for d in op.waits:
            d.inc = True
        for r in W:
            self.last_w[r] = op
            self.readers[r] = []
        for r in R:
            lst = self.readers.setdefault(r, [])
            for i, o in enumerate(lst):
                if o.key == op.key:
                    lst[i] = op
                    break
            else:
                lst.append(op)
        self.ops.append(op)
        return op

    def flush(self):
        nc = self.nc
        per_eng = {e: [] for e in ENGINES}
        for op in self.ops:
            per_eng[op.eng].append(op)
        for e in ENGINES:
            for op in reversed(per_eng[e]):
                if not op.dma:
                    op.inc = True
                    break
        for op in self.ops:
            if op.dma:
                op.inc = True
        for op in self.ops:
            if op.inc:
                k = op.key
                if k not in self.sems:
                    self.sems[k] = self.stack.enter_context(nc.semaphore("s%d" % len(self.sems)))
                    self.counts[k] = 0
                self.counts[k] += 16 if op.dma else 1
                op.idx = self.counts[k]
        finals = dict(self.counts)
        sems, seen_all = self.sems, self.seen

        def make(engname):
            def body(eng):
                seen = seen_all[engname]
                for op in per_eng[engname]:
                    for d in op.waits:
                        if seen.get(d.key, 0) < d.idx:
                            eng.wait_ge(sems[d.key], d.idx)
                            seen[d.key] = d.idx
                    ins = op.fn(eng)
                    if op.inc:
                        ins.then_inc(sems[op.key], 16 if op.dma else 1)
                for k, v in finals.items():
                    if seen.get(k, 0) < v:
                        eng.wait_ge(sems[k], v)
                        seen[k] = v
            return body

        with nc.Block() as block:
            block.sync(make("sync"))
            block.scalar(make("scalar"))
            block.vector(make("vector"))
            block.gpsimd(make("gpsimd"))
            block.tensor(make("tensor"))
        self._reset()

    def dma(self, eng, out, in_, R, W, stream):
        return self.add(eng, lambda e: e.dma_start(out=out, in_=in_), R, W, dma=True, stream=stream)

    def mm(self, out, lhsT, rhs, start, stop, R, W, tp=None):
        if tp is not None:
            return self.add("tensor", lambda e: e.matmul(out, lhsT=lhsT, rhs=rhs, start=start, stop=stop, tile_position=tp), R, W)
        return self.add("tensor", lambda e: e.matmul(out, lhsT=lhsT, rhs=rhs, start=start, stop=stop), R, W)

    def tr(self, out, in_, ident, R, W):
        return self.add("tensor", lambda e: e.transpose(out=out, in_=in_, identity=ident), R, W)

    def act(self, out, in_, func, R, W, scale=None, bias=None, accum=None):
        kw = {}
        if scale is not None:
            kw["scale"] = scale
        if bias is not None:
            kw["bias"] = bias
        if accum is not None:
            kw["accum_out"] = accum
        return self.add("scalar", lambda e: e.activation(out=out, in_=in_, func=func, **kw), R, W)

    def copy(self, eng, out, in_, R, W):
        if eng == "scalar":
            return self.add(eng, lambda e: e.copy(out=out, in_=in_), R, W)
        return self.add(eng, lambda e: e.tensor_copy(out=out, in_=in_), R, W)

    def tt(self, eng, out, in0, in1, op, R, W):
        return self.add(eng, lambda e: e.tensor_tensor(out=out, in0=in0, in1=in1, op=op), R, W)

    def ts(self, eng, out, in0, s1, op0, R, W, s2=None, op1=None):
        if op1 is None:
            return self.add(eng, lambda e: e.tensor_scalar(out=out, in0=in0, scalar1=s1, scalar2=None, op0=op0), R, W)
        return self.add(eng, lambda e: e.tensor_scalar(out=out, in0=in0, scalar1=s1, scalar2=s2, op0=op0, op1=op1), R, W)

    def stt(self, eng, out, in0, scalar, in1, op0, op1, R, W):
        return self.add(eng, lambda e: e.scalar_tensor_tensor(out=out, in0=in0, scalar=scalar, in1=in1, op0=op0, op1=op1), R, W)

    def memset(self, eng, ap, val, W):
        return self.add(eng, lambda e: e.memset(ap, val), (), W)

    def recip(self, out, in_, R, W):
        return self.add("vector", lambda e: e.reciprocal(out=out, in_=in_), R, W)


def build(debug=False, nseq=2, ng=4, passes="MGP", cut=99):
    nc = bass.Bass("TRN2", target_bir_lowering=False)

    def din(name, shape, dt=F32):
        return nc.dram_tensor(name, list(shape), dt, kind="ExternalInput").ap()

    x = din("x", [2 * SEQ, D])
    pin = din("p", [2 * SEQ, 256])
    pos = din("pos", [1, 2 * SEQ], I32)
    w_in = din("w_in", [D, 4784])
    w_krz = din("w_krz", [D, 192])
    w_uq = din("w_uq", [384, 768])
    w_uqsw = din("w_uqsw", [384, 768])
    w_ukvk = din("w_ukvk", [256, 512])
    w_ukvv = din("w_ukvv", [256, 512])
    w_gk17 = din("w_gk17", [17, 256])
    w_mla = din("w_mla", [512, D])
    w_gla = din("w_gla", [512, D])
    w_out = din("w_out", [D, D])
    w_ple = din("w_ple", [256, D])
    w_pg = din("w_pg", [D, D])
    g_in = din("g_in", [1, D])
    g_gla = din("g_gla", [1, 128])
    g_ple = din("g_ple", [1, D])
    g_pg = din("g_pg", [1, D])
    g_fin = din("g_fin", [1, D])
    gcols = din("gcols", [128, 8])
    consts = din("consts", [128, C_END])
    out = nc.dram_tensor("out", [2 * SEQ, D], F32, kind="ExternalOutput").ap()
    if debug:
        dbg_ua = nc.dram_tensor("dbg_ua", [2, 128, 4, SEQ], BF16, kind="ExternalOutput").ap()
        dbg_ub = nc.dram_tensor("dbg_ub", [2, 128, 4, SEQ], BF16, kind="ExternalOutput").ap()

    top = contextlib.ExitStack()
    with top:
        uid = [0]

        def sb(stack, name, shape, dt):
            uid[0] += 1
            return stack.enter_context(nc.sbuf_tensor("%s_%d" % (name, uid[0]), list(shape), dt))

        S = Sched(nc, top)
        cst = sb(top, "cst", [128, C_END], F32)
        gc = sb(top, "gc", [128, 8], F32)
        ident = sb(top, "ident", [128, 128], BF16)
        tri = sb(top, "tri", [128, 4, 128], BF16)
        ones_bf = sb(top, "ones_bf", [128, 128], BF16)
        uaT = sb(top, "uaT", [128, 4, SEQ], BF16)
        ubT = sb(top, "ubT", [128, 4, SEQ], BF16)
        ps = [top.enter_context(nc.psum_tensor("ps%d" % b, [128, 512], F32)) for b in range(8)]
        tpv = ps[0][:].bitcast(BF16)

        S.dma("sync", cst[:], consts, [], ["cst"], "cst")
        S.dma("sync", gc[:], gcols, [], ["gc"], "gc")
        S.copy("vector", ident[:], cst[:, C_ID:C_ID + 128], ["cst"], ["ident"])
        for h in range(4):
            S.copy("vector", tri[:, h, :], cst[:, C_TRI:C_TRI + 128], ["cst"], ["tri"])
        S.memset("vector", ones_bf[:], 1.0, ["ones_bf"])
        Lmat = cst[:, C_L:C_L + 128]
        Umat = cst[:, C_U:C_U + 128]

        def stage_hT(ctx, tok0, xs, col0, evac_eng, hT=None, htag=("hT",), parts="LST"):
            xt, hn, st, junk, gbc = ctx["xt"], ctx["hn"], ctx["st"], ctx["junk"], ctx["gin_bc"]
            if hT is None:
                hT = ctx["hT"]
            hs = xs % len(hn)
            if "L" in parts:
                S.dma("sync", xt[xs][:], x[tok0:tok0 + 128, :], [], [("xt", xs)], ("xt", xs))
            if "S" in parts:
                S.act(junk, xt[xs][:], AF.Square, [("xt", xs)], ["junk", ("ss", xs)], accum=st[:, xs:xs + 1])
                S.act(st[:, 8 + xs:9 + xs], st[:, xs:xs + 1], AF.Ln, [("ss", xs)], [("ln", xs)], scale=1.0 / D, bias=EPS)
                S.act(st[:, 16 + xs:17 + xs], st[:, 8 + xs:9 + xs], AF.Exp, [("ln", xs)], [("rs", xs)], scale=-0.5)
                S.stt("vector", hn[hs][:], xt[xs][:], st[:, 16 + xs:17 + xs], gbc[:], ALU.mult, ALU.mult,
                      [("xt", xs), ("rs", xs), "gin_bc"], [("hn", hs)])
            if "T" in parts:
                for c in range(8):
                    S.tr(tpv[:, c * 128:(c + 1) * 128], hn[hs][:, c * 128:(c + 1) * 128], ident[:],
                         [("hn", hs), "ident"], [("ps", 0)])
                S.copy(evac_eng, hT[:, :, col0:col0 + 128], tpv.rearrange("p (c t) -> p c t", c=8),
                       [("ps", 0)], [htag + (col0,)])

        def load_w(name, dst, src, R=()):
            S.dma("gpsimd", dst, src, list(R), [name], name)

        def kchunks(w):
            return w.rearrange("(c p) n -> p c n", p=128)

        def pass_M(s):
            with contextlib.ExitStack() as st_:
                wM = sb(st_, "wM", [128, 8, 1152], BF16)
                wkr = sb(st_, "wkr", [128, 8, 192], BF16)
                wuq = sb(st_, "wuq", [128, 3, 768], BF16)
                wuqs = sb(st_, "wuqs", [128, 3, 768], BF16)
                wkk = sb(st_, "wkk", [128, 2, 512], BF16)
                wkv = sb(st_, "wkv", [128, 2, 512], BF16)
                kT = sb(st_, "kT", [128, 8, SEQ], BF16)
                Ve = sb(st_, "Ve", [128, 16, 4, 128], BF16)
                Vo = sb(st_, "Vo", [128, 16, 4, 128], BF16)
                gin_bc = sb(st_, "gin_bc", [128, D], F32)
                xt = [sb(st_, "xt%d" % i, [128, D], F32) for i in range(2)]
                hn = [sb(st_, "hn%d" % i, [128, D], BF16) for i in range(2)]
                hTs = [sb(st_, "hT%d" % i, [128, 8, 512], BF16) for i in range(2)]
                scr = sb(st_, "scr", [128, 3, 512], F32)
                sq = sb(st_, "sq", [128, 3, 512], BF16)
                rq = sb(st_, "rq", [128, 512], F32)
                cqn = sb(st_, "cqn", [128, 3, 512], BF16)
                ckvn = sb(st_, "ckvn", [128, 2, 512], BF16)
                sg = sb(st_, "sg", [128, 4, 512], BF16)
                qT = sb(st_, "qT", [128, 8, 512], BF16)
                posi = sb(st_, "posi", [128, 512], I32)
                cos2 = sb(st_, "cos2", [128, 512], F32)
                sin2 = sb(st_, "sin2", [128, 512], F32)
                pT = [sb(st_, "pT%d" % i, [128, 512], BF16) for i in range(4)]
                stt_ = sb(st_, "stM", [128, 24], F32)
                junk = sq[:, 0:2, :].rearrange("p a b -> p (a b)")
                ctx = dict(xt=xt, hn=hn, st=stt_, junk=junk, hT=None, gin_bc=gin_bc)

                def stageM(gn, t):
                    stage_hT(ctx, s * SEQ + gn * 512 + t * 128, t % 2, t * 128, "scalar" if t % 2 else "vector",
                             hT=hTs[gn % 2], htag=("hT", gn % 2))

                wi = kchunks(w_in)
                load_w("wM_a", wM[:, :, 0:640], wi[:, :, 0:640])
                load_w("wM_b", wM[:, :, 640:1152], wi[:, :, 672:1184])
                load_w("wkr", wkr[:], kchunks(w_krz))
                load_w("wuq", wuq[:], kchunks(w_uq))
                load_w("wuqs", wuqs[:], kchunks(w_uqsw))
                load_w("wkk", wkk[:], kchunks(w_ukvk))
                load_w("wkv", wkv[:], kchunks(w_ukvv))
                S.dma("sync", gin_bc[:], g_in.partition_broadcast(128), [], ["gin_bc"], "gin_bc")
                S.memset("vector", Ve[:, :, :, 64:128], 1.0, ["Vones"])
                S.memset("vector", Vo[:, :, :, 0:64], 1.0, ["Vones"])
                WM = ["wM_a", "wM_b"]
                SCALE = float(96 ** -0.5)
                for t in range(4):
                    stageM(0, t)
                r64 = slice(64, 96)
                pj_rot = [1, 2, 3, 4, 7]
                pj_i = [0]

                def pj():
                    b = pj_rot[pj_i[0] % 5]
                    pj_i[0] += 1
                    return b

                for g in range(ng):
                    tok0 = s * SEQ + g * 512
                    c0g = g * 512
                    hT = hTs[g % 2]
                    HT = [("hT", g % 2, c0) for c0 in (0, 128, 256, 384)]
                    S.dma("sync", posi[r64, :], pos[0:1, tok0:tok0 + 512].partition_broadcast(32), [], ["posi"], "posi")
                    S.copy("vector", scr[r64, 0, :], posi[r64, :], ["posi"], [("scr", 0)])
                    S.ts("vector", scr[r64, 0, :], scr[r64, 0, :], cst[r64, C_ROPE:C_ROPE + 1], ALU.mult,
                         [("scr", 0), "cst"], [("scr", 0)])
                    S.copy("vector", posi[r64, :], scr[r64, 0, :], [("scr", 0)], ["posi"])
                    S.copy("vector", scr[r64, 1, :], posi[r64, :], ["posi"], [("scr", 1)])
                    S.tt("vector", scr[r64, 1, :], scr[r64, 0, :], scr[r64, 1, :], ALU.subtract,
                         [("scr", 0), ("scr", 1)], [("scr", 1)])
                    S.act(sin2[r64, :], scr[r64, 1, :], AF.Sin, [("scr", 1), "cst"], ["sin2"],
                          scale=cst[r64, C_ROPE + 1:C_ROPE + 2])
                    S.ts("vector", scr[r64, 2, :], scr[r64, 0, :], 0.25, ALU.add, [("scr", 0)], [("scr", 2)])
                    S.copy("vector", posi[r64, :], scr[r64, 2, :], [("scr", 2)], ["posi"])
                    S.copy("vector", scr[r64, 1, :], posi[r64, :], ["posi"], [("scr", 1)])
                    S.tt("vector", scr[r64, 1, :], scr[r64, 2, :], scr[r64, 1, :], ALU.subtract,
                         [("scr", 2), ("scr", 1)], [("scr", 1)])
                    S.act(cos2[r64, :], scr[r64, 1, :], AF.Sin, [("scr", 1)], ["cos2"], scale=TWO_PI)


                    def proj(col, width=128, w=wM, wn=WM, m0=0):
                        b = pj()
                        for k in range(8):
                            S.mm(ps[b][m0:m0 + width, :], w[:, k, col:col + width], hT[:, k, :],
                                 k == 0, k == 7, wn + HT, [("ps", b)])
                        return b

                    def lowrank_norm(col_base, nch, gcol0, dst, dname, inv_n):
                        for c in range(nch):
                            b = proj(col_base + c * 128)
                            S.act(sq[:, c, :], ps[b][:], AF.Square, [("ps", b)], [("sq", c)])
                            S.copy("vector", scr[:, c, :], ps[b][:], [("ps", b)], [("scr", c)])
                        b = pj()
                        for c in range(nch):
                            S.mm(ps[b][:], ones_bf[:], sq[:, c, :], c == 0, c == nch - 1,
                                 ["ones_bf", ("sq", c)], [("ps", b)])
                        if cut == 24:
                            return
                        S.act(rq[:], ps[b][:], AF.Ln, [("ps", b)], ["rq"], scale=inv_n, bias=EPS)
                        if cut == 25:
                            return
                        S.act(rq[:], rq[:], AF.Exp, ["rq"], ["rq"], scale=-0.5)
                        if cut == 26:
                            return
                        for c in range(nch):
                            S.stt("vector", dst[:, c, :], scr[:, c, :], gc[:, gcol0 + c:gcol0 + c + 1], rq[:],
                                  ALU.mult, ALU.mult, [("scr", c), "gc", "rq"], [(dname, c)])

                    if cut == 20:
                        b = proj(0)
                        S.flush()
                        return
                    if cut == 21:
                        b = proj(0)
                        S.act(sq[:, 0, :], ps[b][:], AF.Square, [("ps", b)], [("sq", 0)])
                        S.flush()
                        return
                    if cut == 22:
                        b = proj(0)
                        S.copy("vector", scr[:, 0, :], ps[b][:], [("ps", b)], [("scr", 0)])
                        S.flush()
                        return
                    lowrank_norm(0, 3, 0, cqn, "cqn", 1.0 / 384)
                    CQN = [("cqn", c) for c in range(3)]
                    if cut in (23, 24, 25, 26):
                        S.flush()
                        return
                    lowrank_norm(384, 2, 3, ckvn, "ckvn", 1.0 / 256)
                    CKV = [("ckvn", c) for c in range(2)]
                    for c in range(4):
                        b = proj(640 + c * 128)
                        S.act(sg[:, c, :], ps[b][:], AF.Silu, [("ps", b)], [("sg", c)])
                    if cut == 3:
                        S.flush()
                        return
                    ba = proj(0, 96, wkr, ["wkr"])
                    bb = proj(96, 96, wkr, ["wkr"])
                    S.tt("vector", scr[r64, 0, :], ps[ba][r64, :], cos2[r64, :], ALU.mult, [("ps", ba), "cos2"], [("scr", 0)])
                    S.tt("vector", scr[r64, 1, :], ps[bb][r64, :], sin2[r64, :], ALU.mult, [("ps", bb), "sin2"], [("scr", 1)])
                    S.tt("vector", kT[r64, 0, c0g:c0g + 512], scr[r64, 0, :], scr[r64, 1, :], ALU.add,
                         [("scr", 0), ("scr", 1)], [("kTr", 0, g)])
                    for h in range(1, 8):
                        S.copy("vector", kT[r64, h, c0g:c0g + 512], kT[r64, 0, c0g:c0g + 512],
                               [("kTr", 0, g)], [("kTr", h, g)])
                    if cut == 4:
                        S.flush()
                        return
                    for h in range(8):
                        ba, bb = pj(), pj()
                        for c in range(3):
                            S.mm(ps[ba][0:96, :], wuq[:, c, 96 * h:96 * h + 96], cqn[:, c, :], c == 0, c == 2,
                                 ["wuq"] + CQN, [("ps", ba)])
                        for c in range(3):
                            S.mm(ps[bb][0:96, :], wuqs[:, c, 96 * h:96 * h + 96], cqn[:, c, :], c == 0, c == 2,
                                 ["wuqs"] + CQN, [("ps", bb)])
                        S.copy("scalar", qT[0:64, h, :], ps[ba][0:64, :], [("ps", ba)], [("qTn", h)])
                        S.tt("vector", scr[r64, 0, :], ps[ba][r64, :], cos2[r64, :], ALU.mult, [("ps", ba), "cos2"], [("scr", 0)])
                        S.tt("vector", scr[r64, 1, :], ps[bb][r64, :], sin2[r64, :], ALU.mult, [("ps", bb), "sin2"], [("scr", 1)])
                        S.tt("vector", qT[r64, h, :], scr[r64, 0, :], scr[r64, 1, :], ALU.add,
                             [("scr", 0), ("scr", 1)], [("qTr", h)])
                    for h in range(8):
                        b = pj()
                        for c in range(2):
                            S.mm(ps[b][0:64, :], wkk[:, c, 64 * h:64 * h + 64], ckvn[:, c, :], c == 0, c == 1,
                                 ["wkk"] + CKV, [("ps", b)])
                        S.copy("scalar" if h % 2 else "vector", kT[0:64, h, c0g:c0g + 512], ps[b][0:64, :],
                               [("ps", b)], [("kTn", h, g)])
                    for t in range(4):
                        T = g * 4 + t
                        b = pj()
                        for c in range(2):
                            S.mm(ps[b][:], ckvn[:, c, t * 128:(t + 1) * 128], wkv[:, c, :], c == 0, c == 1,
                                 ["wkv"] + CKV, [("ps", b)])
                        pv = ps[b][:].rearrange("p (i two d) -> p i two d", two=2, d=64)
                        S.copy("vector", Ve[:, T, :, 0:64], pv[:, :, 0, :], [("ps", b)], [("Ve", T)])
                        S.copy("scalar", Vo[:, T, :, 64:128], pv[:, :, 1, :], [("ps", b)], [("Vo", T)])

                    if cut == 5:
                        S.flush()
                        return
                    nk = 4 * (g + 1)
                    for i in range(4):
                        hA, hB = 2 * i, 2 * i + 1
                        if g + 1 < ng:
                            stageM(g + 1, i)
                        steps = [(j, hh) for j in range(nk) for hh in (0, 1)]
                        pend = None
                        for n, (j, hh) in enumerate(steps):
                            h = hA if hh == 0 else hB
                            r = j - 4 * g
                            c0 = 128 * r if r > 0 else 0
                            gj = j // 4
                            b = pj()
                            slot = n % 4
                            S.mm(ps[b][:, c0:512], kT[0:96, h, j * 128:(j + 1) * 128], qT[0:96, h, c0:512], True, True,
                                 [("kTn", h, gj), ("kTr", h, gj), ("qTn", h), ("qTr", h)], [("ps", b)])
                            S.act(pT[slot][:, c0:512], ps[b][:, c0:512], AF.Exp, [("ps", b)], [("pT", slot)], scale=SCALE)
                            if r >= 0:
                                S.tt("vector", pT[slot][:, c0:c0 + 128], pT[slot][:, c0:c0 + 128], tri[:, 0, :], ALU.mult,
                                     [("pT", slot), "tri"], [("pT", slot)])
                            if pend is not None:
                                pend()
                            ob = 5 + hh
                            if hh == 0:
                                lhsT = Ve[:, j, i, :]
                                orow = slice(0, 128)
                                vr = [("Ve", j), "Vones"]
                            else:
                                lhsT = Vo[:, j, i, :]
                                orow = slice(0, 128)
                                vr = [("Vo", j), "Vones"]

                            def pend(ob=ob, orow=orow, c0=c0, lhsT=lhsT, slot=slot, j=j, vr=vr):
                                S.mm(ps[ob][orow, c0:512], lhsT, pT[slot][:, c0:512], j == 0, j == nk - 1,
                                     vr + [("pT", slot)], [("ps", ob)])
                        pend()
                        if cut == 6:
                            S.flush()
                            return
                        S.act(scr[64:128, 0, :], ps[5][64:128, :], AF.Ln, [("ps", 5)], [("scr", 0, "a")])
                        S.act(scr[64:128, 0, :], scr[64:128, 0, :], AF.Exp, [("scr", 0, "a")], [("scr", 0, "a")], scale=-1.0)
                        S.act(scr[0:64, 0, :], ps[6][0:64, :], AF.Ln, [("ps", 6)], [("scr", 0, "b")])
                        S.act(scr[0:64, 0, :], scr[0:64, 0, :], AF.Exp, [("scr", 0, "b")], [("scr", 0, "b")], scale=-1.0)
                        S.copy("vector", scr[0:64, 2, :], scr[64:128, 0, :], [("scr", 0, "a")], [("scr", 2)])
                        S.copy("vector", scr[64:128, 2, :], scr[0:64, 0, :], [("scr", 0, "b")], [("scr", 2)])
                        S.tt("vector", scr[:, 1, :], scr[:, 2, :], sg[:, i, :], ALU.mult, [("scr", 2), ("sg", i)], [("scr", 1)])
                        S.tt("vector", uaT[0:64, i, c0g:c0g + 512], ps[5][0:64, :], scr[0:64, 1, :], ALU.mult,
                             [("ps", 5), ("scr", 1)], [("uaT", i, g)])
                        S.tt("vector", uaT[64:128, i, c0g:c0g + 512], ps[6][64:128, :], scr[64:128, 1, :], ALU.mult,
                             [("ps", 6), ("scr", 1)], [("uaT", i, g)])
                if debug:
                    S.dma("sync", dbg_ua[s][:, :, 0:ng * 512], uaT[:, :, 0:ng * 512], [("uaT", i, g) for i in range(4) for g in range(ng)], ["dbg_ua"], "dbg")
                S.flush()

        def pass_G(s, pre):
            with contextlib.ExitStack() as st_:
                wG = sb(st_, "wG", [128, 8, 1536], BF16)
                wgl = sb(st_, "wgl", [128, 8, 16], BF16)
                wg17 = sb(st_, "wg17", [128, 256], BF16)
                gin_bc = sb(st_, "gin_bcG", [128, D], F32)
                ggla_bc = sb(st_, "ggla_bc", [128, 128], F32)
                xt = [sb(st_, "xtG%d" % i, [128, D], F32) for i in range(2)]
                hn = [sb(st_, "hnG%d" % i, [128, D], BF16) for i in range(2)]
                hTs = [sb(st_, "hTG%d" % i, [128, 8, 512], BF16) for i in range(2)]
                junk_t = sb(st_, "junkG", [128, D], BF16)
                gqf = sb(st_, "gqf", [128, 4, 512], F32)
                gkf = sb(st_, "gkf", [128, 4, 512], F32)
                sgg = sb(st_, "sgg", [128, 4, 512], BF16)
                gkl = sb(st_, "gkl", [128, 512], BF16)
                vsb = [sb(st_, "vsb%d" % i, [128, 512], BF16) for i in range(2)]
                etmp = sb(st_, "etmp", [128, 256], F32)
                sp = sb(st_, "sp", [128, 256], F32)
                ebT = sb(st_, "ebT", [128, 4, 128], F32)
                enbT = sb(st_, "enbT", [128, 4, 128], F32)
                erev = sb(st_, "erev", [128, 256], F32)
                qtT = sb(st_, "qtT", [128, 4, 128], BF16)
                ktT = sb(st_, "ktT", [128, 4, 128], BF16)
                kdec = sb(st_, "kdec", [128, 256], BF16)
                AT = sb(st_, "AT", [128, 4, 128], BF16)
                onsb = sb(st_, "onsb", [128, 512], BF16)
                Sf = sb(st_, "Sf", [128, 4, 128], F32)
                Sb = sb(st_, "Sb", [128, 4, 128], BF16)
                junk2 = sb(st_, "junk2", [128, 128], BF16)
                stt_ = sb(st_, "stG", [128, 40], F32)
                ctx = dict(xt=xt, hn=hn, st=stt_, junk=junk_t[:], hT=None, gin_bc=gin_bc)
                r0_ = slice(0, 64)

                def stageG(gn, t, parts="LST"):
                    stage_hT(ctx, s * SEQ + gn * 512 + t * 128, t % 2, t * 128, "scalar" if t % 2 else "vector",
                             hT=hTs[gn % 2], htag=("hT", gn % 2), parts=parts)

                wi = kchunks(w_in)
                load_w("wG_a", wG[:, :, 0:1024], wi[:, :, 1184:2208])
                load_w("wG_b", wG[:, :, 1024:1536], wi[:, :, 2224:2736])
                load_w("wgl", wgl[:], wi[:, :, 2208:2224])
                load_w("wg17", wg17[0:17, :], w_gk17)
                load_w("wP", pre[0][:], wi[:, :, 2736:4784])
                load_w("wo", pre[1][:], kchunks(w_out))
                load_w("wpg", pre[2][:], kchunks(w_pg))
                S.dma("sync", gin_bc[:], g_in.partition_broadcast(128), [], ["gin_bc"], "gin_bc")
                S.dma("sync", ggla_bc[:], g_gla.partition_broadcast(128), [], ["ggla_bc"], "ggla_bc")
                S.memset("vector", gkl[:], 1.0, ["gkl"])
                S.memset("vector", Sf[:], 0.0, ["Sf"])
                S.memset("vector", Sb[:], 0.0, ["Sb"])
                WG = ["wG_a", "wG_b"]
                pj_i = [0]

                def pj():
                    b = (1, 2, 3, 4, 5, 6)[pj_i[0] % 6]
                    pj_i[0] += 1
                    return b

                for t in range(4):
                    stageG(0, t)
                for g in range(ng):
                    tok0 = s * SEQ + g * 512
                    c0g = g * 512
                    hT = hTs[g % 2]
                    HT = [("hT", g % 2, c0) for c0 in (0, 128, 256, 384)]

                    def proj(col, width=128, w=wG, wn=WG):
                        b = pj()
                        for k in range(8):
                            S.mm(ps[b][0:width, :], w[:, k, col:col + width], hT[:, k, :], k == 0, k == 7,
                                 wn + HT, [("ps", b)])
                        return b

                    for c in range(2):
                        b = proj(c * 128)
                        S.copy("scalar", gqf[r0_, 2 * c, :], ps[b][0:64, :], [("ps", b)], [("gqf", 2 * c)])
                        S.copy("vector", gqf[r0_, 2 * c + 1, :], ps[b][64:128, :], [("ps", b)], [("gqf", 2 * c + 1)])
                    for c in range(2):
                        b = proj(256 + c * 128)
                        S.copy("scalar", gkf[r0_, 2 * c, :], ps[b][0:64, :], [("ps", b)], [("gkf", 2 * c)])
                        S.copy("vector", gkf[r0_, 2 * c + 1, :], ps[b][64:128, :], [("ps", b)], [("gkf", 2 * c + 1)])
                    GQ = [("gqf", h) for h in range(4)]
                    GK = [("gkf", h) for h in range(4)]
                    for c in range(4):
                        b = proj(1024 + c * 128)
                        S.act(sgg[:, c, :], ps[b][:], AF.Silu, [("ps", b)], [("sgg", c)])
                    b = proj(0, 16, wgl, ["wgl"])
                    S.copy("vector", gkl[0:16, :], ps[b][0:16, :], [("ps", b)], ["gkl"])

                    def F(t):
                        cs = slice(t * 128, (t + 1) * 128)
                        vs = t % 2
                        htr = [("hT", g % 2, t * 128)]
                        for k in range(8):
                            S.mm(ps[3][:], hT[:, k, cs], wG[:, k, 512:1024], k == 0, k == 7, WG + htr, [("ps", 3)])
                        S.copy("scalar", vsb[vs][:], ps[3][:], [("ps", 3)], [("vsb", vs)])
                        for k in range(8):
                            S.mm(ps[4][:, 0:256], hT[:, k, cs], wG[:, k, 256:512], k == 0, k == 7, WG + htr, [("ps", 4)])
                        S.mm(ps[4][:, 256:512], gkl[0:17, cs], wg17[0:17, :], True, True, ["gkl", "wg17"], [("ps", 4)])
                        S.act(etmp[:], ps[4][:, 256:512], AF.Exp, [("ps", 4)], ["etmp"], scale=-1.0)
                        S.act(sp[:], etmp[:], AF.Ln, ["etmp"], ["sp"], bias=1.0)
                        for h in range(4):
                            S.mm(ps[5][0:64, h * 128:(h + 1) * 128], sp[:, h * 64:(h + 1) * 64], Lmat, True, True,
                                 ["sp", "cst"], [("ps", 5)])
                        S.mm(ps[2][:, 0:256], Umat, sp[:], True, True, ["sp", "cst"], [("ps", 2)])
                        bt = ps[5][0:64, :].rearrange("p (h t) -> p h t", h=4)
                        S.act(ebT[r0_], bt, AF.Exp, [("ps", 5)], ["ebT"])
                        S.act(enbT[r0_], bt, AF.Exp, [("ps", 5)], ["enbT"], scale=-1.0)
                        S.act(erev[:], ps[2][:, 0:256], AF.Exp, [("ps", 2)], ["erev"])
                        S.stt("vector", qtT[r0_], gqf[r0_, :, cs], 0.125, ebT[r0_], ALU.mult, ALU.mult, GQ + ["ebT"], ["qtT"])
                        S.tt("vector", ktT[r0_], gkf[r0_, :, cs], enbT[r0_], ALU.mult, GK + ["enbT"], ["ktT"])
                        S.tt("vector", kdec[:], ps[4][:, 0:256], erev[:], ALU.mult, [("ps", 4), "erev"], ["kdec"])
                        for h in range(4):
                            S.mm(ps[6][:, h * 128:(h + 1) * 128], ktT[r0_, h, :], qtT[r0_, h, :], True, True,
                                 ["ktT", "qtT"], [("ps", 6)])
                        S.tt("vector", AT[:], ps[6][:].rearrange("p (h t) -> p h t", h=4), tri[:], ALU.mult,
                             [("ps", 6), "tri"], ["AT"])
                    def O(t):
                        cs = slice(t * 128, (t + 1) * 128)
                        vs = t % 2
                        for h in range(4):
                            hs_ = slice(h * 128, (h + 1) * 128)
                            S.mm(ps[7][:, hs_], AT[:, h, :], vsb[vs][:, hs_], True, False, ["AT", ("vsb", vs)], [("ps", 7)])
                            S.mm(ps[7][:, hs_], qtT[r0_, h, :], Sb[r0_, h, :], False, True, ["qtT", "Sb"], [("ps", 7)])
                        for h in range(4):
                            hs_ = slice(h * 128, (h + 1) * 128)
                            S.mm(ps[1][0:64, hs_], kdec[:, h * 64:(h + 1) * 64], vsb[vs][:, hs_], True, True,
                                 ["kdec", ("vsb", vs)], [("ps", 1)])
                        for h in range(4):
                            hs_ = slice(h * 128, (h + 1) * 128)
                            S.stt("vector", Sf[r0_, h, :], Sf[r0_, h, :], ebT[r0_, h, 127:128], ps[1][0:64, hs_], ALU.mult, ALU.add,
                                  ["Sf", "ebT", ("ps", 1)], ["Sf"])
                        S.copy("scalar", Sb[r0_], Sf[r0_], ["Sf"], ["Sb"])
                    def N(t):
                        cs = slice(t * 128, (t + 1) * 128)
                        vs = t % 2
                        for h in range(4):
                            S.act(junk2[:], ps[7][:, h * 128:(h + 1) * 128], AF.Square, [("ps", 7)], ["junk2", "oss"],
                                  accum=stt_[:, 24 + h:25 + h])
                        S.act(stt_[:, 28:32], stt_[:, 24:28], AF.Ln, ["oss"], ["oln"], scale=1.0 / 128, bias=EPS)
                        S.act(stt_[:, 32:36], stt_[:, 28:32], AF.Exp, ["oln"], ["ors"], scale=-0.5)
                        for h in range(4):
                            hs_ = slice(h * 128, (h + 1) * 128)
                            S.stt("vector", onsb[:, hs_], ps[7][:, hs_], stt_[:, 32 + h:33 + h], ggla_bc[:], ALU.mult, ALU.mult,
                                  [("ps", 7), "ors", "ggla_bc"], ["onsb"])
                        for h in range(4):
                            hs_ = slice(h * 128, (h + 1) * 128)
                            S.tr(tpv[:, hs_], onsb[:, hs_], ident[:], ["onsb", "ident"], [("ps", 0)])
                        S.tt("vector", ubT[:, :, c0g + t * 128:c0g + (t + 1) * 128],
                             tpv[:, 0:512].rearrange("p (h t) -> p h t", h=4), sgg[:, :, cs], ALU.mult,
                             [("ps", 0)] + [("sgg", c) for c in range(4)], [("ubT", g, t)])
                    F(0)
                    for t in range(4):
                        if g + 1 < ng:
                            stageG(g + 1, t, "L")
                        O(t)
                        if t + 1 < 4:
                            F(t + 1)
                        N(t)
                        if g + 1 < ng:
                            stageG(g + 1, t, "ST")
                if debug:
                    S.dma("sync", dbg_ub[s][:, :, 0:ng * 512], ubT[:, :, 0:ng * 512], [("ubT", g, t) for g in range(ng) for t in range(4)], ["dbg_ub"], "dbg")
                S.flush()

        def pass_P(s, pre):
            wP, wo, wpg = pre
            with contextlib.ExitStack() as st_:
                wml = sb(st_, "wml", [128, 4, D], BF16)
                wgl_ = sb(st_, "wglb", [128, 4, D], BF16)
                wpl = sb(st_, "wpl", [128, 2, D], BF16)
                gin_bc = sb(st_, "gin_bcP", [128, D], F32)
                gpg_bc = sb(st_, "gpg_bc", [128, D], F32)
                gple_bc = sb(st_, "gple_bc", [128, D], F32)
                gfin_bc = sb(st_, "gfin_bc", [128, D], F32)
                NX = 6
                xt = [sb(st_, "xtP%d" % i, [128, D], F32) for i in range(NX)]
                hn = [sb(st_, "hnP%d" % i, [128, D], BF16) for i in range(1)]
                hT = sb(st_, "hTP", [128, 8, 512], BF16)
                junk_t = sb(st_, "junkP", [128, D], BF16)
                sa = [sb(st_, "sa%d" % i, [128, 512], BF16) for i in range(2)]
                sbb = [sb(st_, "sbb%d" % i, [128, 512], BF16) for i in range(2)]
                t1 = [sb(st_, "t1_%d" % i, [128, 512], BF16) for i in range(2)]
                t2 = [sb(st_, "t2_%d" % i, [128, 512], BF16) for i in range(2)]
                mg = sb(st_, "mg", [128, 8, 512], BF16)
                x1n = sb(st_, "x1n", [128, D], BF16)
                x1nT = sb(st_, "x1nT", [128, 8, 128], BF16)
                pbf = [sb(st_, "pbf%d" % i, [128, 256], BF16) for i in range(2)]
                pT_ = sb(st_, "pTP", [128, 2, 128], BF16)
                sig = sb(st_, "sig", [128, 512], F32)
                et = sb(st_, "et", [128, D], F32)
                ot = [sb(st_, "ot%d" % i, [128, D], F32) for i in range(2)]
                stt_ = sb(st_, "stP", [128, 48], F32)
                ctx = dict(xt=xt, hn=hn, st=stt_, junk=junk_t[:], hT=hT, gin_bc=gin_bc)

                load_w("wml", wml[:], kchunks(w_mla))
                load_w("wglb", wgl_[:], kchunks(w_gla))
                load_w("wpl", wpl[:], kchunks(w_ple))
                S.dma("sync", gin_bc[:], g_in.partition_broadcast(128), [], ["gin_bc"], "gin_bc")
                S.dma("sync", gpg_bc[:], g_pg.partition_broadcast(128), [], ["gpg_bc"], "gpg_bc")
                S.dma("sync", gple_bc[:], g_ple.partition_broadcast(128), [], ["gple_bc"], "gple_bc")
                S.dma("sync", gfin_bc[:], g_fin.partition_broadcast(128), [], ["gfin_bc"], "gfin_bc")
                pj_i = [0]
                rot = [1, 2, 3, 4, 5]

                def pj():
                    b = rot[pj_i[0] % 5]
                    pj_i[0] += 1
                    return b

                HT = [("hT", c0) for c0 in (0, 128, 256, 384)]

                def stageP(gn, t, parts="LST"):
                    stage_hT(ctx, s * SEQ + gn * 512 + t * 128, (4 * gn + t) % NX, t * 128, "vector", parts=parts)

                for t in range(4):
                    stageP(0, t)
                for g in range(ng):
                    tok0 = s * SEQ + g * 512
                    c0g = g * 512
                    for m in range(8):
                        sl = m % 2
                        ba = pj()
                        for k in range(8):
                            S.mm(ps[ba][:], wP[:, k, m * 128:(m + 1) * 128], hT[:, k, :], k == 0, k == 7, ["wP"] + HT, [("ps", ba)])
                        S.act(sa[sl][:], ps[ba][:], AF.Sigmoid, [("ps", ba)], [("sa", sl)])
                        by = pj()
                        for c in range(4):
                            S.mm(ps[by][:], wml[:, c, m * 128:(m + 1) * 128], uaT[:, c, c0g:c0g + 512], c == 0, c == 3, ["wml"], [("ps", by)])
                        S.tt("vector", t1[sl][:], ps[by][:], sa[sl][:], ALU.mult, [("ps", by), ("sa", sl)], [("t1", sl)])
                        bb = pj()
                        for k in range(8):
                            S.mm(ps[bb][:], wP[:, k, 1024 + m * 128:1024 + (m + 1) * 128], hT[:, k, :], k == 0, k == 7, ["wP"] + HT, [("ps", bb)])
                        S.act(sbb[sl][:], ps[bb][:], AF.Sigmoid, [("ps", bb)], [("sbb", sl)])
                        bz = pj()
                        for c in range(4):
                            S.mm(ps[bz][:], wgl_[:, c, m * 128:(m + 1) * 128], ubT[:, c, c0g:c0g + 512], c == 0, c == 3, ["wglb"], [("ps", bz)])
                        S.tt("vector", t2[sl][:], ps[bz][:], sbb[sl][:], ALU.mult, [("ps", bz), ("sbb", sl)], [("t2", sl)])
                        S.tt("gpsimd", mg[:, m, :], t1[sl][:], t2[sl][:], ALU.add, [("t1", sl), ("t2", sl)], [("mg", m)])
                    MG = [("mg", m) for m in range(8)]

                    def xs_of(t):
                        return (4 * g + t) % NX

                    def A1(t):
                        xs = xs_of(t)
                        cs = slice(t * 128, (t + 1) * 128)
                        for half in range(2):
                            b = 4 + half
                            hs_ = slice(half * 512, (half + 1) * 512)
                            for m in range(8):
                                S.mm(ps[b][:], mg[:, m, cs], wo[:, m, hs_], m == 0, m == 7, ["wo"] + MG, [("ps", b)])
                            S.tt("vector", xt[xs][:, hs_], xt[xs][:, hs_], ps[b][:], ALU.add, [("xt", xs), ("ps", b)], [("xt", xs)])
                        S.act(junk_t[:], xt[xs][:], AF.Square, [("xt", xs)], ["junk", "s1"], accum=stt_[:, 24:25])
                        S.act(stt_[:, 25:26], stt_[:, 24:25], AF.Ln, ["s1"], ["l1"], scale=1.0 / D, bias=EPS)
                        S.act(stt_[:, 26:27], stt_[:, 25:26], AF.Exp, ["l1"], ["r1"], scale=-0.5)

                    def A2(t):
                        xs = xs_of(t)
                        S.stt("vector", x1n[:], xt[xs][:], stt_[:, 26:27], gpg_bc[:], ALU.mult, ALU.mult, [("xt", xs), "r1", "gpg_bc"], ["x1n"])

                    def B(t):
                        tok = tok0 + t * 128
                        ps_ = t % 2
                        S.dma("gpsimd", pbf[ps_][:], pin[tok:tok + 128, :], [], [("pbf", ps_)], ("pbf", ps_))
                        for c in range(2):
                            S.tr(tpv[:, c * 128:(c + 1) * 128], pbf[ps_][:, c * 128:(c + 1) * 128], ident[:], [("pbf", ps_), "ident"], [("ps", 0)])
                        S.copy("vector", pT_[:], tpv[:, 0:256].rearrange("p (c t) -> p c t", c=2), [("ps", 0)], ["pTP"])
                        for half in range(2):
                            b = 1 + half
                            for c in range(2):
                                S.mm(ps[b][:], pT_[:, c, :], wpl[:, c, half * 512:(half + 1) * 512], c == 0, c == 1, ["wpl", "pTP"], [("ps", b)])
                            S.act(junk_t[:, 0:512], ps[b][:], AF.Square, [("ps", b)], ["junk", ("se", half)], accum=stt_[:, 27 + half:28 + half])
                        S.tt("vector", stt_[:, 29:30], stt_[:, 27:28], stt_[:, 28:29], ALU.add, [("se", 0), ("se", 1)], ["se2"])
                        S.act(stt_[:, 30:31], stt_[:, 29:30], AF.Ln, ["se2"], ["le"], scale=1.0 / D, bias=EPS)
                        S.act(stt_[:, 31:32], stt_[:, 30:31], AF.Exp, ["le"], ["re"], scale=-0.5)
                        for half in range(2):
                            b = 1 + half
                            hs_ = slice(half * 512, (half + 1) * 512)
                            S.stt("vector", et[:, hs_], ps[b][:], stt_[:, 31:32], gple_bc[:, hs_], ALU.mult, ALU.mult,
                                  [("ps", b), "re", "gple_bc"], [("et", half)])

                    def C1(t):
                        for c in range(8):
                            S.tr(tpv[:, c * 128:(c + 1) * 128], x1n[:, c * 128:(c + 1) * 128], ident[:], ["x1n", "ident"], [("ps", 0)])
                        S.copy("scalar", x1nT[:], tpv.rearrange("p (c t) -> p c t", c=8), [("ps", 0)], ["x1nT"])

                    def C2a(t):
                        for half in range(2):
                            b = 6 + half
                            hs_ = slice(half * 512, (half + 1) * 512)
                            for c in range(8):
                                S.mm(ps[b][:], x1nT[:, c, :], wpg[:, c, hs_], c == 0, c == 7, ["wpg", "x1nT"], [("ps", b)])

                    def C2b(t):
                        tok = tok0 + t * 128
                        xs = xs_of(t)
                        osl = t % 2
                        for half in range(2):
                            b = 6 + half
                            hs_ = slice(half * 512, (half + 1) * 512)
                            S.act(sig[:], ps[b][:], AF.Sigmoid, [("ps", b)], ["sig"])
                            S.tt("vector", et[:, hs_], et[:, hs_], sig[:], ALU.mult, [("et", half), "sig"], [("et", half)])
                        S.tt("vector", xt[xs][:], xt[xs][:], et[:], ALU.add, [("xt", xs), ("et", 0), ("et", 1)], [("xt", xs)])
                        S.act(junk_t[:], xt[xs][:], AF.Square, [("xt", xs)], ["junk", "s2"], accum=stt_[:, 32:33])
                        S.act(stt_[:, 33:34], stt_[:, 32:33], AF.Ln, ["s2"], ["l2"], scale=1.0 / D, bias=EPS)
                        S.act(stt_[:, 34:35], stt_[:, 33:34], AF.Exp, ["l2"], ["r2"], scale=-0.5)
                        S.stt("vector", ot[osl][:], xt[xs][:], stt_[:, 34:35], gfin_bc[:], ALU.mult, ALU.mult,
                              [("xt", xs), "r2", "gfin_bc"], [("ot", osl)])
                        S.dma("sync", out[tok:tok + 128, :], ot[osl][:], [("ot", osl)], [("out", tok)], ("ot", osl))

                    nxt = g + 1 < ng
                    A1(0)
                    A2(0)
                    for t in range(4):
                        if nxt:
                            if t == 0:
                                stageP(g + 1, 0, "L")
                            if t + 1 < 4:
                                stageP(g + 1, t + 1, "L")
                        C1(t)
                        if t + 1 < 4:
                            A1(t + 1)
                            A2(t + 1)
                        B(t)
                        if nxt:
                            stageP(g + 1, t, "S")
                        C2a(t)
                        if nxt:
                            stageP(g + 1, t, "T")
                        C2b(t)
                S.flush()

        for s in range(nseq):
            if "M" in passes:
                pass_M(s)
            with contextlib.ExitStack() as pw:
                pre = (sb(pw, "wP", [128, 8, 2048], BF16), sb(pw, "wo", [128, 8, D], BF16), sb(pw, "wpg", [128, 8, D], BF16))
                if "G" in passes:
                    pass_G(s, pre)
                if "P" in passes:
                    pass_P(s, pre)
        if S.ops:
            S.flush()
    return nc


def _consts():
    c = np.zeros((128, C_END), np.float32)
    idx = np.arange(128)
    c[:, C_ID:C_ID + 128] = np.eye(128, dtype=np.float32)
    triu = (idx[None, :] >= idx[:, None]).astype(np.float32)
    c[:, C_TRI:C_TRI + 128] = triu
    c[:, C_L:C_L + 128] = -triu / 16.0
    c[:, C_U:C_U + 128] = -(idx[:, None] > idx[None, :]).astype(np.float32) / 16.0
    c[64, C_SEL:C_SEL + 64] = 1.0
    c[0, C_SEL + 64:C_SEL + 128] = 1.0
    inv_freq = 1.0 / (10000.0 ** (np.arange(0, 32, 2, dtype=np.float64) / 32.0))
    for r in range(32):
        c[64 + r, C_ROPE] = inv_freq[r % 16] / (2.0 * np.pi)
        c[64 + r, C_ROPE + 1] = -TWO_PI if r < 16 else TWO_PI
    return c


_NC_CACHE = {}


def _prep_shared(inp):
    f = lambda a: np.ascontiguousarray(np.asarray(a, dtype=np.float32))
    w_in = f(inp["w_in"][0])
    kr = w_in[:, 640:672]
    w_krz = np.zeros((D, 192), np.float32)
    w_krz[:, 64:96] = kr
    w_krz[:, 96 + 64:96 + 80] = kr[:, 16:32]
    w_krz[:, 96 + 80:96 + 96] = kr[:, 0:16]
    w_uq = f(inp["w_uq"][0])
    w_uqsw = np.zeros((384, 768), np.float32)
    for h in range(8):
        rp = w_uq[:, 96 * h + 64:96 * h + 96]
        w_uqsw[:, 96 * h + 64:96 * h + 80] = rp[:, 16:32]
        w_uqsw[:, 96 * h + 80:96 * h + 96] = rp[:, 0:16]
    w_ukv = f(inp["w_ukv"][0]).reshape(256, 8, 2, 64)
    w_ukvk = np.ascontiguousarray(w_ukv[:, :, 0, :].reshape(256, 512))
    w_ukvv = np.ascontiguousarray(w_ukv[:, :, 1, :].reshape(256, 512))
    w_gk17 = np.concatenate([f(inp["w_gk_up"][0]), f(inp["b_gk"][0]).reshape(1, 256)], axis=0)
    gcols = np.zeros((128, 8), np.float32)
    gcols[:, 0:3] = f(inp["q_norm_g"][0]).reshape(3, 128).T
    gcols[:, 3:5] = f(inp["kv_norm_g"][0]).reshape(2, 128).T
    return {
        "w_in": w_in, "w_krz": w_krz, "w_uq": w_uq, "w_uqsw": w_uqsw, "w_ukvk": w_ukvk, "w_ukvv": w_ukvv,
        "w_gk17": np.ascontiguousarray(w_gk17), "w_mla": f(inp["w_mla_br"][0]), "w_gla": f(inp["w_gla_br"][0]),
        "w_out": f(inp["w_out"][0]), "w_ple": f(inp["w_ple"][0]), "w_pg": f(inp["w_ple_gate"][0]),
        "g_in": f(inp["norm_in_g"][0]).reshape(1, D), "g_gla": f(inp["gla_norm_g"][0]).reshape(1, 128),
        "g_ple": f(inp["ple_norm_g"][0]).reshape(1, D), "g_pg": f(inp["ple_gate_norm_g"][0]).reshape(1, D),
        "g_fin": f(inp["final_norm_g"]).reshape(1, D), "gcols": gcols, "consts": _consts(),
    }


def kernel(**inputs):
    if "nc" not in _NC_CACHE:
        _NC_CACHE["nc"] = build(False)
    nc = _NC_CACHE["nc"]
    shared = _prep_shared(inputs)
    x = np.asarray(inputs["x"], dtype=np.float32)
    p = np.asarray(inputs["p"], dtype=np.float32)[0]
    pos = np.asarray(inputs["positions"], dtype=np.int32)
    in_maps = []
    for c in range(NCORES):
        m = dict(shared)
        m["x"] = np.ascontiguousarray(x[2 * c:2 * c + 2].reshape(2 * SEQ, D))
        m["p"] = np.ascontiguousarray(p[2 * c:2 * c + 2].reshape(2 * SEQ, 256))
        m["pos"] = np.ascontiguousarray(pos[2 * c:2 * c + 2].reshape(1, 2 * SEQ))
        in_maps.append(m)
    res = run_bass_kernel_spmd(nc, in_maps, core_ids=list(range(NCORES)))
    outs = [np.asarray(r["out"]).reshape(2, SEQ, D) for r in res.results]
    return np.concatenate(outs, axis=0).astype(np.float32)
```

```python
import contextlib
import numpy as np
import concourse.bass as bass
import concourse.mybir as mybir
from concourse.bass_utils import run_bass_kernel_spmd

F32 = mybir.dt.float32
BF16 = mybir.dt.bfloat16
I32 = mybir.dt.int32
AF = mybir.ActivationFunctionType
ALU = mybir.AluOpType

NCORES = 8
SEQ = 2048
D = 1024
EPS = 1e-6
TWO_PI = float(2.0 * np.pi * (1.0 - 2e-7))
ENGINES = ("sync", "scalar", "vector", "gpsimd", "tensor")

C_ID, C_TRI, C_L, C_U, C_SEL, C_ROPE, C_END = 0, 128, 256, 384, 512, 640, 644


class _Op:
    __slots__ = ("eng", "fn", "dma", "waits", "inc", "idx", "key", "pos")


class Sched:
    def __init__(self, nc, stack):
        self.nc = nc
        self.stack = stack
        self.sems = {}
        self.counts = {}
        self.seen = {e: {} for e in ENGINES}
        self._reset()

    def _reset(self):
        self.ops = []
        self.last_w = {}
        self.readers = {}

    def add(self, eng, fn, R=(), W=(), dma=False, stream=None):
        R = tuple(R) + tuple((r[0], r[1], h) for r in R if r == ("scr", 0) for h in ("a", "b"))
        W = tuple(W) + tuple((r[0], r[1], h) for r in W if r == ("scr", 0) for h in ("a", "b"))
        op = _Op()
        op.eng, op.fn, op.dma, op.inc, op.idx = eng, fn, dma, False, 0
        op.key = ("dma", stream) if dma else ("eng", eng)
        op.pos = len(self.ops)
        best = {}
        W = tuple(W) + tuple(r for r in R if isinstance(r, tuple) and r[0] == "ps" and r not in W)

        def dep(d, kind):
            if d is None or d is op:
                return
            if (not d.dma) and d.eng == eng and not dma:
                if eng == "tensor":
                    return
            cur = best.get(d.key)
            if cur is None or d.pos > cur.pos:
                best[d.key] = d

        for r in R:
            dep(self.last_w.get(r), "raw")
        for r in W:
            dep(self.last_w.get(r), "waw")
            for rd in self.readers.get(r, ()):
                dep(rd, "war")
        op.waits = list(best.values())
        for d in op.waits:
            d.inc = True
        for r in W:
            self.last_w[r] = op
            self.readers[r] = []
        for r in R:
            lst = self.readers.setdefault(r, [])
            for i, o in enumerate(lst):
                if o.key == op.key:
                    lst[i] = op
                    break
            else:
                lst.append(op)
        self.ops.append(op)
        return op

    def flush(self):
        nc = self.nc
        per_eng = {e: [] for e in ENGINES}
        for op in self.ops:
            per_eng[op.eng].append(op)
        for e in ENGINES:
            for op in reversed(per_eng[e]):
                if not op.dma:
                    op.inc = True
                    break
        for op in self.ops:
            if op.dma:
                op.inc = True
        for op in self.ops:
            if op.inc:
                k = op.key
                if k not in self.sems:
                    self.sems[k] = self.stack.enter_context(nc.semaphore("s%d" % len(self.sems)))
                    self.counts[k] = 0
                self.counts[k] += 16 if op.dma else 1
                op.idx = self.counts[k]
        finals = dict(self.counts)
        sems, seen_all = self.sems, self.seen

        def make(engname):
            def body(eng):
                seen = seen_all[engname]
                for op in per_eng[engname]:
                    for d in op.waits:
                        if seen.get(d.key, 0) < d.idx:
                            eng.wait_ge(sems[d.key], d.idx)
                            seen[d.key] = d.idx
                    ins = op.fn(eng)
                    if op.inc:
                        ins.then_inc(sems[op.key], 16 if op.dma else 1)
                for k, v in finals.items():
                    if seen.get(k, 0) < v:
                        eng.wait_ge(sems[k], v)
                        seen[k] = v
            return body

        with nc.Block() as block:
            block.sync(make("sync"))
            block.scalar(make("scalar"))
            block.vector(make("vector"))
            block.gpsimd(make("gpsimd"))
            block.tensor(make("tensor"))
        self._reset()

    def dma(self, eng, out, in_, R, W, stream):
        return self.add(eng, lambda e: e.dma_start(out=out, in_=in_), R, W, dma=True, stream=stream)

    def mm(self, out, lhsT, rhs, start, stop, R, W, tp=None):
        if tp is not None:
            return self.add("tensor", lambda e: e.matmul(out, lhsT=lhsT, rhs=rhs, start=start, stop=stop, tile_position=tp), R, W)
        return self.add("tensor", lambda e: e.matmul(out, lhsT=lhsT, rhs=rhs, start=start, stop=stop), R, W)

    def tr(self, out, in_, ident, R, W):
        return self.add("tensor", lambda e: e.transpose(out=out, in_=in_, identity=ident), R, W)

    def act(self, out, in_, func, R, W, scale=None, bias=None, accum=None):
        kw = {}
        if scale is not None:
            kw["scale"] = scale
        if bias is not None:
            kw["bias"] = bias
        if accum is not None:
            kw["accum_out"] = accum
        return self.add("scalar", lambda e: e.activation(out=out, in_=in_, func=func, **kw), R, W)

    def copy(self, eng, out, in_, R, W):
        if eng == "scalar":
            return self.add(eng, lambda e: e.copy(out=out, in_=in_), R, W)
        return self.add(eng, lambda e: e.tensor_copy(out=out, in_=in_), R, W)

    def tt(self, eng, out, in0, in1, op, R, W):
        return self.add(eng, lambda e: e.tensor_tensor(out=out, in0=in0, in1=in1, op=op), R, W)

    def ts(self, eng, out, in0, s1, op0, R, W, s2=None, op1=None):
        if op1 is None:
            return self.add(eng, lambda e: e.tensor_scalar(out=out, in0=in0, scalar1=s1, scalar2=None, op0=op0), R, W)
        return self.add(eng, lambda e: e.tensor_scalar(out=out, in0=in0, scalar1=s1, scalar2=s2, op0=op0, op1=op1), R, W)

    def stt(self, eng, out, in0, scalar, in1, op0, op1, R, W):
        return self.add(eng, lambda e: e.scalar_tensor_tensor(out=out, in0=in0, scalar=scalar, in1=in1, op0=op0, op1=op1), R, W)

    def memset(self, eng, ap, val, W):
        return self.add(eng, lambda e: e.memset(ap, val), (), W)

    def recip(self, out, in_, R, W):
        return self.add("vector", lambda e: e.reciprocal(out=out, in_=in_), R, W)


def build(debug=False, nseq=2, ng=4, passes="MGP", cut=99):
    nc = bass.Bass("TRN2", target_bir_lowering=False)

    def din(name, shape, dt=F32):
        return nc.dram_tensor(name, list(shape), dt, kind="ExternalInput").ap()

    x = din("x", [2 * SEQ, D])
    pin = din("p", [2 * SEQ, 256])
    pos = din("pos", [1, 2 * SEQ], I32)
    w_in = din("w_in", [D, 4784])
    w_krz = din("w_krz", [D, 192])
    w_uq = din("w_uq", [384, 768])
    w_uqsw = din("w_uqsw", [384, 768])
    w_ukvk = din("w_ukvk", [256, 512])
    w_ukvv = din("w_ukvv", [256, 512])
    w_gk17 = din("w_gk17", [17, 256])
    w_mla = din("w_mla", [512, D])
    w_gla = din("w_gla", [512, D])
    w_out = din("w_out", [D, D])
    w_ple = din("w_ple", [256, D])
    w_pg = din("w_pg", [D, D])
    g_in = din("g_in", [1, D])
    g_gla = din("g_gla", [1, 128])
    g_ple = din("g_ple", [1, D])
    g_pg = din("g_pg", [1, D])
    g_fin = din("g_fin", [1, D])
    gcols = din("gcols", [128, 8])
    consts = din("consts", [128, C_END])
    out = nc.dram_tensor("out", [2 * SEQ, D], F32, kind="ExternalOutput").ap()
    if debug:
        dbg_ua = nc.dram_tensor("dbg_ua", [2, 128, 4, SEQ], BF16, kind="ExternalOutput").ap()
        dbg_ub = nc.dram_tensor("dbg_ub", [2, 128, 4, SEQ], BF16, kind="ExternalOutput").ap()

    top = contextlib.ExitStack()
    with top:
        uid = [0]

        def sb(stack, name, shape, dt):
            uid[0] += 1
            return stack.enter_context(nc.sbuf_tensor("%s_%d" % (name, uid[0]), list(shape), dt))

        S = Sched(nc, top)
        cst = sb(top, "cst", [128, C_END], F32)
        gc = sb(top, "gc", [128, 8], F32)
        ident = sb(top, "ident", [128, 128], BF16)
        tri = sb(top, "tri", [128, 4, 128], BF16)
        ones_bf = sb(top, "ones_bf", [128, 128], BF16)
        uaT = sb(top, "uaT", [128, 4, SEQ], BF16)
        ubT = sb(top, "ubT", [128, 4, SEQ], BF16)
        ps = [top.enter_context(nc.psum_tensor("ps%d" % b, [128, 512], F32)) for b in range(8)]
        tpv = ps[0][:].bitcast(BF16)

        S.dma("sync", cst[:], consts, [], ["cst"], "cst")
        S.dma("sync", gc[:], gcols, [], ["gc"], "gc")
        S.copy("vector", ident[:], cst[:, C_ID:C_ID + 128], ["cst"], ["ident"])
        for h in range(4):
            S.copy("vector", tri[:, h, :], cst[:, C_TRI:C_TRI + 128], ["cst"], ["tri"])
        S.memset("vector", ones_bf[:], 1.0, ["ones_bf"])
        Lmat = cst[:, C_L:C_L + 128]
        Umat = cst[:, C_U:C_U + 128]

        def stage_hT(ctx, tok0, xs, col0, evac_eng, hT=None, htag=("hT",), parts="LST"):
            xt, hn, st, junk, gbc = ctx["xt"], ctx["hn"], ctx["st"], ctx["junk"], ctx["gin_bc"]
            if hT is None:
                hT = ctx["hT"]
            hs = xs % len(hn)
            if "L" in parts:
                S.dma("sync", xt[xs][:], x[tok0:tok0 + 128, :], [], [("xt", xs)], ("xt", xs))
            if "S" in parts:
                S.act(junk, xt[xs][:], AF.Square, [("xt", xs)], ["junk", ("ss", xs)], accum=st[:, xs:xs + 1])
                S.act(st[:, 8 + xs:9 + xs], st[:, xs:xs + 1], AF.Ln, [("ss", xs)], [("ln", xs)], scale=1.0 / D, bias=EPS)
                S.act(st[:, 16 + xs:17 + xs], st[:, 8 + xs:9 + xs], AF.Exp, [("ln", xs)], [("rs", xs)], scale=-0.5)
                S.stt("vector", hn[hs][:], xt[xs][:], st[:, 16 + xs:17 + xs], gbc[:], ALU.mult, ALU.mult,
                      [("xt", xs), ("rs", xs), "gin_bc"], [("hn", hs)])
            if "T" in parts:
                for c in range(8):
                    S.tr(tpv[:, c * 128:(c + 1) * 128], hn[hs][:, c * 128:(c + 1) * 128], ident[:],
                         [("hn", hs), "ident"], [("ps", 0)])
                S.copy(evac_eng, hT[:, :, col0:col0 + 128], tpv.rearrange("p (c t) -> p c t", c=8),
                       [("ps", 0)], [htag + (col0,)])

        def load_w(name, dst, src, R=()):
            S.dma("gpsimd", dst, src, list(R), [name], name)

        def kchunks(w):
            return w.rearrange("(c p) n -> p c n", p=128)

        def pass_M(s):
            with contextlib.ExitStack() as st_:
                wM = sb(st_, "wM", [128, 8, 1152], BF16)
                wkr = sb(st_, "wkr", [128, 8, 192], BF16)
                wuq = sb(st_, "wuq", [128, 3, 768], BF16)
                wuqs = sb(st_, "wuqs", [128, 3, 768], BF16)
                wkk = sb(st_, "wkk", [128, 2, 512], BF16)
                wkv = sb(st_, "wkv", [128, 2, 512], BF16)
                kT = sb(st_, "kT", [128, 8, SEQ], BF16)
                Ve = sb(st_, "Ve", [128, 16, 4, 128], BF16)
                Vo = sb(st_, "Vo", [128, 16, 4, 128], BF16)
                gin_bc = sb(st_, "gin_bc", [128, D], F32)
                xt = [sb(st_, "xt%d" % i, [128, D], F32) for i in range(2)]
                hn = [sb(st_, "hn%d" % i, [128, D], BF16) for i in range(2)]
                hTs = [sb(st_, "hT%d" % i, [128, 8, 512], BF16) for i in range(2)]
                scr = sb(st_, "scr", [128, 3, 512], F32)
                sq = sb(st_, "sq", [128, 3, 512], BF16)
                rq = sb(st_, "rq", [128, 512], F32)
                cqn = sb(st_, "cqn", [128, 3, 512], BF16)
                ckvn = sb(st_, "ckvn", [128, 2, 512], BF16)
                sg = sb(st_, "sg", [128, 4, 512], BF16)
                qT = sb(st_, "qT", [128, 8, 512], BF16)
                posi = sb(st_, "posi", [128, 512], I32)
                posd = sb(st_, "posd", [128, 512], I32)
                cos2 = sb(st_, "cos2", [128, 512], F32)
                sin2 = sb(st_, "sin2", [128, 512], F32)
                pT = [sb(st_, "pT%d" % i, [128, 512], BF16) for i in range(4)]
                stt_ = sb(st_, "stM", [128, 24], F32)
                junk = sq[:, 0:2, :].rearrange("p a b -> p (a b)")
                ctx = dict(xt=xt, hn=hn, st=stt_, junk=junk, hT=None, gin_bc=gin_bc)

                def stageM(gn, t):
                    stage_hT(ctx, s * SEQ + gn * 512 + t * 128, t % 2, t * 128, "vector",
                             hT=hTs[gn % 2], htag=("hT", gn % 2))

                wi = kchunks(w_in)
                load_w("wM_a", wM[:, :, 0:640], wi[:, :, 0:640])
                load_w("wM_b", wM[:, :, 640:1152], wi[:, :, 672:1184])
                load_w("wkr", wkr[:], kchunks(w_krz))
                load_w("wuq", wuq[:], kchunks(w_uq))
                load_w("wuqs", wuqs[:], kchunks(w_uqsw))
                load_w("wkk", wkk[:], kchunks(w_ukvk))
                load_w("wkv", wkv[:], kchunks(w_ukvv))
                S.dma("sync", gin_bc[:], g_in.partition_broadcast(128), [], ["gin_bc"], "gin_bc")
                S.memset("vector", Ve[:, :, :, 64:128], 1.0, ["Vones"])
                S.memset("vector", Vo[:, :, :, 0:64], 1.0, ["Vones"])
                WM = ["wM_a", "wM_b"]
                SCALE = float(96 ** -0.5)
                for t in range(4):
                    stageM(0, t)
                r64 = slice(64, 96)
                pj_rot = [1, 2, 3, 4, 7]
                pj_i = [0]

                def pj():
                    b = pj_rot[pj_i[0] % 5]
                    pj_i[0] += 1
                    return b

                for g in range(ng):
                    tok0 = s * SEQ + g * 512
                    c0g = g * 512
                    hT = hTs[g % 2]
                    HT = [("hT", g % 2, c0) for c0 in (0, 128, 256, 384)]
                    S.dma("sync", posd[r64, :], pos[0:1, tok0:tok0 + 512].partition_broadcast(32), [], ["posd"], "posd")
                    S.copy("vector", scr[r64, 0, :], posd[r64, :], ["posd"], [("scr", 0)])
                    S.ts("vector", scr[r64, 0, :], scr[r64, 0, :], cst[r64, C_ROPE:C_ROPE + 1], ALU.mult,
                         [("scr", 0), "cst"], [("scr", 0)])
                    S.copy("vector", posi[r64, :], scr[r64, 0, :], [("scr", 0)], ["posi"])
                    S.copy("vector", scr[r64, 1, :], posi[r64, :], ["posi"], [("scr", 1)])
                    S.tt("vector", scr[r64, 1, :], scr[r64, 0, :], scr[r64, 1, :], ALU.subtract,
                         [("scr", 0), ("scr", 1)], [("scr", 1)])
                    S.act(sin2[r64, :], scr[r64, 1, :], AF.Sin, [("scr", 1), "cst"], ["sin2"],
                          scale=cst[r64, C_ROPE + 1:C_ROPE + 2])
                    S.ts("vector", scr[r64, 2, :], scr[r64, 0, :], 0.25, ALU.add, [("scr", 0)], [("scr", 2)])
                    S.copy("vector", posi[r64, :], scr[r64, 2, :], [("scr", 2)], ["posi"])
                    S.copy("vector", scr[r64, 1, :], posi[r64, :], ["posi"], [("scr", 1)])
                    S.tt("vector", scr[r64, 1, :], scr[r64, 2, :], scr[r64, 1, :], ALU.subtract,
                         [("scr", 2), ("scr", 1)], [("scr", 1)])
                    S.act(cos2[r64, :], scr[r64, 1, :], AF.Sin, [("scr", 1)], ["cos2"], scale=TWO_PI)


                    def proj(col, width=128, w=wM, wn=WM, m0=0):
                        b = pj()
                        for k in range(8):
                            S.mm(ps[b][m0:m0 + width, :], w[:, k, col:col + width], hT[:, k, :],
                                 k == 0, k == 7, wn + HT, [("ps", b)])
                        return b

                    def lowrank_norm(col_base, nch, gcol0, dst, dname, inv_n):
                        for c in range(nch):
                            b = proj(col_base + c * 128)
                            S.act(sq[:, c, :], ps[b][:], AF.Square, [("ps", b)], [("sq", c)])
                            S.copy("vector", scr[:, c, :], ps[b][:], [("ps", b)], [("scr", c)])
                        b = pj()
                        for c in range(nch):
                            S.mm(ps[b][:], ones_bf[:], sq[:, c, :], c == 0, c == nch - 1,
                                 ["ones_bf", ("sq", c)], [("ps", b)])
                        if cut == 24:
                            return
                        S.act(rq[:], ps[b][:], AF.Ln, [("ps", b)], ["rq"], scale=inv_n, bias=EPS)
                        if cut == 25:
                            return
                        S.act(rq[:], rq[:], AF.Exp, ["rq"], ["rq"], scale=-0.5)
                        if cut == 26:
                            return
                        for c in range(nch):
                            S.stt("vector", dst[:, c, :], scr[:, c, :], gc[:, gcol0 + c:gcol0 + c + 1], rq[:],
                                  ALU.mult, ALU.mult, [("scr", c), "gc", "rq"], [(dname, c)])

                    if cut == 20:
                        b = proj(0)
                        S.flush()
                        return
                    if cut == 21:
                        b = proj(0)
                        S.act(sq[:, 0, :], ps[b][:], AF.Square, [("ps", b)], [("sq", 0)])
                        S.flush()
                        return
                    if cut == 22:
                        b = proj(0)
                        S.copy("vector", scr[:, 0, :], ps[b][:], [("ps", b)], [("scr", 0)])
                        S.flush()
                        return
                    lowrank_norm(0, 3, 0, cqn, "cqn", 1.0 / 384)
                    CQN = [("cqn", c) for c in range(3)]
                    if cut in (23, 24, 25, 26):
                        S.flush()
                        return
                    lowrank_norm(384, 2, 3, ckvn, "ckvn", 1.0 / 256)
                    CKV = [("ckvn", c) for c in range(2)]
                    for c in range(4):
                        b = proj(640 + c * 128)
                        S.act(sg[:, c, :], ps[b][:], AF.Silu, [("ps", b)], [("sg", c)])
                    if cut == 3:
                        S.flush()
                        return
                    ba = proj(0, 96, wkr, ["wkr"])
                    bb = proj(96, 96, wkr, ["wkr"])
                    S.tt("vector", scr[r64, 0, :], ps[ba][r64, :], cos2[r64, :], ALU.mult, [("ps", ba), "cos2"], [("scr", 0)])
                    S.tt("vector", scr[r64, 1, :], ps[bb][r64, :], sin2[r64, :], ALU.mult, [("ps", bb), "sin2"], [("scr", 1)])
                    S.tt("vector", kT[r64, 0, c0g:c0g + 512], scr[r64, 0, :], scr[r64, 1, :], ALU.add,
                         [("scr", 0), ("scr", 1)], [("kTr", 0, g)])
                    for h in range(1, 8):
                        S.copy("vector", kT[r64, h, c0g:c0g + 512], kT[r64, 0, c0g:c0g + 512],
                               [("kTr", 0, g)], [("kTr", h, g)])
                    if cut == 4:
                        S.flush()
                        return
                    for h in range(8):
                        ba, bb = pj(), pj()
                        for c in range(3):
                            S.mm(ps[ba][0:96, :], wuq[:, c, 96 * h:96 * h + 96], cqn[:, c, :], c == 0, c == 2,
                                 ["wuq"] + CQN, [("ps", ba)])
                        for c in range(3):
                            S.mm(ps[bb][0:96, :], wuqs[:, c, 96 * h:96 * h + 96], cqn[:, c, :], c == 0, c == 2,
                                 ["wuqs"] + CQN, [("ps", bb)])
                        S.copy("scalar", qT[0:64, h, :], ps[ba][0:64, :], [("ps", ba)], [("qTn", h)])
                        S.tt("vector", scr[r64, 0, :], ps[ba][r64, :], cos2[r64, :], ALU.mult, [("ps", ba), "cos2"], [("scr", 0)])
                        S.tt("vector", scr[r64, 1, :], ps[bb][r64, :], sin2[r64, :], ALU.mult, [("ps", bb), "sin2"], [("scr", 1)])
                        S.tt("vector", qT[r64, h, :], scr[r64, 0, :], scr[r64, 1, :], ALU.add,
                             [("scr", 0), ("scr", 1)], [("qTr", h)])
                    for h in range(8):
                        b = pj()
                        for c in range(2):
                            S.mm(ps[b][0:64, :], wkk[:, c, 64 * h:64 * h + 64], ckvn[:, c, :], c == 0, c == 1,
                                 ["wkk"] + CKV, [("ps", b)])
                        S.copy("scalar" if h % 2 else "vector", kT[0:64, h, c0g:c0g + 512], ps[b][0:64, :],
                               [("ps", b)], [("kTn", h, g)])
                    for t in range(4):
                        T = g * 4 + t
                        b = pj()
                        for c in range(2):
                            S.mm(ps[b][:], ckvn[:, c, t * 128:(t + 1) * 128], wkv[:, c, :], c == 0, c == 1,
                                 ["wkv"] + CKV, [("ps", b)])
                        pv = ps[b][:].rearrange("p (i two d) -> p i two d", two=2, d=64)
                        S.copy("vector", Ve[:, T, :, 0:64], pv[:, :, 0, :], [("ps", b)], [("Ve", T)])
                        S.copy("scalar", Vo[:, T, :, 64:128], pv[:, :, 1, :], [("ps", b)], [("Vo", T)])

                    if cut == 5:
                        S.flush()
                        return
                    nk = 4 * (g + 1)
                    for i in range(4):
                        hA, hB = 2 * i, 2 * i + 1
                        if g + 1 < ng:
                            stageM(g + 1, i)
                        steps = [(j, hh) for j in range(nk) for hh in (0, 1)]
                        pend = None
                        for n, (j, hh) in enumerate(steps):
                            h = hA if hh == 0 else hB
                            r = j - 4 * g
                            c0 = 128 * r if r > 0 else 0
                            gj = j // 4
                            b = pj()
                            slot = n % 4
                            S.mm(ps[b][:, c0:512], kT[0:96, h, j * 128:(j + 1) * 128], qT[0:96, h, c0:512], True, True,
                                 [("kTn", h, gj), ("kTr", h, gj), ("qTn", h), ("qTr", h)], [("ps", b)])
                            S.act(pT[slot][:, c0:512], ps[b][:, c0:512], AF.Exp, [("ps", b)], [("pT", slot)], scale=SCALE)
                            if r >= 0:
                                S.tt("vector", pT[slot][:, c0:c0 + 128], pT[slot][:, c0:c0 + 128], tri[:, 0, :], ALU.mult,
                                     [("pT", slot), "tri"], [("pT", slot)])
                            if pend is not None:
                                pend()
                            ob = 5 + hh
                            if hh == 0:
                                lhsT = Ve[:, j, i, :]
                                orow = slice(0, 128)
                                vr = [("Ve", j), "Vones"]
                            else:
                                lhsT = Vo[:, j, i, :]
                                orow = slice(0, 128)
                                vr = [("Vo", j), "Vones"]

                            def pend(ob=ob, orow=orow, c0=c0, lhsT=lhsT, slot=slot, j=j, vr=vr):
                                S.mm(ps[ob][orow, c0:512], lhsT, pT[slot][:, c0:512], j == 0, j == nk - 1,
                                     vr + [("pT", slot)], [("ps", ob)])
                        pend()
                        if cut == 6:
                            S.flush()
                            return
                        S.act(scr[64:128, 0, :], ps[5][64:128, :], AF.Ln, [("ps", 5)], [("scr", 0, "a")])
                        S.act(scr[64:128, 0, :], scr[64:128, 0, :], AF.Exp, [("scr", 0, "a")], [("scr", 0, "a")], scale=-1.0)
                        S.act(scr[0:64, 0, :], ps[6][0:64, :], AF.Ln, [("ps", 6)], [("scr", 0, "b")])
                        S.act(scr[0:64, 0, :], scr[0:64, 0, :], AF.Exp, [("scr", 0, "b")], [("scr", 0, "b")], scale=-1.0)
                        S.copy("vector", scr[0:64, 2, :], scr[64:128, 0, :], [("scr", 0, "a")], [("scr", 2)])
                        S.copy("vector", scr[64:128, 2, :], scr[0:64, 0, :], [("scr", 0, "b")], [("scr", 2)])
                        S.tt("vector", scr[:, 1, :], scr[:, 2, :], sg[:, i, :], ALU.mult, [("scr", 2), ("sg", i)], [("scr", 1)])
                        S.tt("vector", uaT[0:64, i, c0g:c0g + 512], ps[5][0:64, :], scr[0:64, 1, :], ALU.mult,
                             [("ps", 5), ("scr", 1)], [("uaT", i, g)])
                        S.tt("vector", uaT[64:128, i, c0g:c0g + 512], ps[6][64:128, :], scr[64:128, 1, :], ALU.mult,
                             [("ps", 6), ("scr", 1)], [("uaT", i, g)])
                if debug:
                    S.dma("sync", dbg_ua[s][:, :, 0:ng * 512], uaT[:, :, 0:ng * 512], [("uaT", i, g) for i in range(4) for g in range(ng)], ["dbg_ua"], "dbg")
                S.flush()

        def pass_G(s, pre):
            with contextlib.ExitStack() as st_:
                wG = sb(st_, "wG", [128, 8, 1536], BF16)
                wgl = sb(st_, "wgl", [128, 8, 16], BF16)
                wg17 = sb(st_, "wg17", [128, 256], BF16)
                gin_bc = sb(st_, "gin_bcG", [128, D], F32)
                ggla_bc = sb(st_, "ggla_bc", [128, 128], F32)
                xt = [sb(st_, "xtG%d" % i, [128, D], F32) for i in range(2)]
                hn = [sb(st_, "hnG%d" % i, [128, D], BF16) for i in range(2)]
                hTs = [sb(st_, "hTG%d" % i, [128, 8, 512], BF16) for i in range(2)]
                junk_t = sb(st_, "junkG", [128, D], BF16)
                gqf = sb(st_, "gqf", [128, 4, 512], F32)
                gkf = sb(st_, "gkf", [128, 4, 512], F32)
                sgg = sb(st_, "sgg", [128, 4, 512], BF16)
                gkl = sb(st_, "gkl", [128, 512], BF16)
                vsb = [sb(st_, "vsb%d" % i, [128, 512], BF16) for i in range(2)]
                etmp = sb(st_, "etmp", [128, 256], F32)
                sp = sb(st_, "sp", [128, 256], F32)
                ebT = sb(st_, "ebT", [128, 4, 128], F32)
                enbT = sb(st_, "enbT", [128, 4, 128], F32)
                erev = sb(st_, "erev", [128, 256], F32)
                qtT = sb(st_, "qtT", [128, 4, 128], BF16)
                ktT = sb(st_, "ktT", [128, 4, 128], BF16)
                kdec = sb(st_, "kdec", [128, 256], BF16)
                AT = sb(st_, "AT", [128, 4, 128], BF16)
                onsb = sb(st_, "onsb", [128, 512], BF16)
                Sf = sb(st_, "Sf", [128, 4, 128], F32)
                Sb = sb(st_, "Sb", [128, 4, 128], BF16)
                junk2 = sb(st_, "junk2", [128, 128], BF16)
                stt_ = sb(st_, "stG", [128, 40], F32)
                ctx = dict(xt=xt, hn=hn, st=stt_, junk=junk_t[:], hT=None, gin_bc=gin_bc)
                r0_ = slice(0, 64)

                def stageG(gn, t, parts="LST"):
                    stage_hT(ctx, s * SEQ + gn * 512 + t * 128, t % 2, t * 128, "scalar" if t % 2 else "vector",
                             hT=hTs[gn % 2], htag=("hT", gn % 2), parts=parts)

                wi = kchunks(w_in)
                load_w("wG_a", wG[:, :, 0:1024], wi[:, :, 1184:2208])
                load_w("wG_b", wG[:, :, 1024:1536], wi[:, :, 2224:2736])
                load_w("wgl", wgl[:], wi[:, :, 2208:2224])
                load_w("wg17", wg17[0:17, :], w_gk17)
                load_w("wP", pre[0][:], wi[:, :, 2736:4784])
                load_w("wo", pre[1][:], kchunks(w_out))
                load_w("wpg", pre[2][:], kchunks(w_pg))
                S.dma("sync", gin_bc[:], g_in.partition_broadcast(128), [], ["gin_bc"], "gin_bc")
                S.dma("sync", ggla_bc[:], g_gla.partition_broadcast(128), [], ["ggla_bc"], "ggla_bc")
                S.memset("vector", gkl[:], 1.0, ["gkl"])
                S.memset("vector", Sf[:], 0.0, ["Sf"])
                S.memset("vector", Sb[:], 0.0, ["Sb"])
                WG = ["wG_a", "wG_b"]
                pj_i = [0]

                def pj():
                    b = (1, 2, 3, 4, 5, 6)[pj_i[0] % 6]
                    pj_i[0] += 1
                    return b

                for t in range(4):
                    stageG(0, t)
                for g in range(ng):
                    tok0 = s * SEQ + g * 512
                    c0g = g * 512
                    hT = hTs[g % 2]
                    HT = [("hT", g % 2, c0) for c0 in (0, 128, 256, 384)]

                    def proj(col, width=128, w=wG, wn=WG):
                        b = pj()
                        for k in range(8):
                            S.mm(ps[b][0:width, :], w[:, k, col:col + width], hT[:, k, :], k == 0, k == 7,
                                 wn + HT, [("ps", b)])
                        return b

                    for c in range(2):
                        b = proj(c * 128)
                        S.copy("scalar", gqf[r0_, 2 * c, :], ps[b][0:64, :], [("ps", b)], [("gqf", 2 * c)])
                        S.copy("vector", gqf[r0_, 2 * c + 1, :], ps[b][64:128, :], [("ps", b)], [("gqf", 2 * c + 1)])
                    for c in range(2):
                        b = proj(256 + c * 128)
                        S.copy("scalar", gkf[r0_, 2 * c, :], ps[b][0:64, :], [("ps", b)], [("gkf", 2 * c)])
                        S.copy("vector", gkf[r0_, 2 * c + 1, :], ps[b][64:128, :], [("ps", b)], [("gkf", 2 * c + 1)])
                    GQ = [("gqf", h) for h in range(4)]
                    GK = [("gkf", h) for h in range(4)]
                    for c in range(4):
                        b = proj(1024 + c * 128)
                        S.act(sgg[:, c, :], ps[b][:], AF.Silu, [("ps", b)], [("sgg", c)])
                    b = proj(0, 16, wgl, ["wgl"])
                    S.copy("vector", gkl[0:16, :], ps[b][0:16, :], [("ps", b)], ["gkl"])

                    def F(t):
                        cs = slice(t * 128, (t + 1) * 128)
                        vs = t % 2
                        htr = [("hT", g % 2, t * 128)]
                        for k in range(8):
                            S.mm(ps[3][:], hT[:, k, cs], wG[:, k, 512:1024], k == 0, k == 7, WG + htr, [("ps", 3)])
                        S.copy("scalar", vsb[vs][:], ps[3][:], [("ps", 3)], [("vsb", vs)])
                        for k in range(8):
                            S.mm(ps[4][:, 0:256], hT[:, k, cs], wG[:, k, 256:512], k == 0, k == 7, WG + htr, [("ps", 4)])
                        S.mm(ps[4][:, 256:512], gkl[0:17, cs], wg17[0:17, :], True, True, ["gkl", "wg17"], [("ps", 4)])
                        S.act(etmp[:], ps[4][:, 256:512], AF.Exp, [("ps", 4)], ["etmp"], scale=-1.0)
                        S.act(sp[:], etmp[:], AF.Ln, ["etmp"], ["sp"], bias=1.0)
                        for h in range(4):
                            S.mm(ps[5][0:64, h * 128:(h + 1) * 128], sp[:, h * 64:(h + 1) * 64], Lmat, True, True,
                                 ["sp", "cst"], [("ps", 5)])
                        S.mm(ps[2][:, 0:256], Umat, sp[:], True, True, ["sp", "cst"], [("ps", 2)])
                        bt = ps[5][0:64, :].rearrange("p (h t) -> p h t", h=4)
                        S.act(ebT[r0_], bt, AF.Exp, [("ps", 5)], ["ebT"])
                        S.act(enbT[r0_], bt, AF.Exp, [("ps", 5)], ["enbT"], scale=-1.0)
                        S.act(erev[:], ps[2][:, 0:256], AF.Exp, [("ps", 2)], ["erev"])
                        S.stt("vector", qtT[r0_], gqf[r0_, :, cs], 0.125, ebT[r0_], ALU.mult, ALU.mult, GQ + ["ebT"], ["qtT"])
                        S.tt("vector", ktT[r0_], gkf[r0_, :, cs], enbT[r0_], ALU.mult, GK + ["enbT"], ["ktT"])
                        S.tt("vector", kdec[:], ps[4][:, 0:256], erev[:], ALU.mult, [("ps", 4), "erev"], ["kdec"])
                        for h in range(4):
                            S.mm(ps[6][:, h * 128:(h + 1) * 128], ktT[r0_, h, :], qtT[r0_, h, :], True, True,
                                 ["ktT", "qtT"], [("ps", 6)])
                        S.tt("vector", AT[:], ps[6][:].rearrange("p (h t) -> p h t", h=4), tri[:], ALU.mult,
                             [("ps", 6), "tri"], ["AT"])
                    def O(t):
                        cs = slice(t * 128, (t + 1) * 128)
                        vs = t % 2
                        for h in range(4):
                            hs_ = slice(h * 128, (h + 1) * 128)
                            S.mm(ps[7][:, hs_], AT[:, h, :], vsb[vs][:, hs_], True, False, ["AT", ("vsb", vs)], [("ps", 7)])
                            S.mm(ps[7][:, hs_], qtT[r0_, h, :], Sb[r0_, h, :], False, True, ["qtT", "Sb"], [("ps", 7)])
                        for h in range(4):
                            hs_ = slice(h * 128, (h + 1) * 128)
                            S.mm(ps[1][0:64, hs_], kdec[:, h * 64:(h + 1) * 64], vsb[vs][:, hs_], True, True,
                                 ["kdec", ("vsb", vs)], [("ps", 1)])
                        for h in range(4):
                            hs_ = slice(h * 128, (h + 1) * 128)
                            S.stt("vector", Sf[r0_, h, :], Sf[r0_, h, :], ebT[r0_, h, 127:128], ps[1][0:64, hs_], ALU.mult, ALU.add,
                                  ["Sf", "ebT", ("ps", 1)], ["Sf"])
                        S.copy("scalar", Sb[r0_], Sf[r0_], ["Sf"], ["Sb"])
                    def N(t):
                        cs = slice(t * 128, (t + 1) * 128)
                        vs = t % 2
                        for h in range(4):
                            S.act(junk2[:], ps[7][:, h * 128:(h + 1) * 128], AF.Square, [("ps", 7)], ["junk2", "oss"],
                                  accum=stt_[:, 24 + h:25 + h])
                        S.act(stt_[:, 28:32], stt_[:, 24:28], AF.Ln, ["oss"], ["oln"], scale=1.0 / 128, bias=EPS)
                        S.act(stt_[:, 32:36], stt_[:, 28:32], AF.Exp, ["oln"], ["ors"], scale=-0.5)
                        for h in range(4):
                            hs_ = slice(h * 128, (h + 1) * 128)
                            S.stt("vector", onsb[:, hs_], ps[7][:, hs_], stt_[:, 32 + h:33 + h], ggla_bc[:], ALU.mult, ALU.mult,
                                  [("ps", 7), "ors", "ggla_bc"], ["onsb"])
                        for h in range(4):
                            hs_ = slice(h * 128, (h + 1) * 128)
                            S.tr(tpv[:, hs_], onsb[:, hs_], ident[:], ["onsb", "ident"], [("ps", 0)])
                        S.tt("vector", ubT[:, :, c0g + t * 128:c0g + (t + 1) * 128],
                             tpv[:, 0:512].rearrange("p (h t) -> p h t", h=4), sgg[:, :, cs], ALU.mult,
                             [("ps", 0)] + [("sgg", c) for c in range(4)], [("ubT", g, t)])
                    F(0)
                    for t in range(4):
                        if g + 1 < ng:
                            stageG(g + 1, t, "L")
                        O(t)
                        if t + 1 < 4:
                            F(t + 1)
                        N(t)
                        if g + 1 < ng:
                            stageG(g + 1, t, "ST")
                if debug:
                    S.dma("sync", dbg_ub[s][:, :, 0:ng * 512], ubT[:, :, 0:ng * 512], [("ubT", g, t) for g in range(ng) for t in range(4)], ["dbg_ub"], "dbg")
                S.flush()

        def pass_P(s, pre):
            wP, wo, wpg = pre
            with contextlib.ExitStack() as st_:
                wml = sb(st_, "wml", [128, 4, D], BF16)
                wgl_ = sb(st_, "wglb", [128, 4, D], BF16)
                wpl = sb(st_, "wpl", [128, 2, D], BF16)
                gin_bc = sb(st_, "gin_bcP", [128, D], F32)
                gpg_bc = sb(st_, "gpg_bc", [128, D], F32)
                gple_bc = sb(st_, "gple_bc", [128, D], F32)
                gfin_bc = sb(st_, "gfin_bc", [128, D], F32)
                NX = 6
                xt = [sb(st_, "xtP%d" % i, [128, D], F32) for i in range(NX)]
                hn = [sb(st_, "hnP%d" % i, [128, D], BF16) for i in range(1)]
                hT = sb(st_, "hTP", [128, 8, 512], BF16)
                junk_t = sb(st_, "junkP", [128, D], BF16)
                sa = [sb(st_, "sa%d" % i, [128, 512], BF16) for i in range(2)]
                sbb = [sb(st_, "sbb%d" % i, [128, 512], BF16) for i in range(2)]
                t1 = [sb(st_, "t1_%d" % i, [128, 512], BF16) for i in range(2)]
                t2 = [sb(st_, "t2_%d" % i, [128, 512], BF16) for i in range(2)]
                mg = sb(st_, "mg", [128, 8, 512], BF16)
                x1n = sb(st_, "x1n", [128, D], BF16)
                x1nT = sb(st_, "x1nT", [128, 8, 128], BF16)
                pbf = [sb(st_, "pbf%d" % i, [128, 256], BF16) for i in range(2)]
                pT_ = sb(st_, "pTP", [128, 2, 128], BF16)
                sig = sb(st_, "sig", [128, 512], F32)
                et = sb(st_, "et", [128, D], F32)
                ot = [sb(st_, "ot%d" % i, [128, D], F32) for i in range(2)]
                stt_ = sb(st_, "stP", [128, 48], F32)
                ctx = dict(xt=xt, hn=hn, st=stt_, junk=junk_t[:], hT=hT, gin_bc=gin_bc)

                load_w("wml", wml[:], kchunks(w_mla))
                load_w("wglb", wgl_[:], kchunks(w_gla))
                load_w("wpl", wpl[:], kchunks(w_ple))
                S.dma("sync", gin_bc[:], g_in.partition_broadcast(128), [], ["gin_bc"], "gin_bc")
                S.dma("sync", gpg_bc[:], g_pg.partition_broadcast(128), [], ["gpg_bc"], "gpg_bc")
                S.dma("sync", gple_bc[:], g_ple.partition_broadcast(128), [], ["gple_bc"], "gple_bc")
                S.dma("sync", gfin_bc[:], g_fin.partition_broadcast(128), [], ["gfin_bc"], "gfin_bc")
                pj_i = [0]
                rot = [1, 2, 3, 4, 5]

                def pj():
                    b = rot[pj_i[0] % 5]
                    pj_i[0] += 1
                    return b

                HT = [("hT", c0) for c0 in (0, 128, 256, 384)]

                def stageP(gn, t, parts="LST"):
                    stage_hT(ctx, s * SEQ + gn * 512 + t * 128, (4 * gn + t) % NX, t * 128, "vector", parts=parts)

                for t in range(4):
                    stageP(0, t)
                for g in range(ng):
                    tok0 = s * SEQ + g * 512
                    c0g = g * 512
                    for m in range(8):
                        sl = m % 2
                        ba = pj()
                        for k in range(8):
                            S.mm(ps[ba][:], wP[:, k, m * 128:(m + 1) * 128], hT[:, k, :], k == 0, k == 7, ["wP"] + HT, [("ps", ba)])
                        S.act(sa[sl][:], ps[ba][:], AF.Sigmoid, [("ps", ba)], [("sa", sl)])
                        by = pj()
                        for c in range(4):
                            S.mm(ps[by][:], wml[:, c, m * 128:(m + 1) * 128], uaT[:, c, c0g:c0g + 512], c == 0, c == 3, ["wml"], [("ps", by)])
                        S.tt("vector", t1[sl][:], ps[by][:], sa[sl][:], ALU.mult, [("ps", by), ("sa", sl)], [("t1", sl)])
                        bb = pj()
                        for k in range(8):
                            S.mm(ps[bb][:], wP[:, k, 1024 + m * 128:1024 + (m + 1) * 128], hT[:, k, :], k == 0, k == 7, ["wP"] + HT, [("ps", bb)])
                        S.act(sbb[sl][:], ps[bb][:], AF.Sigmoid, [("ps", bb)], [("sbb", sl)])
                        bz = pj()
                        for c in range(4):
                            S.mm(ps[bz][:], wgl_[:, c, m * 128:(m + 1) * 128], ubT[:, c, c0g:c0g + 512], c == 0, c == 3, ["wglb"], [("ps", bz)])
                        S.tt("vector", t2[sl][:], ps[bz][:], sbb[sl][:], ALU.mult, [("ps", bz), ("sbb", sl)], [("t2", sl)])
                        S.tt("gpsimd", mg[:, m, :], t1[sl][:], t2[sl][:], ALU.add, [("t1", sl), ("t2", sl)], [("mg", m)])
                    MG = [("mg", m) for m in range(8)]

                    def xs_of(t):
                        return (4 * g + t) % NX

                    def A1(t):
                        xs = xs_of(t)
                        cs = slice(t * 128, (t + 1) * 128)
                        for half in range(2):
                            b = 4 + half
                            hs_ = slice(half * 512, (half + 1) * 512)
                            for m in range(8):
                                S.mm(ps[b][:], mg[:, m, cs], wo[:, m, hs_], m == 0, m == 7, ["wo"] + MG, [("ps", b)])
                            S.tt("vector", xt[xs][:, hs_], xt[xs][:, hs_], ps[b][:], ALU.add, [("xt", xs), ("ps", b)], [("xt", xs)])
                        S.act(junk_t[:], xt[xs][:], AF.Square, [("xt", xs)], ["junk", "s1"], accum=stt_[:, 24:25])
                        S.act(stt_[:, 25:26], stt_[:, 24:25], AF.Ln, ["s1"], ["l1"], scale=1.0 / D, bias=EPS)
                        S.act(stt_[:, 26:27], stt_[:, 25:26], AF.Exp, ["l1"], ["r1"], scale=-0.5)

                    def A2(t):
                        xs = xs_of(t)
                        S.stt("vector", x1n[:], xt[xs][:], stt_[:, 26:27], gpg_bc[:], ALU.mult, ALU.mult, [("xt", xs), "r1", "gpg_bc"], ["x1n"])

                    def B(t):
                        tok = tok0 + t * 128
                        ps_ = t % 2
                        S.dma("gpsimd", pbf[ps_][:], pin[tok:tok + 128, :], [], [("pbf", ps_)], ("pbf", ps_))
                        for c in range(2):
                            S.tr(tpv[:, c * 128:(c + 1) * 128], pbf[ps_][:, c * 128:(c + 1) * 128], ident[:], [("pbf", ps_), "ident"], [("ps", 0)])
                        S.copy("vector", pT_[:], tpv[:, 0:256].rearrange("p (c t) -> p c t", c=2), [("ps", 0)], ["pTP"])
                        for half in range(2):
                            b = 1 + half
                            for c in range(2):
                                S.mm(ps[b][:], pT_[:, c, :], wpl[:, c, half * 512:(half + 1) * 512], c == 0, c == 1, ["wpl", "pTP"], [("ps", b)])
                            S.act(junk_t[:, 0:512], ps[b][:], AF.Square, [("ps", b)], ["junk", ("se", half)], accum=stt_[:, 27 + half:28 + half])
                        S.tt("vector", stt_[:, 29:30], stt_[:, 27:28], stt_[:, 28:29], ALU.add, [("se", 0), ("se", 1)], ["se2"])
                        S.act(stt_[:, 30:31], stt_[:, 29:30], AF.Ln, ["se2"], ["le"], scale=1.0 / D, bias=EPS)
                        S.act(stt_[:, 31:32], stt_[:, 30:31], AF.Exp, ["le"], ["re"], scale=-0.5)
                        for half in range(2):
                            b = 1 + half
                            hs_ = slice(half * 512, (half + 1) * 512)
                            S.stt("vector", et[:, hs_], ps[b][:], stt_[:, 31:32], gple_bc[:, hs_], ALU.mult, ALU.mult,
                                  [("ps", b), "re", "gple_bc"], [("et", half)])

                    def C1(t):
                        for c in range(8):
                            S.tr(tpv[:, c * 128:(c + 1) * 128], x1n[:, c * 128:(c + 1) * 128], ident[:], ["x1n", "ident"], [("ps", 0)])
                        S.copy("scalar", x1nT[:], tpv.rearrange("p (c t) -> p c t", c=8), [("ps", 0)], ["x1nT"])

                    def C2a(t):
                        for half in range(2):
                            b = 6 + half
                            hs_ = slice(half * 512, (half + 1) * 512)
                            for c in range(8):
                                S.mm(ps[b][:], x1nT[:, c, :], wpg[:, c, hs_], c == 0, c == 7, ["wpg", "x1nT"], [("ps", b)])

                    def C2b(t):
                        tok = tok0 + t * 128
                        xs = xs_of(t)
                        osl = t % 2
                        for half in range(2):
                            b = 6 + half
                            hs_ = slice(half * 512, (half + 1) * 512)
                            S.act(sig[:], ps[b][:], AF.Sigmoid, [("ps", b)], ["sig"])
                            S.tt("vector", et[:, hs_], et[:, hs_], sig[:], ALU.mult, [("et", half), "sig"], [("et", half)])
                        S.tt("vector", xt[xs][:], xt[xs][:], et[:], ALU.add, [("xt", xs), ("et", 0), ("et", 1)], [("xt", xs)])
                        S.act(junk_t[:], xt[xs][:], AF.Square, [("xt", xs)], ["junk", "s2"], accum=stt_[:, 32:33])
                        S.act(stt_[:, 33:34], stt_[:, 32:33], AF.Ln, ["s2"], ["l2"], scale=1.0 / D, bias=EPS)
                        S.act(stt_[:, 34:35], stt_[:, 33:34], AF.Exp, ["l2"], ["r2"], scale=-0.5)
                        S.stt("vector", ot[osl][:], xt[xs][:], stt_[:, 34:35], gfin_bc[:], ALU.mult, ALU.mult,
                              [("xt", xs), "r2", "gfin_bc"], [("ot", osl)])
                        S.dma("sync", out[tok:tok + 128, :], ot[osl][:], [("ot", osl)], [("out", tok)], ("ot", osl))

                    nxt = g + 1 < ng
                    A1(0)
                    A2(0)
                    for t in range(4):
                        if nxt:
                            if t == 0:
                                stageP(g + 1, 0, "L")
                            if t + 1 < 4:
                                stageP(g + 1, t + 1, "L")
                        C1(t)
                        if t + 1 < 4:
                            A1(t + 1)
                            A2(t + 1)
                        B(t)
                        if nxt:
                            stageP(g + 1, t, "S")
                        C2a(t)
                        if nxt:
                            stageP(g + 1, t, "T")
                        C2b(t)
                S.flush()

        for s in range(nseq):
            if "M" in passes:
                pass_M(s)
            with contextlib.ExitStack() as pw:
                pre = (sb(pw, "wP", [128, 8, 2048], BF16), sb(pw, "wo", [128, 8, D], BF16), sb(pw, "wpg", [128, 8, D], BF16))
                if "G" in passes:
                    pass_G(s, pre)
                if "P" in passes:
                    pass_P(s, pre)
        if S.ops:
            S.flush()
    return nc


def _consts():
    c = np.zeros((128, C_END), np.float32)
    idx = np.arange(128)
    c[:, C_ID:C_ID + 128] = np.eye(128, dtype=np.float32)
    triu = (idx[None, :] >= idx[:, None]).astype(np.float32)
    c[:, C_TRI:C_TRI + 128] = triu
    c[:, C_L:C_L + 128] = -triu / 16.0
    c[:, C_U:C_U + 128] = -(idx[:, None] > idx[None, :]).astype(np.float32) / 16.0
    c[64, C_SEL:C_SEL + 64] = 1.0
    c[0, C_SEL + 64:C_SEL + 128] = 1.0
    inv_freq = 1.0 / (10000.0 ** (np.arange(0, 32, 2, dtype=np.float64) / 32.0))
    for r in range(32):
        c[64 + r, C_ROPE] = inv_freq[r % 16] / (2.0 * np.pi)
        c[64 + r, C_ROPE + 1] = -TWO_PI if r < 16 else TWO_PI
    return c


_NC_CACHE = {}


def _prep_shared(inp):
    f = lambda a: np.ascontiguousarray(np.asarray(a, dtype=np.float32))
    w_in = f(inp["w_in"][0])
    kr = w_in[:, 640:672]
    w_krz = np.zeros((D, 192), np.float32)
    w_krz[:, 64:96] = kr
    w_krz[:, 96 + 64:96 + 80] = kr[:, 16:32]
    w_krz[:, 96 + 80:96 + 96] = kr[:, 0:16]
    w_uq = f(inp["w_uq"][0])
    w_uqsw = np.zeros((384, 768), np.float32)
    for h in range(8):
        rp = w_uq[:, 96 * h + 64:96 * h + 96]
        w_uqsw[:, 96 * h + 64:96 * h + 80] = rp[:, 16:32]
        w_uqsw[:, 96 * h + 80:96 * h + 96] = rp[:, 0:16]
    w_ukv = f(inp["w_ukv"][0]).reshape(256, 8, 2, 64)
    w_ukvk = np.ascontiguousarray(w_ukv[:, :, 0, :].reshape(256, 512))
    w_ukvv = np.ascontiguousarray(w_ukv[:, :, 1, :].reshape(256, 512))
    w_gk17 = np.concatenate([f(inp["w_gk_up"][0]), f(inp["b_gk"][0]).reshape(1, 256)], axis=0)
    gcols = np.zeros((128, 8), np.float32)
    gcols[:, 0:3] = f(inp["q_norm_g"][0]).reshape(3, 128).T
    gcols[:, 3:5] = f(inp["kv_norm_g"][0]).reshape(2, 128).T
    return {
        "w_in": w_in, "w_krz": w_krz, "w_uq": w_uq, "w_uqsw": w_uqsw, "w_ukvk": w_ukvk, "w_ukvv": w_ukvv,
        "w_gk17": np.ascontiguousarray(w_gk17), "w_mla": f(inp["w_mla_br"][0]), "w_gla": f(inp["w_gla_br"][0]),
        "w_out": f(inp["w_out"][0]), "w_ple": f(inp["w_ple"][0]), "w_pg": f(inp["w_ple_gate"][0]),
        "g_in": f(inp["norm_in_g"][0]).reshape(1, D), "g_gla": f(inp["gla_norm_g"][0]).reshape(1, 128),
        "g_ple": f(inp["ple_norm_g"][0]).reshape(1, D), "g_pg": f(inp["ple_gate_norm_g"][0]).reshape(1, D),
        "g_fin": f(inp["final_norm_g"]).reshape(1, D), "gcols": gcols, "consts": _consts(),
    }


def kernel(**inputs):
    if "nc" not in _NC_CACHE:
        _NC_CACHE["nc"] = build(False)
    nc = _NC_CACHE["nc"]
    shared = _prep_shared(inputs)
    x = np.asarray(inputs["x"], dtype=np.float32)
    p = np.asarray(inputs["p"], dtype=np.float32)[0]
    pos = np.asarray(inputs["positions"], dtype=np.int32)
    in_maps = []
    for c in range(NCORES):
        m = dict(shared)
        m["x"] = np.ascontiguousarray(x[2 * c:2 * c + 2].reshape(2 * SEQ, D))
        m["p"] = np.ascontiguousarray(p[2 * c:2 * c + 2].reshape(2 * SEQ, 256))
        m["pos"] = np.ascontiguousarray(pos[2 * c:2 * c + 2].reshape(1, 2 * SEQ))
        in_maps.append(m)
    res = run_bass_kernel_spmd(nc, in_maps, core_ids=list(range(NCORES)))
    outs = [np.asarray(r["out"]).reshape(2, SEQ, D) for r in res.results]
    return np.concatenate(outs, axis=0).astype(np.float32)
```
